# Optimizing a Trainium2 kernel written in Bass

```python
import jax, jax.numpy as jnp
from jax import lax
import numpy as np

D_MODEL = 1024
BATCH = 8
SEQ = 2048
DEPTH = 2

GRID_W = 64
CTX_LEN = 256
HEAD_DIM = 64
N_HEADS_TOTAL = D_MODEL // HEAD_DIM
RET_HEADS = N_HEADS_TOTAL // 4
MLSTM_HEADS = N_HEADS_TOTAL // 4
NA_HEADS = N_HEADS_TOTAL - RET_HEADS - MLSTM_HEADS
RET_W = RET_HEADS * HEAD_DIM
MLSTM_W = MLSTM_HEADS * HEAD_DIM
NA_W = NA_HEADS * HEAD_DIM
D_MIX = RET_W + MLSTM_W + NA_W
CHUNK = 128
MLSTM_CONV = 3
NA_WIN_R = 8
NA_WIN_C = 16
ROPE_BASE = 10000.0
D_FF = 2816
N_EXPERTS = 8
TOP_K = 2
N_DENSE = (DEPTH + 1) // 2
N_MOE = DEPTH // 2
EPS = 1e-6
IN_SIZES = (RET_W,) * 4 + (MLSTM_W,) * 4 + (4 * MLSTM_HEADS,) + (NA_W,) * 3
IN_SPLITS = tuple(int(s) for s in np.cumsum(IN_SIZES)[:-1])
D_IN = int(sum(IN_SIZES))

kernel_name = "hybrid_ret_mlstm_natten_moe_dit"

F32 = jnp.float32


def _identity(a):
    return a


def _flip(a):
    return a[:, ::-1]


def rmsnorm(x, g):
    x32 = x.astype(F32)
    y = x32 * lax.rsqrt(jnp.mean(x32 * x32, axis=-1, keepdims=True) + EPS)
    return (y * g.astype(F32)).astype(x.dtype)


def head_rms(y):
    y32 = y.astype(F32)
    return y32 * lax.rsqrt(jnp.mean(y32 * y32, axis=-1, keepdims=True) + EPS)


def qk_norm(y, g):
    return (head_rms(y) * g.astype(F32)).astype(y.dtype)


def _rotate(x, ang):
    x1, x2 = jnp.split(x, 2, axis=-1)
    cos = jnp.cos(ang)[None, :, None, :].astype(x.dtype)
    sin = jnp.sin(ang)[None, :, None, :].astype(x.dtype)
    return jnp.concatenate([x1 * cos - x2 * sin, x1 * sin + x2 * cos], axis=-1)


def rope_2d(x):
    T = x.shape[1]
    t = jnp.arange(T)
    row = (t // GRID_W).astype(F32)
    col = (t % GRID_W).astype(F32)
    half = HEAD_DIM // 2
    inv = ROPE_BASE ** (-jnp.arange(0, half, 2, dtype=F32) / half)
    xr = _rotate(x[..., :half], row[:, None] * inv[None, :])
    xc = _rotate(x[..., half:], col[:, None] * inv[None, :])
    return jnp.concatenate([xr, xc], axis=-1)


def dwconv_centered(x, w, b):
    K = w.shape[0]
    y = lax.conv_general_dilated(x, w[:, None, :].astype(x.dtype), window_strides=(1,),
                                 padding=[((K - 1) // 2, K // 2)],
                                 dimension_numbers=('NWC', 'WIO', 'NWC'),
                                 feature_group_count=x.shape[-1])
    return y + b.astype(x.dtype)


def _chunks(a):
    B, T, H, d = a.shape
    return a.reshape(B, T // CHUNK, CHUNK, H, d).transpose(1, 0, 3, 2, 4).astype(F32)


def _unchunks(a):
    n, B, H, C, d = a.shape
    return a.transpose(1, 0, 3, 2, 4).reshape(B, n * C, H, d)


def _gate_chunks(a):
    B, T, H = a.shape
    return a.reshape(B, T // CHUNK, CHUNK, H).transpose(1, 0, 3, 2).astype(F32)


def retention_scan(q, k, v, log_gamma, s0, with_output):
    d = q.shape[-1]
    qc, kc, vc = _chunks(q), _chunks(k * (d ** -0.5)), _chunks(v)
    lg = log_gamma.astype(F32)
    idx = jnp.arange(CHUNK, dtype=F32)
    diff = idx[:, None] - idx[None, :]
    intra = jnp.where(diff >= 0, jnp.exp(lg[:, None, None] * jnp.maximum(diff, 0.0)), 0.0)
    q_dec = jnp.exp(lg[:, None] * (idx[None, :] + 1.0))
    k_dec = jnp.exp(lg[:, None] * (CHUNK - 1.0 - idx[None, :]))
    c_dec = jnp.exp(lg * CHUNK)

    def step(s, inp):
        qj, kj, vj = inp
        s_new = c_dec[None, :, None, None] * s + jnp.einsum(
            'bhcd,bhce->bhde', kj * k_dec[None, :, :, None], vj)
        if not with_output:
            return s_new, None
        att = jnp.einsum('bhid,bhjd->bhij', qj, kj) * intra[None]
        o = jnp.einsum('bhij,bhje->bhie', att, vj) + jnp.einsum(
            'bhid,bhde->bhie', qj * q_dec[None, :, :, None], s)
        return s_new, o

    s_final, o = lax.scan(step, s0.astype(F32), (qc, kc, vc))
    return (_unchunks(o) if with_output else None), s_final


def mlstm_scan(q, k, v, i_pre, logf, state, with_output):
    d = q.shape[-1]
    qc, kc, vc = _chunks(q), _chunks(k * (d ** -0.5)), _chunks(v)
    ic, fc = _gate_chunks(i_pre), _gate_chunks(logf)
    causal = jnp.tril(jnp.ones((CHUNK, CHUNK), dtype=bool))

    def step(carry, inp):
        C_, n_, m_ = carry
        qj, kj, vj, ij, fj = inp
        b = jnp.cumsum(fj, axis=-1)
        b_last = b[..., -1]
        w_state = b_last[..., None] - b + ij
        m_new = jnp.maximum(b_last + m_, jnp.max(w_state, axis=-1))
        decay_prev = jnp.exp(b_last + m_ - m_new)
        wk = jnp.exp(w_state - m_new[..., None])
        C_new = decay_prev[..., None, None] * C_ + jnp.einsum('bhc,bhcd,bhce->bhde', wk, kj, vj)
        n_new = decay_prev[..., None] * n_ + jnp.einsum('bhc,bhcd->bhd', wk, kj)
        if not with_output:
            return (C_new, n_new, m_new), None
        Dm = jnp.where(causal, b[..., :, None] - b[..., None, :] + ij[..., None, :], -jnp.inf)
        inter = b + m_[..., None]
        m_row = jnp.maximum(jnp.max(Dm, axis=-1), inter)
        s = jnp.einsum('bhid,bhjd->bhij', qj, kj) * jnp.exp(Dm - m_row[..., None])
        g = jnp.exp(inter - m_row)
        num = jnp.einsum('bhij,bhje->bhie', s, vj) + g[..., None] * jnp.einsum('bhid,bhde->bhie', qj, C_)
        den = jnp.sum(s, axis=-1) + g * jnp.einsum('bhid,bhd->bhi', qj, n_)
        h = num / jnp.maximum(jnp.abs(den), jnp.exp(-m_row))[..., None]
        return (C_new, n_new, m_new), h

    st, h = lax.scan(step, state, (qc, kc, vc, ic, fc))
    return (_unchunks(h) if with_output else None), st


def bidir_retention(q, k, v, q_c, k_c, v_c, log_gamma, with_ctx_out):
    B, _, H, d = q.shape
    s0 = jnp.zeros((B, H, d, d), F32)
    y, y_c = 0.0, 0.0
    for dn in range(2):
        f = _identity if dn == 0 else _flip
        oc, sc = retention_scan(f(q_c), f(k_c), f(v_c), log_gamma[dn], s0, with_ctx_out)
        ox, _ = retention_scan(f(q), f(k), f(v), log_gamma[dn], sc, True)
        y = y + f(ox)
        if with_ctx_out:
            y_c = y_c + f(oc)
    return y, (y_c if with_ctx_out else None)


def bidir_mlstm(q, k, v, g, q_c, k_c, v_c, g_c, with_ctx_out):
    B, _, H, d = q.shape
    init = (jnp.zeros((B, H, d, d), F32), jnp.zeros((B, H, d), F32), jnp.zeros((B, H), F32))
    y, y_c = 0.0, 0.0
    for dn in range(2):
        f = _identity if dn == 0 else _flip
        oc, st = mlstm_scan(f(q_c), f(k_c), f(v_c), f(g_c[:, :, dn]),
                            f(jax.nn.log_sigmoid(g_c[:, :, 2 + dn])), init, with_ctx_out)
        ox, _ = mlstm_scan(f(q), f(k), f(v), f(g[:, :, dn]),
                           f(jax.nn.log_sigmoid(g[:, :, 2 + dn])), st, True)
        y = y + f(ox)
        if with_ctx_out:
            y_c = y_c + f(oc)
    return y, (y_c if with_ctx_out else None)


def neighbourhood_attention(q, k, v, k_ctx, v_ctx, rpb):
    B, T, H, d = q.shape
    rows = T // GRID_W
    kr = min(NA_WIN_R, rows)
    r = jnp.arange(rows)
    key_rows = jnp.clip(r - kr // 2, 0, rows - kr)[:, None] + jnp.arange(kr)[None, :]
    cq = jnp.arange(GRID_W)
    start_c = jnp.clip(cq - NA_WIN_C // 2, 0, GRID_W - NA_WIN_C)
    col_ok = (cq[None, :] >= start_c[:, None]) & (cq[None, :] < start_c[:, None] + NA_WIN_C)
    dr = (key_rows - r[:, None] + NA_WIN_R - 1)[:, None, :, None]
    dc = jnp.clip(cq[None, :] - cq[:, None] + NA_WIN_C - 1, 0, 2 * NA_WIN_C - 2)[None, :, None, :]
    bias = rpb.astype(F32)[:, dr, dc]
    scale = HEAD_DIM ** -0.5
    qg = q.reshape(B, rows, GRID_W, H, d)
    kg = k.reshape(B, rows, GRID_W, H, d)[:, key_rows]
    vg = v.reshape(B, rows, GRID_W, H, d)[:, key_rows]
    s_loc = jnp.einsum('brqhd,brkwhd->bhrqkw', qg, kg).astype(F32) * scale + bias[None]
    s_loc = jnp.where(col_ok[None, None, None, :, None, :], s_loc, -jnp.inf)
    s_ctx = jnp.einsum('brqhd,bchd->bhrqc', qg, k_ctx).astype(F32) * scale
    n_loc = kr * GRID_W
    p = jax.nn.softmax(jnp.concatenate([s_loc.reshape(B, H, rows, GRID_W, n_loc), s_ctx], axis=-1), axis=-1)
    p_loc = p[..., :n_loc].reshape(B, H, rows, GRID_W, kr, GRID_W)
    o = jnp.einsum('bhrqkw,brkwhd->brqhd', p_loc, vg) + jnp.einsum('bhrqc,bchd->brqhd', p[..., n_loc:], v_ctx)
    return o.reshape(B, T, H, d).astype(q.dtype)


def context_attention(q, k, v):
    s = jnp.einsum('bqhd,bkhd->bhqk', q, k).astype(F32) * (HEAD_DIM ** -0.5)
    p = jax.nn.softmax(s, axis=-1)
    return jnp.einsum('bhqk,bkhd->bqhd', p, v).astype(q.dtype)


def _prep(h, w_in, conv_w, conv_b, gate_b, latent):
    B, T, _ = h.shape
    rq, rk, rv, rg, mq, mk, mv, mo, mg, nq, nk, nv = jnp.split(h @ w_in, IN_SPLITS, axis=-1)
    rq = rq.reshape(B, T, RET_HEADS, HEAD_DIM)
    rk = rk.reshape(B, T, RET_HEADS, HEAD_DIM)
    if latent:
        rq, rk = rope_2d(rq), rope_2d(rk)
    rv = rv.reshape(B, T, RET_HEADS, HEAD_DIM)
    mqk = jax.nn.silu(dwconv_centered(jnp.concatenate([mq, mk], axis=-1), conv_w, conv_b))
    mq, mk = jnp.split(mqk, 2, axis=-1)
    mq = mq.reshape(B, T, MLSTM_HEADS, HEAD_DIM)
    mk = mk.reshape(B, T, MLSTM_HEADS, HEAD_DIM)
    mv = mv.reshape(B, T, MLSTM_HEADS, HEAD_DIM)
    mg = mg.reshape(B, T, 4, MLSTM_HEADS).astype(F32) + gate_b.astype(F32)
    nq = nq.reshape(B, T, NA_HEADS, HEAD_DIM)
    nk = nk.reshape(B, T, NA_HEADS, HEAD_DIM)
    nv = nv.reshape(B, T, NA_HEADS, HEAD_DIM)
    return rq, rk, rv, rg, mq, mk, mv, mo, mg, nq, nk, nv


def _merge(rg, ret, mo, ml, na, w_out):
    B, T = rg.shape[:2]
    ret = (jax.nn.silu(rg.astype(F32)) * head_rms(ret).reshape(B, T, RET_W)).astype(rg.dtype)
    ml = (jax.nn.sigmoid(mo.astype(F32)) * head_rms(ml).reshape(B, T, MLSTM_W)).astype(rg.dtype)
    return jnp.concatenate([ret, ml, na.reshape(B, T, NA_W)], axis=-1) @ w_out


def mixer(hx, hc, w_in, ret_decay, conv_w, conv_b, gate_b, q_gain, k_gain, rpb, w_out, with_ctx_out):
    X = _prep(hx, w_in, conv_w, conv_b, gate_b, True)
    C = _prep(hc, w_in, conv_w, conv_b, gate_b, False)
    log_gamma = jax.nn.log_sigmoid(ret_decay.astype(F32))
    ret_x, ret_c = bidir_retention(X[0], X[1], X[2], C[0], C[1], C[2], log_gamma, with_ctx_out)
    ml_x, ml_c = bidir_mlstm(X[4], X[5], X[6], X[8], C[4], C[5], C[6], C[8], with_ctx_out)
    nk_c = qk_norm(C[10], k_gain)
    na_x = neighbourhood_attention(qk_norm(X[9], q_gain), qk_norm(X[10], k_gain), X[11], nk_c, C[11], rpb)
    out_x = _merge(X[3], ret_x, X[7], ml_x, na_x, w_out)
    if not with_ctx_out:
        return out_x, None
    na_c = context_attention(qk_norm(C[9], q_gain), nk_c, C[11])
    return out_x, _merge(C[3], ret_c, C[7], ml_c, na_c, w_out)


def swiglu(h, wg, wu, wd):
    return (jax.nn.silu(h @ wg) * (h @ wu)) @ wd


def moe_swiglu(h, w_router, w_gate, w_up, w_down):
    B, T, D = h.shape
    t = h.reshape(B * T, D)
    logits = (t @ w_router).astype(F32)
    top_v, top_i = lax.top_k(logits, TOP_K)
    wts = jax.nn.softmax(top_v, axis=-1)
    combine = jnp.sum(jax.nn.one_hot(top_i, N_EXPERTS, dtype=F32) * wts[..., None], axis=1)
    out = jnp.zeros_like(t)
    for e in range(N_EXPERTS):
        out = out + combine[:, e:e + 1].astype(t.dtype) * swiglu(t, w_gate[e], w_up[e], w_down[e])
    return out.reshape(B, T, D)


def setup_inputs(seed: int = 0) -> dict:
    key = jax.random.key(seed)
    ks = jax.random.split(key, 26)
    D = D_MODEL

    def nrm(k, shape, s):
        return jax.random.normal(k, shape, F32) * s

    gam = 1.0 - 2.0 ** (-5.0 - jnp.arange(RET_HEADS, dtype=F32))
    i_b = nrm(ks[12], (DEPTH, 2, MLSTM_HEADS), 0.1)
    f_b = jnp.linspace(3.0, 6.0, MLSTM_HEADS, dtype=F32) + nrm(ks[13], (DEPTH, 2, MLSTM_HEADS), 0.1)
    return {
        "x": nrm(ks[0], (BATCH, SEQ, D), 1.0),
        "c": nrm(ks[1], (BATCH, D), 1.0),
        "ctx": nrm(ks[2], (BATCH, CTX_LEN, D), 1.0),
        "c_ctx": nrm(ks[3], (D,), 1.0),
        "norm_mix": 1.0 + nrm(ks[4], (DEPTH, D), 0.02),
        "norm_ffn": 1.0 + nrm(ks[5], (DEPTH, D), 0.02),
        "w_mod": nrm(ks[6], (DEPTH, D, 6 * D), 0.5 * D ** -0.5),
        "b_mod": nrm(ks[7], (DEPTH, 6 * D), 0.02),
        "w_in": nrm(ks[8], (DEPTH, D, D_IN), D ** -0.5),
        "ret_decay": (jnp.log(gam) - jnp.log1p(-gam)) + nrm(ks[9], (DEPTH, 2, RET_HEADS), 0.05),
        "mlstm_conv_w": nrm(ks[10], (DEPTH, MLSTM_CONV, 2 * MLSTM_W), MLSTM_CONV ** -0.5),
        "mlstm_conv_b": nrm(ks[11], (DEPTH, 2 * MLSTM_W), 0.02),
        "mlstm_gate_b": jnp.concatenate([i_b, f_b], axis=1),
        "na_q_gain": 1.0 + nrm(ks[14], (DEPTH, HEAD_DIM), 0.02),
        "na_k_gain": 1.0 + nrm(ks[15], (DEPTH, HEAD_DIM), 0.02),
        "na_rpb": nrm(ks[16], (DEPTH, NA_HEADS, 2 * NA_WIN_R - 1, 2 * NA_WIN_C - 1), 0.1),
        "w_out": nrm(ks[17], (DEPTH, D_MIX, D), D_MIX ** -0.5),
        "ffn_w_gate": nrm(ks[18], (N_DENSE, D, D_FF), D ** -0.5),
        "ffn_w_up": nrm(ks[19], (N_DENSE, D, D_FF), D ** -0.5),
        "ffn_w_down": nrm(ks[20], (N_DENSE, D_FF, D), D_FF ** -0.5),
        "moe_router": nrm(ks[21], (N_MOE, D, N_EXPERTS), D ** -0.5),
        "moe_w_gate": nrm(ks[22], (N_MOE, N_EXPERTS, D, D_FF), D ** -0.5),
        "moe_w_up": nrm(ks[23], (N_MOE, N_EXPERTS, D, D_FF), D ** -0.5),
        "moe_w_down": nrm(ks[24], (N_MOE, N_EXPERTS, D_FF, D), D_FF ** -0.5),
    }


def reference(x, c, ctx, c_ctx, norm_mix, norm_ffn, w_mod, b_mod, w_in, ret_decay, mlstm_conv_w,
              mlstm_conv_b, mlstm_gate_b, na_q_gain, na_k_gain, na_rpb, w_out, ffn_w_gate, ffn_w_up,
              ffn_w_down, moe_router, moe_w_gate, moe_w_up, moe_w_down):
    sc_x = jax.nn.silu(c)[:, None, :]
    sc_c = jax.nn.silu(c_ctx)[None, None, :]
    for l in range(DEPTH):
        need_ctx = l < DEPTH - 1
        sh1, s1, g1, sh2, s2, g2 = jnp.split(sc_x @ w_mod[l] + b_mod[l], 6, axis=-1)
        csh1, cs1, cg1, csh2, cs2, cg2 = jnp.split(sc_c @ w_mod[l] + b_mod[l], 6, axis=-1)
        hx = rmsnorm(x, norm_mix[l]) * (1.0 + s1) + sh1
        hc = rmsnorm(ctx, norm_mix[l]) * (1.0 + cs1) + csh1
        mx, mc = mixer(hx, hc, w_in[l], ret_decay[l], mlstm_conv_w[l], mlstm_conv_b[l], mlstm_gate_b[l],
                       na_q_gain[l], na_k_gain[l], na_rpb[l], w_out[l], need_ctx)
        x = x + g1 * mx
        if need_ctx:
            ctx = ctx + cg1 * mc
        h2 = rmsnorm(x, norm_ffn[l]) * (1.0 + s2) + sh2
        if l % 2 == 0:
            j = l // 2
            x = x + g2 * swiglu(h2, ffn_w_gate[j], ffn_w_up[j], ffn_w_down[j])
            if need_ctx:
                h2c = rmsnorm(ctx, norm_ffn[l]) * (1.0 + cs2) + csh2
                ctx = ctx + cg2 * swiglu(h2c, ffn_w_gate[j], ffn_w_up[j], ffn_w_down[j])
        else:
            j = l // 2
            x = x + g2 * moe_swiglu(h2, moe_router[j], moe_w_gate[j], moe_w_up[j], moe_w_down[j])
            if need_ctx:
                h2c = rmsnorm(ctx, norm_ffn[l]) * (1.0 + cs2) + csh2
                ctx = ctx + cg2 * moe_swiglu(h2c, moe_router[j], moe_w_gate[j], moe_w_up[j], moe_w_down[j])
    return x
```

```python
import contextlib
import math
import numpy as np
import concourse.bass as bass
import concourse.mybir as mybir
from concourse.bass_utils import run_bass_kernel_spmd

F32 = mybir.dt.float32
BF16 = mybir.dt.bfloat16
ALU = mybir.AluOpType
AF = mybir.ActivationFunctionType
AX = mybir.AxisListType

D = 1024
T = 2048
LC = 256
TT = T + LC
NCH = 8
DFF = 2816
NFC = DFF // 128
NE = 8
EPS = 1e-6
LN8 = math.log(0.125)
QT = [(0, 256), (256, 512), (768, 512), (1280, 512), (1792, 512)]
NKT = TT // 128


class Sched:
    ENGS = ("pe", "act", "dve", "pool", "sp")

    def __init__(self, nc, stack, n_dma_sems=24):
        self.nc = nc
        self.streams = {e: [] for e in self.ENGS}
        self.esem = {e: stack.enter_context(nc.semaphore("s_" + e)) for e in self.ENGS}
        self.ecnt = {e: 0 for e in self.ENGS}
        self.dsem = {}
        self.dcnt = {}
        self.dnext = {}
        for q in ("sp", "pool", "act"):
            self.dsem[q] = [stack.enter_context(nc.semaphore("d_%s%d" % (q, i))) for i in range(n_dma_sems)]
            self.dcnt[q] = [0] * n_dma_sems
            self.dnext[q] = 0
        self.waited = {e: {} for e in self.ENGS}
        self.st = {}
        self.ninst = 0
        self.marks = []

    def _deps(self, reads, writes):
        deps = {}

        def add(t):
            if t is None:
                return
            s, v = t
            if deps.get(id(s), (None, -1))[1] < v:
                deps[id(s)] = (s, v)

        for k in reads:
            b = self.st.get(k)
            if b:
                add(b["w"])
        for k in writes:
            b = self.st.get(k)
            if b:
                add(b["w"])
                for t in b["r"].values():
                    add(t)
        return deps

    def _emit_waits(self, eng, deps):
        for sid, (s, v) in deps.items():
            if eng == "pe" and s is self.esem["pe"]:
                continue
            if self.waited[eng].get(sid, -1) >= v:
                continue
            self.waited[eng][sid] = v
            self.streams[eng].append(lambda e, s=s, v=v: e.wait_ge(s, v))

    def _update(self, reads, writes, ticket):
        s, v = ticket
        for k in reads:
            b = self.st.setdefault(k, {"w": None, "r": {}})
            b["r"][id(s)] = ticket
        for k in writes:
            self.st[k] = {"w": ticket, "r": {}}

    def ops(self, eng, insts, reads=(), writes=()):
        deps = self._deps(reads, writes)
        self._emit_waits(eng, deps)
        self.ecnt[eng] += 1
        sem = self.esem[eng]
        for name, kw in insts[:-1]:
            self.streams[eng].append(lambda e, name=name, kw=kw: getattr(e, name)(**kw))
        name, kw = insts[-1]
        self.streams[eng].append(lambda e, name=name, kw=kw, sem=sem: getattr(e, name)(**kw).then_inc(sem, 1))
        t = (sem, self.ecnt[eng])
        self._update(reads, writes, t)
        self.ninst += len(insts)
        return t

    def op(self, eng, name, reads=(), writes=(), **kw):
        return self.ops(eng, [(name, kw)], reads, writes)

    def dma(self, q, out, in_, reads=(), writes=(), **kw):
        deps = self._deps(reads, writes)
        i = self.dnext[q]
        self.dnext[q] = (i + 1) % len(self.dsem[q])
        sem = self.dsem[q][i]
        if self.dcnt[q][i] > 0:
            deps[id(sem)] = (sem, 16 * self.dcnt[q][i])
        self._emit_waits(q, deps)
        self.dcnt[q][i] += 1
        self.streams[q].append(
            lambda e, out=out, in_=in_, sem=sem, kw=kw: e.dma_start(out=out, in_=in_, **kw).then_inc(sem, 16))
        t = (sem, 16 * self.dcnt[q][i])
        self._update(reads, writes, t)
        self.ninst += 1
        return t

    def mark(self, label):
        self.marks.append((label, dict(self.ecnt)))

    def barrier(self):
        deps = {}
        for e in self.ENGS:
            if self.ecnt[e] > 0:
                deps[id(self.esem[e])] = (self.esem[e], self.ecnt[e])
        for q in self.dsem:
            for i, s in enumerate(self.dsem[q]):
                if self.dcnt[q][i] > 0:
                    deps[id(s)] = (s, 16 * self.dcnt[q][i])
        for e in self.ENGS:
            self._emit_waits(e, deps)

    @contextlib.contextmanager
    def phase(self):
        with contextlib.ExitStack() as ph:
            yield ph
        self.barrier()
        self.mark('phase_end')

    def wait_all(self, eng, keys):
        self._emit_waits(eng, self._deps(keys, ()))

    def finish(self):
        nc = self.nc
        with nc.Block() as block:
            @block.tensor
            def _(e):
                for f in self.streams["pe"]:
                    f(e)

            @block.scalar
            def _(e):
                for f in self.streams["act"]:
                    f(e)

            @block.vector
            def _(e):
                for f in self.streams["dve"]:
                    f(e)

            @block.gpsimd
            def _(e):
                for f in self.streams["pool"]:
                    f(e)

            @block.sync
            def _(e):
                for f in self.streams["sp"]:
                    f(e)


class Rot:
    def __init__(self, items):
        self.items = list(items)
        self.i = 0

    def next(self):
        v = self.items[self.i]
        self.i = (self.i + 1) % len(self.items)
        return v


def _fm(v):
    v = np.asarray(v)
    n = v.shape[-1] // 128
    r = v.reshape(v.shape[:-1] + (n, 128))
    return np.ascontiguousarray(np.moveaxis(r, -1, 0))


def _rope_perm():
    f = np.arange(64)
    w = f % 32
    return np.where(w < 16, f + 16, f - 16)


def _fm_cols():
    perm64 = _rope_perm()
    perm256 = np.concatenate([h * 64 + perm64 for h in range(4)])
    a = np.arange
    cols = np.concatenate([
        a(0, 256), perm256, 256 + a(0, 256), 256 + perm256, 768 + a(0, 256), 1792 + a(0, 256),
        1024 + a(0, 256), 1280 + a(0, 256), 2064 + a(0, 512), 2576 + a(0, 512)])
    return cols


def _v_cols():
    a = np.arange
    return np.concatenate([512 + a(0, 256), 1536 + a(0, 256), 3088 + a(0, 512)])


def _na_tables():
    dr = np.zeros((128, 3, 6, 4, 64), dtype=np.int64)
    dc = np.zeros((128, 3, 6, 4, 64), dtype=np.int64)
    ok = np.zeros((128, 3, 6, 4, 64), dtype=bool)
    p = np.arange(128)
    par = (p // 64)[:, None, None]
    w = (p % 64)[:, None, None]
    i = np.arange(4)[None, :, None]
    cq = np.arange(64)[None, None, :]
    start_c = np.clip(cq - 8, 0, 48)
    col_ok = (w >= start_c) & (w < start_c + 16)
    dcv = np.clip(w - cq + 15, 0, 30)
    for gt in range(3):
        for a in range(6):
            m = 2 * a + par
            if gt == 1:
                row_ok = (m >= i) & (m <= i + 7)
                drv = m - i + 3
            else:
                row_ok = (m <= 7) & (i >= 0)
                drv = (m - i + 7) if gt == 0 else (m - i + 3)
            okk = row_ok & col_ok
            ok[:, gt, a] = okk
            dr[:, gt, a] = np.clip(np.broadcast_to(drv, okk.shape), 0, 14)
            dc[:, gt, a] = np.broadcast_to(dcv, okk.shape)
    return dr.reshape(128, 3, 6, 256), dc.reshape(128, 3, 6, 256), ok.reshape(128, 3, 6, 256)


def host_constants():
    c = {}
    c["ident"] = np.eye(128, dtype=np.float32)
    c["ones"] = np.ones((128, 128), dtype=np.float32)
    bd = np.zeros((128, 128), dtype=np.float32)
    bd[:64, :64] = 1.0
    bd[64:, 64:] = 1.0
    c["onesbd"] = bd
    sel = np.zeros((8, 8, 128), dtype=np.float32)
    for r in range(8):
        sel[r, r, :] = 1.0
    c["sel"] = sel
    t = np.arange(T)
    row = (t // 64).astype(np.float32)
    col = (t % 64).astype(np.float32)
    inv = (np.float32(10000.0) ** (-np.arange(0, 32, 2, dtype=np.float32) / np.float32(32))).astype(np.float32)
    f = np.arange(64)
    w = f % 32
    ii = w % 16
    pos = np.where((f < 32)[:, None], row[None, :], col[None, :]).astype(np.float32)
    ang = (pos * inv[ii][:, None]).astype(np.float32)
    cosv = np.cos(ang).astype(np.float32)
    sinv = np.sin(ang).astype(np.float32)
    sinv = np.where((w < 16)[:, None], -sinv, sinv).astype(np.float32)
    c["rope"] = np.ascontiguousarray(np.stack([np.tile(cosv, (2, 1)), np.tile(sinv, (2, 1))], axis=1))
    pp = np.arange(128, dtype=np.float32)[:, None]
    cc_ = np.arange(512, dtype=np.float32)[None, :]
    c["rtab"] = np.ascontiguousarray(cc_ - pp)
    c["dtab"] = np.ascontiguousarray(np.tile((128.0 * (np.arange(32) - 17)).astype(np.float32)[None, :], (128, 1)))
    c["ctab"] = np.ascontiguousarray(np.tile(np.arange(512, dtype=np.float32)[None, :], (128, 1)))
    c["dmp"] = np.ascontiguousarray((128.0 * (np.arange(32) - 17)).astype(np.float32)[None, :] - pp)
    sel2 = np.zeros((8, 2, 2, 128), dtype=np.float32)
    for dn in range(2):
        for u in range(2):
            sel2[dn * 4 + 2 * u, dn, u, 0:64] = 1.0
            sel2[dn * 4 + 2 * u + 1, dn, u, 64:128] = 1.0
    c["sel2"] = sel2
    _, _, ok = _na_tables()
    c["namask"] = np.ascontiguousarray(ok.astype(np.float32))
    return c


def host_prep(inp):
    shared = dict(host_constants())
    f32 = np.float32
    shared["w_mod"] = np.ascontiguousarray(inp["w_mod"])
    shared["bmod"] = _fm(inp["b_mod"])
    shared["nrm"] = np.ascontiguousarray(
        np.stack([_fm(inp["norm_mix"]), _fm(inp["norm_ffn"])], axis=2))
    w_in = np.asarray(inp["w_in"])
    shared["w_in_fm"] = np.ascontiguousarray(w_in[:, :, _fm_cols()])
    shared["w_in_v"] = np.ascontiguousarray(w_in[:, :, _v_cols()])
    shared["w_in_g"] = np.ascontiguousarray(w_in[:, :, 2048:2064])
    gb = np.asarray(inp["mlstm_gate_b"]).reshape(2, 16)
    shared["gateb"] = np.ascontiguousarray(np.stack([gb[:, 0:8], gb[:, 8:16]], axis=0).transpose(2, 1, 0))
    shared["retd"] = np.ascontiguousarray(np.tile(np.asarray(inp["ret_decay"]).reshape(1, 2, 8), (128, 1, 1)))
    rd = np.asarray(inp["ret_decay"])
    hidx = (np.arange(128) // 64)[:, None] + 2 * np.arange(2)[None, :]
    shared["retdpp"] = np.ascontiguousarray(rd[:, :, hidx].transpose(2, 0, 1, 3))
    cw = np.asarray(inp["mlstm_conv_w"])
    shared["convw"] = np.ascontiguousarray(_fm(cw).astype(f32))
    shared["convb"] = np.ascontiguousarray(_fm(np.asarray(inp["mlstm_conv_b"])))
    gq = np.tile(np.asarray(inp["na_q_gain"]), (1, 2))
    gk = np.tile(np.asarray(inp["na_k_gain"]), (1, 2))
    shared["nagain"] = np.ascontiguousarray(np.stack([gq, gk], axis=2).transpose(1, 0, 2))
    dr, dc, ok = _na_tables()
    rpb = np.asarray(inp["na_rpb"])
    shared["nab"] = np.ascontiguousarray(rpb[:, :, dr, dc])
    shared["w_out"] = np.ascontiguousarray(inp["w_out"])
    shared["ffn_wg"] = np.ascontiguousarray(inp["ffn_w_gate"][0])
    shared["ffn_wu"] = np.ascontiguousarray(inp["ffn_w_up"][0])
    shared["ffn_wd"] = np.ascontiguousarray(inp["ffn_w_down"][0])
    shared["moe_wr"] = np.ascontiguousarray(inp["moe_router"][0])
    shared["moe_wg"] = np.ascontiguousarray(inp["moe_w_gate"][0])
    shared["moe_wu"] = np.ascontiguousarray(inp["moe_w_up"][0])
    shared["moe_wd"] = np.ascontiguousarray(inp["moe_w_down"][0])
    per_core = []
    for b in range(8):
        m = {}
        m["x"] = np.ascontiguousarray(inp["x"][b])
        m["ctx"] = np.ascontiguousarray(inp["ctx"][b])
        m["cc"] = np.ascontiguousarray(np.stack([_fm(inp["c"][b]), _fm(inp["c_ctx"])], axis=2))
        per_core.append(m)
    return shared, per_core


N_LAYERS = 2
LAST_SCHED = None


class StopBuild(Exception):
    pass


class JobPipe:
    def __init__(self, la):
        self.la = la
        self.q = []

    def run_stage(self, jobs, es, ep, ea, fin):
        nj = len(jobs)
        for idx, job in enumerate(jobs):
            es(job)
            ep(job)
            self.q.append((ea, job, idx == 0, idx == nj - 1, fin if idx == nj - 1 else None))
            while len(self.q) > self.la:
                self._pop()

    def _pop(self):
        ea, job, f, l, fin = self.q.pop(0)
        ea(job, f, l)
        if fin is not None:
            fin()

    def flush(self):
        while self.q:
            self._pop()


def build(stage="full", dbg=None, n_layers=N_LAYERS):
    nc = bass.Bass("TRN2", target_bir_lowering=False)

    def dram_in(name, shape, dt=F32):
        return nc.dram_tensor(name, list(shape), dt, kind="ExternalInput").ap()

    x_d = dram_in("x", [T, D])
    ctx_d = dram_in("ctx", [LC, D])
    cc_d = dram_in("cc", [128, 8, 2])
    ident_d = dram_in("ident", [128, 128])
    ones_d = dram_in("ones", [128, 128])
    onesbd_d = dram_in("onesbd", [128, 128])
    sel_d = dram_in("sel", [8, 8, 128])
    rope_d = dram_in("rope", [128, 2, T])
    rtab_d = dram_in("rtab", [128, 512])
    dtab_d = dram_in("dtab", [128, 32])
    namask_d = dram_in("namask", [128, 3, 6, 256])
    wmod_d = dram_in("w_mod", [2, D, 6 * D])
    bmod_d = dram_in("bmod", [128, 2, 48])
    nrm_d = dram_in("nrm", [128, 2, 2, 8])
    winfm_d = dram_in("w_in_fm", [2, D, 3072])
    winv_d = dram_in("w_in_v", [2, D, 1024])
    wing_d = dram_in("w_in_g", [2, D, 16])
    gateb_d = dram_in("gateb", [8, 2, 2])
    retd_d = dram_in("retd", [128, 2, 8])
    convw_d = dram_in("convw", [128, 2, 3, 4])
    convb_d = dram_in("convb", [128, 2, 4])
    nagain_d = dram_in("nagain", [128, 2, 2])
    nab_d = dram_in("nab", [2, 8, 128, 3, 6, 256])
    ctab_d = dram_in("ctab", [128, 512])
    dmp_d = dram_in("dmp", [128, 32])
    sel2_d = dram_in("sel2", [8, 2, 2, 128])
    retdpp_d = dram_in("retdpp", [128, 2, 2, 2])
    wout_d = dram_in("w_out", [2, D, D])
    ffn_wg_d = dram_in("ffn_wg", [D, DFF])
    ffn_wu_d = dram_in("ffn_wu", [D, DFF])
    ffn_wd_d = dram_in("ffn_wd", [DFF, D])
    moe_wr_d = dram_in("moe_wr", [D, NE])
    moe_wg_d = dram_in("moe_wg", [NE, D, DFF])
    moe_wu_d = dram_in("moe_wu", [NE, D, DFF])
    moe_wd_d = dram_in("moe_wd", [NE, DFF, D])

    out_d = nc.dram_tensor("out", [T, D], F32, kind="ExternalOutput").ap()
    scr_kind = "ExternalOutput" if (dbg and "scr" in dbg) else "Internal"
    qkg_d = nc.dram_tensor("qkg_scr", [20, 128, TT], BF16, kind=scr_kind).ap()
    v_d = nc.dram_tensor("v_scr", [NKT, 128, 1024], BF16, kind=scr_kind).ap()
    comb_d = nc.dram_tensor("comb_scr", [NE, T], F32, kind="Internal").ap()
    dbg_d = {}
    if dbg:
        for name, spec in dbg.items():
            if name == "scr":
                continue
            shape, dt = spec
            dbg_d[name] = nc.dram_tensor("dbg_" + name, list(shape), dt, kind="ExternalOutput").ap()

    with contextlib.ExitStack() as st:
        S = Sched(nc, st)

        sb_cnt = [0]

        def sb(name, shape, dt, stack=None):
            sb_cnt[0] += 1
            return (stack or st).enter_context(nc.sbuf_tensor("sb%d_%s" % (sb_cnt[0], name), list(shape), dt))

        ps = st.enter_context(nc.psum_tensor("ps", [128, 8, 512], F32))
        bankrot = Rot(range(6))

        def PS(b):
            return ("ps", b)

        def mmi(out, lhsT, rhs, start, stop):
            return ("matmul", dict(out=out, lhsT=lhsT, rhs=rhs, start=start, stop=stop))

        def tile_keys(name, s0, n):
            return [(name, tt) for tt in range(s0 // 128, (s0 + n + 127) // 128)]

        def dbg_dump(name, src_ap, reads):
            if dbg and name in dbg:
                S.dma("sp", dbg_d[name], src_ap, reads=reads, writes=[("dbgout", name)])

        xres = sb("xres", [128, NCH, TT], F32)
        hT = sb("hT", [128, NCH, TT], BF16)
        ident = sb("ident", [128, 128], F32)
        ones = sb("ones", [128, 128], F32)
        onesbd = sb("onesbd", [128, 128], F32)
        cc = sb("cc", [128, 8, 2], F32)
        sc2 = sb("sc2", [128, 8, 2], BF16)
        bmod = sb("bmod", [128, 2, 48], F32)
        nrm = sb("nrm", [128, 2, 2, 8], F32)
        modL = [sb("mod%d" % i, [128, 48, 2], F32) for i in range(2)]
        AvecL = [sb("Avec%d" % i, [128, 2, 8, 2], F32) for i in range(2)]
        mod, Avec = modL[0], AvecL[0]
        convw = sb("convw", [128, 2, 3, 4], F32)
        convb = sb("convb", [128, 2, 4], F32)
        nagain = sb("nagain", [128, 2, 2], F32)
        gateb = sb("gateb", [8, 2, 2], F32)
        retd = sb("retd", [128, 2, 8], F32)
        lg = sb("lg", [128, 2, 8], F32)
        nlg = sb("nlg", [128, 2, 8], F32)
        dtab = sb("dtab", [128, 32], F32)
        rtab = sb("rtab", [128, 512], F32)
        sel = sb("sel", [8, 8, 128], F32)
        onesb = sb("onesb", [128, 64], BF16)

        for (t_, d_, k_) in [(ident, ident_d, "ident"), (ones, ones_d, "ones"), (onesbd, onesbd_d, "onesbd"),
                             (cc, cc_d, "cc"), (bmod, bmod_d, "bmod"), (nrm, nrm_d, "nrm"), (convw, convw_d, "convw"),
                             (convb, convb_d, "convb"), (nagain, nagain_d, "nagain"), (gateb, gateb_d, "gateb"),
                             (retd, retd_d, "retd"), (dtab, dtab_d, "dtab"), (rtab, rtab_d, "rtab"), (sel, sel_d, "sel")]:
            S.dma("sp", t_[:], d_, writes=[k_])
        S.op("act", "activation", reads=["cc"], writes=["sc2"], out=sc2[:], in_=cc[:], func=AF.Silu)
        S.op("pool", "memset", writes=["onesb"], ap=onesb[:], constant=1.0)
        S.op("act", "activation", reads=["retd"], writes=["lg"], out=lg[:], in_=retd[:], func=AF.Exp, scale=-1.0)
        S.op("act", "activation", reads=["lg"], writes=["nlg"], out=nlg[:], in_=lg[:], func=AF.Ln, bias=1.0)
        S.op("dve", "tensor_scalar", reads=["nlg"], writes=["lg"], out=lg[:], in0=nlg[:], scalar1=-1.0, scalar2=None, op0=ALU.mult)

        with S.phase() as ph:
            xin = [sb("xin%d" % i, [128, D], F32, ph) for i in range(2)]
            for tt in range(NKT):
                buf = xin[tt % 2]
                key = "xin%d" % (tt % 2)
                src = ctx_d[tt * 128:(tt + 1) * 128, :] if tt < 2 else x_d[(tt - 2) * 128:(tt - 1) * 128, :]
                S.dma("sp", buf[:], src, writes=[key])
                for half in range(2):
                    b = bankrot.next()
                    S.ops("pe", [("transpose", dict(out=ps[:, b, j * 128:(j + 1) * 128],
                                                    in_=buf[:, (half * 4 + j) * 128:(half * 4 + j + 1) * 128],
                                                    identity=ident[:])) for j in range(4)],
                          reads=[key, "ident"], writes=[PS(b)])
                    dst = xres[:, half * 4:half * 4 + 4, tt * 128:(tt + 1) * 128]
                    srcp = ps[:, b, :].rearrange("p (j n) -> p j n", j=4)
                    if half == 0:
                        S.op("act", "activation", reads=[PS(b)], writes=[("xres", tt, c_) for c_ in range(0, 4)], out=dst, in_=srcp, func=AF.Identity)
                    else:
                        S.op("dve", "tensor_copy", reads=[PS(b)], writes=[("xres", tt, c_) for c_ in range(4, 8)], out=dst, in_=srcp)

        def xres_keys(s0, n, chunks=None):
            cs = range(NCH) if chunks is None else chunks
            return [("xres", tt, c_) for tt in range(s0 // 128, (s0 + n + 127) // 128) for c_ in cs]

        def rmsnorm_to_hT(kind, tiles, router=None):
            b_lo = 0 if kind == 0 else 24
            with S.phase() as ph:
                sq = [sb("sq%d" % i, [128, 512], F32, ph) for i in range(4)]
                rstd = [sb("rstd%d" % i, [128, 512], F32, ph) for i in range(2)]
                tmp = [sb("ntmp%d" % i, [128, 512], F32, ph) for i in range(6)]
                sqr, tmr = Rot(range(4)), Rot(range(6))
                sq_on_dve = set(range(NCH)) if router is not None else {1, 3, 5, 7}
                nbanks = {}

                def emit_sq(ti):
                    (s0, n) = tiles[ti]
                    b = bankrot.next()
                    nbanks[ti] = b
                    for c in range(NCH):
                        i = sqr.next()
                        if c in sq_on_dve:
                            S.op("dve", "tensor_tensor", reads=xres_keys(s0, n, [c]), writes=["sq%d" % i],
                                 out=sq[i][:, :n], in0=xres[:, c, s0:s0 + n], in1=xres[:, c, s0:s0 + n], op=ALU.mult)
                        else:
                            S.op("act", "activation", reads=xres_keys(s0, n, [c]), writes=["sq%d" % i],
                                 out=sq[i][:, :n], in_=xres[:, c, s0:s0 + n], func=AF.Square)
                        S.ops("pe", [mmi(ps[:, b, :n], ones[:], sq[i][:, :n], c == 0, c == NCH - 1)],
                              reads=["sq%d" % i, "ones"], writes=[PS(b)])

                emit_sq(0)
                for ti, (s0, n) in enumerate(tiles):
                    j = 1 if s0 < LC else 0
                    xk = xres_keys(s0, n)
                    if ti + 1 < len(tiles):
                        emit_sq(ti + 1)
                    b = nbanks[ti]
                    r = rstd[ti % 2]
                    rk = "rstd%d" % (ti % 2)
                    S.op("act", "activation", reads=[PS(b)], writes=[rk],
                         out=r[:, :n], in_=ps[:, b, :n], func=AF.Ln, scale=1.0 / D, bias=EPS)
                    S.op("act", "activation", reads=[rk], writes=[rk], out=r[:, :n], in_=r[:, :n], func=AF.Exp, scale=-0.5)
                    for c in range(NCH):
                        i = tmr.next()
                        S.op("dve", "scalar_tensor_tensor", reads=xres_keys(s0, n, [c]) + [rk, "Avec"], writes=["ntmp%d" % i],
                             out=tmp[i][:, :n], in0=xres[:, c, s0:s0 + n], scalar=Avec[:, kind, c, j:j + 1], in1=r[:, :n],
                             op0=ALU.mult, op1=ALU.mult)
                        S.op("act", "activation", reads=["ntmp%d" % i, "mod"], writes=tile_keys("hT", s0, n),
                             out=hT[:, c, s0:s0 + n], in_=tmp[i][:, :n], func=AF.Identity, bias=mod[:, b_lo + c, j:j + 1])
                        if router is not None:
                            wr_, lgT_ = router
                            S.op("act", "activation", reads=["ntmp%d" % i, "mod"], writes=["ntmp%d" % i], out=tmp[i][:, :n], in_=tmp[i][:, :n],
                                 func=AF.Identity, bias=mod[:, b_lo + c, j:j + 1])
                            S.ops("pe", [mmi(ps[0:8, 7, :n], wr_[:, c, :], tmp[i][:, :n], c == 0, c == NCH - 1)],
                                  reads=["ntmp%d" % i, "wr"], writes=[PS(7)])
                    if router is not None:
                        S.op("act", "activation", reads=[PS(7)], writes=["lgT"], out=lgT_[:, s0 - LC:s0 - LC + n], in_=ps[0:8, 7, :n], func=AF.Identity)

        bmd = 6

        def adaln_load(lx, nb, wm):
            w = wm[nb % 2]
            wk = "wm%d" % (nb % 2)
            for kc in range(8):
                S.dma("pool", w[:, kc, :], wmod_d[lx, kc * 128:(kc + 1) * 128, nb * 512:(nb + 1) * 512], writes=[(wk, kc)])

        def adaln_mm(nb, wm):
            w = wm[nb % 2]
            wk = "wm%d" % (nb % 2)
            insts = []
            for mm_ in range(4):
                m = nb * 4 + mm_
                for kc in range(8):
                    insts.append(mmi(ps[:, bmd, m * 2:m * 2 + 2], w[:, kc, mm_ * 128:(mm_ + 1) * 128], sc2[:, kc, :],
                                     kc == 0, kc == 7))
            S.ops("pe", insts, reads=[(wk, kc) for kc in range(8)] + ["sc2"], writes=[PS(bmd)])

        def adaln_fin(lx, mod_t, Avec_t):
            for j in range(2):
                S.op("dve", "tensor_tensor", reads=[PS(bmd), "bmod"], writes=["mod"],
                     out=mod_t[:, :, j], in0=ps[:, bmd, 0:96].rearrange("p (m j) -> p m j", j=2)[:, :, j],
                     in1=bmod[:, lx, :], op=ALU.add)
            for kind in range(2):
                s_lo = 8 if kind == 0 else 32
                for j in range(2):
                    S.op("dve", "scalar_tensor_tensor", reads=["mod", "nrm"], writes=["Avec"],
                         out=Avec_t[:, kind, :, j], in0=mod_t[:, s_lo:s_lo + 8, j], scalar=1.0, in1=nrm[:, lx, kind, :],
                         op0=ALU.add, op1=ALU.mult)

        Ls_holder = []
        adaln_done = set()
        for l in range(n_layers):
            need_ctx = l < N_LAYERS - 1
            mod, Avec = modL[l], AvecL[l]
            if l not in adaln_done:
                with S.phase() as ph:
                    wm = [sb("wm%d" % i, [128, 8, 512], BF16, ph) for i in range(2)]
                    for nb in range(12):
                        adaln_load(l, nb, wm)
                        adaln_mm(nb, wm)
                    adaln_fin(l, mod, Avec)

            blk_i = [0]
            Ls = contextlib.ExitStack()
            Ls_holder.append(Ls)
            Fq = sb("Fq", [8, TT], F32, Ls)
            aT = sb("aT", [128, NKT, 8], F32, Ls)
            Ws = contextlib.ExitStack()
            wblk = [sb("wblk%d" % i, [128, 8, 512], BF16, Ws) for i in range(2)]
            for i_ in range(2):
                for kc in range(8):
                    S.dma("pool", wblk[i_][:, kc, :], winfm_d[l, kc * 128:(kc + 1) * 128, i_ * 512:(i_ + 1) * 512],
                          writes=[("wblk", i_, kc)])

            wcv = sb("wcv", [128, 8, 512], BF16, Ws)
            wgt = sb("wgt", [128, 8, 16], BF16, Ws)

            rmsnorm_to_hT(0, QT)
            if stage == "norm":
                break

            for kc in range(8):
                S.dma("pool", wcv[:, kc, :], winfm_d[l, kc * 128:(kc + 1) * 128, 1536:2048], writes=[("wcv", kc)])
            for kc in range(8):
                S.dma("pool", wgt[:, kc, :], wing_d[l, kc * 128:(kc + 1) * 128, :], writes=[("wgt", kc)])
            with S.phase() as ph:
                stg = [sb("stg%d" % i, [128, TT], BF16, ph) for i in range(2)]
                ropeb = [sb("ropeb%d" % i, [128, 2, 512], F32, ph) for i in range(2)]
                t12 = [sb("t12_%d" % i, [128, 512], F32, ph) for i in range(9)]
                vst = [sb("vst%d" % i, [128, 512], BF16, ph) for i in range(2)]
                t12r = Rot(range(9))

                def load_block(src2d):
                    i = blk_i[0] % 2
                    blk_i[0] += 1
                    if blk_i[0] > 2:
                        for kc in range(8):
                            S.dma("pool", wblk[i][:, kc, :], src2d[kc * 128:(kc + 1) * 128, :], writes=[("wblk", i, kc)])
                    return wblk[i], [("wblk", i, kc) for kc in range(8)]

                def proj(w, wkeys, mcol, s0, n):
                    b = bankrot.next()
                    S.ops("pe", [mmi(ps[:, b, :n], w[:, kc, mcol * 128:(mcol + 1) * 128], hT[:, kc, s0:s0 + n], kc == 0, kc == 7)
                                 for kc in range(8)], reads=wkeys + tile_keys("hT", s0, n), writes=[PS(b)])
                    return b

                def store_chunk(si, chunk_id):
                    S.dma("sp", qkg_d[chunk_id, :, :], stg[si][:], reads=[("stg", si)], writes=[("qkg", chunk_id)])

                for blk, chunk0 in ((0, 0), (1, 2)):
                    w, wk = load_block(winfm_d[l, :, blk * 512:(blk + 1) * 512])
                    for (s0, n) in QT:
                        if s0 >= LC:
                            rb = ropeb[(s0 // 512) % 2]
                            rkey = ("ropeb", (s0 // 512) % 2)
                            S.dma("sp", rb[:], rope_d[:, :, s0 - LC:s0 - LC + n], writes=[rkey])
                        for hh in range(2):
                            bA = proj(w, wk, hh, s0, n)
                            if s0 < LC:
                                S.op("act", "activation", reads=[PS(bA)], writes=[("stg", hh)],
                                     out=stg[hh][:, s0:s0 + n], in_=ps[:, bA, :n], func=AF.Identity)
                                continue
                            bB = proj(w, wk, 2 + hh, s0, n)
                            i1, i2 = t12r.next(), t12r.next()
                            S.op("dve", "tensor_tensor", reads=[PS(bA), rkey], writes=[("t12", i1)],
                                 out=t12[i1][:, :n], in0=ps[:, bA, :n], in1=rb[:, 0, :n], op=ALU.mult)
                            S.op("dve", "tensor_tensor", reads=[PS(bB), rkey], writes=[("t12", i2)],
                                 out=t12[i2][:, :n], in0=ps[:, bB, :n], in1=rb[:, 1, :n], op=ALU.mult)
                            S.op("pool", "tensor_tensor", reads=[("t12", i1), ("t12", i2)], writes=[("stg", hh)],
                                 out=stg[hh][:, s0:s0 + n], in0=t12[i1][:, :n], in1=t12[i2][:, :n], op=ALU.add)
                    for hh in range(2):
                        store_chunk(hh, chunk0 + hh)

                def plain_chunk(w, wk, mcol, si):
                    for (s0, n) in QT:
                        b = proj(w, wk, mcol, s0, n)
                        S.op("act", "activation", reads=[PS(b)], writes=[("stg", si)],
                             out=stg[si][:, s0:s0 + n], in_=ps[:, b, :n], func=AF.Identity)

                w, wk = load_block(winfm_d[l, :, 1024:1536])
                for hh in range(2):
                    plain_chunk(w, wk, hh, hh)
                    store_chunk(hh, 4 + hh)
                for hh in range(2):
                    plain_chunk(w, wk, 2 + hh, hh)
                    store_chunk(hh, 10 + hh)
                for blk, chunk0, gi in ((4, 12, 0), (5, 16, 1)):
                    w, wk = load_block(winfm_d[l, :, blk * 512:(blk + 1) * 512])
                    items = [(cidx, s0, n) for cidx in range(4) for (s0, n) in QT]
                    pend = None

                    def qk_finish(pv):
                        cidx_, s0_, n_, i1_, i2_, i3_ = pv
                        si_ = cidx_ % 2
                        b2 = bankrot.next()
                        S.ops("pe", [mmi(ps[:, b2, :n_], onesbd[:], t12[i2_][:, :n_], True, True)],
                              reads=[("t12", i2_), "onesbd"], writes=[PS(b2)])
                        S.op("act", "activation", reads=[PS(b2)], writes=[("t12", i3_)],
                             out=t12[i3_][:, :n_], in_=ps[:, b2, :n_], func=AF.Ln, scale=1.0 / 64, bias=EPS)
                        S.op("act", "activation", reads=[("t12", i3_)], writes=[("t12", i3_)], out=t12[i3_][:, :n_], in_=t12[i3_][:, :n_],
                             func=AF.Exp, scale=-0.5)
                        S.op("dve", "scalar_tensor_tensor", reads=[("t12", i1_), ("t12", i3_), "nagain"], writes=[("stg", si_)],
                             out=stg[si_][:, s0_:s0_ + n_], in0=t12[i1_][:, :n_], scalar=nagain[:, l, gi:gi + 1], in1=t12[i3_][:, :n_],
                             op0=ALU.mult, op1=ALU.mult)
                        if s0_ == QT[-1][0]:
                            store_chunk(si_, chunk0 + cidx_)

                    for (cidx, s0, n) in items:
                        b = proj(w, wk, cidx, s0, n)
                        i1, i2, i3 = t12r.next(), t12r.next(), t12r.next()
                        S.op("act", "activation", reads=[PS(b)], writes=[("t12", i1)],
                             out=t12[i1][:, :n], in_=ps[:, b, :n], func=AF.Identity)
                        S.op("act", "activation", reads=[PS(b)], writes=[("t12", i2)],
                             out=t12[i2][:, :n], in_=ps[:, b, :n], func=AF.Square)
                        if pend is not None:
                            qk_finish(pend)
                        pend = (cidx, s0, n, i1, i2, i3)
                    qk_finish(pend)
                for half in range(2):
                    w, wk = load_block(winv_d[l, :, half * 512:(half + 1) * 512])
                    for tt in range(NKT):
                        b = bankrot.next()
                        S.ops("pe", [mmi(ps[:, b, :], hT[:, kc, tt * 128:(tt + 1) * 128], w[:, kc, :], kc == 0, kc == 7)
                                     for kc in range(8)], reads=wk + [("hT", tt)], writes=[PS(b)])
                        vi = tt % 2
                        if tt % 2 == 0:
                            S.op("act", "activation", reads=[PS(b)], writes=[("vst", vi)], out=vst[vi][:], in_=ps[:, b, :], func=AF.Identity)
                        else:
                            S.op("dve", "tensor_copy", reads=[PS(b)], writes=[("vst", vi)], out=vst[vi][:], in_=ps[:, b, :])
                        S.dma("sp", v_d[tt, :, half * 512:(half + 1) * 512], vst[vi][:], reads=[("vst", vi)], writes=[("v_d", tt, half)])


            with S.phase() as ph:
                stg = [sb("stg%d" % i, [128, TT], BF16, ph) for i in range(2)]
                cpad = sb("cpad", [128, TT + 4], F32, ph)
                ybuf = sb("ybuf", [128, TT], F32, ph)
                S.op("pool", "memset", writes=["cpad"], ap=cpad[:], constant=0.0)
                igt = sb("igt", [8, TT], F32, ph)
                fgt = sb("fgt", [8, TT], F32, ph)
                gall = sb("gall", [8, TT], F32, ph)
                wgk = [("wgt", kc) for kc in range(8)]
                for (s0, n) in QT:
                    for gi, dst in enumerate((igt, fgt)):
                        b = bankrot.next()
                        S.ops("pe", [mmi(ps[0:8, b, :n], wgt[:, kc, gi * 8:(gi + 1) * 8], hT[:, kc, s0:s0 + n], kc == 0, kc == 7)
                                     for kc in range(8)], reads=wgk + tile_keys("hT", s0, n), writes=[PS(b)])
                        S.op("act", "activation", reads=[PS(b), "gateb"], writes=["igt" if gi == 0 else "fgt"],
                             out=dst[:, s0:s0 + n], in_=ps[0:8, b, :n], func=AF.Identity, bias=gateb[:, l, gi:gi + 1])

                S.op("act", "activation", reads=["fgt"], writes=["fgt"], out=fgt[:], in_=fgt[:], func=AF.Exp, scale=-1.0)
                S.op("act", "activation", reads=["fgt"], writes=["fgt"], out=fgt[:], in_=fgt[:], func=AF.Ln, bias=1.0)

                def conv_chunk(w, wk, mcol, si, cj):
                    for (s0, n) in QT:
                        b = proj(w, wk, mcol, s0, n)
                        o = 1 + s0 if s0 < LC else 3 + s0
                        S.op("dve", "tensor_copy", reads=[PS(b)], writes=["cpad"], out=cpad[:, o:o + n], in_=ps[:, b, :n])
                    for (s0, n, o) in ((0, LC, 1), (LC, T, 259)):
                        S.op("act", "activation", reads=["cpad", "convw", "convb"], writes=["ybuf"],
                             out=ybuf[:, s0:s0 + n], in_=cpad[:, o:o + n], func=AF.Identity,
                             scale=convw[:, l, 1, cj:cj + 1], bias=convb[:, l, cj:cj + 1])
                        S.op("dve", "scalar_tensor_tensor", reads=["cpad", "ybuf"], writes=["ybuf"],
                             out=ybuf[:, s0:s0 + n], in0=cpad[:, o - 1:o - 1 + n], scalar=convw[:, l, 0, cj:cj + 1],
                             in1=ybuf[:, s0:s0 + n], op0=ALU.mult, op1=ALU.add)
                        S.op("dve", "scalar_tensor_tensor", reads=["cpad", "ybuf"], writes=["ybuf"],
                             out=ybuf[:, s0:s0 + n], in0=cpad[:, o + 1:o + 1 + n], scalar=convw[:, l, 2, cj:cj + 1],
                             in1=ybuf[:, s0:s0 + n], op0=ALU.mult, op1=ALU.add)
                        S.op("act", "activation", reads=["ybuf"], writes=[("stg", si)],
                             out=stg[si][:, s0:s0 + n], in_=ybuf[:, s0:s0 + n], func=AF.Silu)

                w, wk = wcv, [("wcv", kc) for kc in range(8)]
                for hh in range(2):
                    conv_chunk(w, wk, hh, hh, hh)
                    store_chunk(hh, 6 + hh)
                for hh in range(2):
                    conv_chunk(w, wk, 2 + hh, hh, 2 + hh)
                    store_chunk(hh, 8 + hh)

                S.op("dve", "tensor_scalar", reads=["fgt"], writes=["fgt"], out=fgt[:], in0=fgt[:], scalar1=-1.0, scalar2=None, op0=ALU.mult)
                S.op("dve", "tensor_tensor_scan", reads=["fgt", "ones"], writes=["gall"],
                     out=gall[:], data0=ones[0:8, 0:1].to_broadcast([8, TT]), data1=fgt[:], initial=0.0, op0=ALU.mult, op1=ALU.add)
                dbg_dump("logf", fgt[:], ["fgt"])
                dbg_dump("gall", gall[:], ["gall"])
                sgn = sb("sgn", [8, 4], F32, ph)
                S.op("dve", "tensor_tensor", reads=["sel"], writes=["sgn"], out=sgn[:, 1:2], in0=sel[:, 4, 0:1], in1=sel[:, 5, 0:1], op=ALU.add)
                S.op("dve", "tensor_tensor", reads=["sel", "sgn"], writes=["sgn"], out=sgn[:, 2:3], in0=sel[:, 6, 0:1], in1=sel[:, 7, 0:1], op=ALU.add)
                S.op("dve", "tensor_tensor", reads=["sgn"], writes=["sgn"], out=sgn[:, 1:2], in0=sgn[:, 1:2], in1=sgn[:, 2:3], op=ALU.add)
                S.op("dve", "tensor_scalar", reads=["sgn"], writes=["sgn"], out=sgn[:, 0:1], in0=sgn[:, 1:2], scalar1=-2.0, scalar2=1.0,
                     op0=ALU.mult, op1=ALU.add)
                S.op("dve", "tensor_tensor", reads=["sgn", "gall"], writes=["sgn"], out=sgn[:, 2:3], in0=sgn[:, 1:2], in1=gall[:, LC - 1:LC], op=ALU.mult)
                S.op("dve", "tensor_tensor", reads=["sgn", "gall"], writes=["sgn"], out=sgn[:, 3:4], in0=gall[:, LC - 1:LC], in1=gall[:, TT - 1:TT], op=ALU.add)
                S.op("dve", "tensor_tensor", reads=["sgn"], writes=["sgn"], out=sgn[:, 3:4], in0=sgn[:, 3:4], in1=sgn[:, 1:2], op=ALU.mult)
                S.op("dve", "tensor_scalar", reads=["gall", "sgn"], writes=["Fq"], out=Fq[:], in0=gall[:], scalar1=sgn[:, 0:1], scalar2=None, op0=ALU.mult)
                S.op("dve", "scalar_tensor_tensor", reads=["fgt", "sgn", "Fq"], writes=["Fq"], out=Fq[:], in0=fgt[:], scalar=sgn[:, 1:2], in1=Fq[:],
                     op0=ALU.mult, op1=ALU.add)
                S.op("dve", "tensor_scalar", reads=["Fq", "sgn"], writes=["Fq"], out=Fq[:, 0:LC], in0=Fq[:, 0:LC], scalar1=sgn[:, 2:3], scalar2=None, op0=ALU.add)
                S.op("dve", "tensor_scalar", reads=["Fq", "sgn"], writes=["Fq"], out=Fq[:, LC:TT], in0=Fq[:, LC:TT], scalar1=sgn[:, 3:4], scalar2=None, op0=ALU.add)
                S.op("dve", "tensor_tensor", reads=["igt", "Fq"], writes=["igt"], out=igt[:], in0=igt[:], in1=Fq[:], op=ALU.subtract)
                S.op("dve", "tensor_scalar", reads=["igt"], writes=["igt"], out=igt[:], in0=igt[:], scalar1=LN8, scalar2=None, op0=ALU.add)
                bt = 7
                S.ops("pe", [("transpose", dict(out=ps[:, bt, kt * 8:(kt + 1) * 8], in_=igt[:, kt * 128:(kt + 1) * 128],
                                                identity=ident[0:8, 0:8])) for kt in range(NKT)],
                      reads=["igt", "ident"], writes=[PS(bt)])
                S.op("dve", "tensor_copy", reads=[PS(bt)], writes=["aT"], out=aT[:].rearrange("p k r -> p (k r)"), in_=ps[:, bt, 0:NKT * 8])
                dbg_dump("Fq", Fq[:], ["Fq"])
                dbg_dump("aT", aT[:], ["aT"])

            Ws.close()
            if stage == "inproj":
                break

            q_tiles = QT if need_ctx else QT[1:]
            NPT = 8
            LA = 5
            with S.phase() as ph:
                qm = [sb("qm%d" % i, [128, TT], BF16, ph) for i in range(2)]
                kb = sb("kb", [128, TT], BF16, ph)
                gbuf = sb("gbuf", [128, TT], BF16, ph)
                vpad = sb("vpad", [128, NKT, 2, 128], BF16, ph)
                onespad = sb("onespad", [128, 2, 128], BF16, ph)
                pt = [sb("pt%d" % i, [128, 512], BF16, ph) for i in range(NPT)]
                dA = [sb("dA%d" % i, [128, 512], BF16, ph) for i in range(8)]
                dB = [sb("dB%d" % i, [128, 512], BF16, ph) for i in range(2)]
                dDg = [[sb("dDg%d_%d" % (hh_, j_), [128, 512], BF16, ph) for j_ in range(4)] for hh_ in range(2)]
                fw = [sb("fw%d" % i, [128, 512], F32, ph) for i in range(3)]
                hacc = [sb("hacc%d" % i, [128, 512], F32, ph) for i in range(2)]
                cfa = [sb("cfa%d" % i, [128, 512], BF16, ph) for i in range(2)]
                qs = [[sb("qs%d_%d" % (hh, i), [128, 512], BF16, ph) for i in range(4)] for hh in range(2)]
                btab = sb("btab", [128, 2, 3, 32], F32, ph)
                rft = sb("rft", [128, 2, 2, 32], F32, ph)
                c4 = sb("c4", [128, 4], F32, ph)
                lgpp = sb("lgpp", [128, 2, 2, 2], F32, ph)
                ctab = sb("ctab", [128, 512], F32, ph)
                dmp = sb("dmp", [128, 32], F32, ph)
                rfm = [sb("rfm%d" % i, [128, NKT], F32, ph) for i in range(4)]
                nfr = [sb("nfr%d" % i, [128, 1], F32, ph) for i in range(4)]
                ptr, dAr, dBr, fwr = Rot(range(NPT)), Rot(range(8)), Rot(range(2)), Rot(range(3))
                haccr, cfr, rfmr, nfrr = Rot(range(2)), Rot(range(2)), Rot(range(4)), Rot(range(4))
                qsr = [Rot(range(4)), Rot(range(4))]
                accrot = Rot([(6, None), (7, None)])
                evr = Rot(["act", "dve"])

                S.dma("sp", ctab[:], ctab_d, writes=["ctab"])
                S.dma("sp", dmp[:], dmp_d, writes=["dmp"])
                S.dma("sp", lgpp[:, 0, :, :], retdpp_d[:, l, :, :], writes=["lgpp"])
                S.op("act", "activation", reads=["lgpp"], writes=["lgpp"], out=lgpp[:, 1, :, :], in_=lgpp[:, 0, :, :], func=AF.Exp, scale=-1.0)
                S.op("act", "activation", reads=["lgpp"], writes=["lgpp"], out=lgpp[:, 1, :, :], in_=lgpp[:, 1, :, :], func=AF.Ln, bias=1.0)
                S.op("dve", "tensor_scalar", reads=["lgpp"], writes=["lgpp"], out=lgpp[:, 0, :, :], in0=lgpp[:, 1, :, :], scalar1=-1.0, scalar2=None,
                     op0=ALU.mult)
                S.op("pool", "memset", writes=["qm0z"], ap=qm[0][64:128, :], constant=0.0)
                S.op("pool", "memset", writes=["qm1z"], ap=qm[1][0:64, :], constant=0.0)
                S.op("pool", "memset", writes=["vpad"], ap=vpad[:], constant=0.0)
                S.op("pool", "memset", writes=["onespad"], ap=onespad[:], constant=0.0)
                S.op("pool", "memset", writes=["onespad"], ap=onespad[:, 0, 0:64], constant=1.0)
                S.op("pool", "memset", writes=["onespad"], ap=onespad[:, 1, 64:128], constant=1.0)
                for i in range(4):
                    S.op("pool", "memset", writes=[("qs", 0, i)], ap=qs[0][i][64:128, :], constant=0.0)
                    S.op("pool", "memset", writes=[("qs", 1, i)], ap=qs[1][i][0:64, :], constant=0.0)
                HS = [slice(0, 64), slice(64, 128)]

                def load_unit(qc, kc, gc, vcol, want_qf=True, want_qm=True):
                    if want_qf:
                        S.dma("sp", qf[:], qkg_d[qc, :, :], reads=[("qkg", qc)], writes=["qf"])
                    if want_qm:
                        S.dma("sp", qm[0][0:64, :], qkg_d[qc, 0:64, :], reads=[("qkg", qc)], writes=[("qm", 0)])
                        S.dma("sp", qm[1][64:128, :], qkg_d[qc, 64:128, :], reads=[("qkg", qc)], writes=[("qm", 1)])
                    S.dma("sp", kb[:], qkg_d[kc, :, :], reads=[("qkg", kc)], writes=["kb"])
                    if gc is not None:
                        S.dma("sp", gbuf[:], qkg_d[gc, :, :], reads=[("qkg", gc)], writes=["gbuf"])
                    vk = [("v_d", tt, vcol // 512) for tt in range(NKT)]
                    S.dma("sp", vpad[:, :, 0, 0:64], v_d[:, :, vcol:vcol + 64].rearrange("t p c -> p t c"), reads=vk + ["vpad"], writes=[("vpad", 0)])
                    S.dma("sp", vpad[:, :, 1, 64:128], v_d[:, :, vcol + 64:vcol + 128].rearrange("t p c -> p t c"), reads=vk + ["vpad"],
                          writes=[("vpad", 1)])

                def run_jobs(jobs, emit_score, emit_p, emit_av):
                    nj = len(jobs)
                    for idx in range(nj + LA):
                        if idx < nj:
                            emit_score(jobs[idx])
                            emit_p(jobs[idx])
                        if idx >= LA:
                            emit_av(jobs[idx - LA], idx - LA == 0, idx - LA == nj - 1)

                def evac_scaled(b, n, ip, scal):
                    eng = evr.next()
                    if eng == "act":
                        S.op("act", "activation", reads=[PS(b), "rf"], writes=[("pt", ip)], out=pt[ip][:, :n], in_=ps[:, b, :n], func=AF.Identity, scale=scal)
                    else:
                        S.op("dve", "tensor_scalar", reads=[PS(b), "rf"], writes=[("pt", ip)], out=pt[ip][:, :n], in0=ps[:, b, :n], scalar1=scal,
                             scalar2=None, op0=ALU.mult)

                def post_gate(chunk, func, hi, s0, n):
                    i1, i2, i3 = fwr.next(), fwr.next(), fwr.next()
                    hk = ("hacc", hi)
                    S.op("act", "activation", reads=[hk], writes=[("fw", i1)], out=fw[i1][:, :n], in_=hacc[hi][:, :n], func=AF.Square)
                    b = bankrot.next()
                    S.ops("pe", [mmi(ps[:, b, :n], onesbd[:], fw[i1][:, :n], True, True)], reads=[("fw", i1), "onesbd"], writes=[PS(b)])
                    S.op("act", "activation", reads=[PS(b)], writes=[("fw", i2)], out=fw[i2][:, :n], in_=ps[:, b, :n], func=AF.Ln,
                         scale=1.0 / 64, bias=EPS)
                    S.op("act", "activation", reads=[("fw", i2)], writes=[("fw", i2)], out=fw[i2][:, :n], in_=fw[i2][:, :n], func=AF.Exp, scale=-0.5)
                    S.op("act", "activation", reads=["gbuf"], writes=[("fw", i3)], out=fw[i3][:, :n], in_=gbuf[:, s0:s0 + n], func=func)
                    S.op("pool", "tensor_tensor", reads=[hk, ("fw", i2)], writes=[("fw", i2)], out=fw[i2][:, :n], in0=hacc[hi][:, :n],
                         in1=fw[i2][:, :n], op=ALU.mult)
                    S.op("pool", "tensor_tensor", reads=[("fw", i2), ("fw", i3)], writes=tile_keys("hT", s0, n),
                         out=hT[:, chunk, s0:s0 + n], in0=fw[i2][:, :n], in1=fw[i3][:, :n], op=ALU.mult)

                def gen_dec(a_, akey, hh, u, ty, dl, n):
                    h = 2 * u + hh
                    lgf, nlgb = lg[:, l, h:h + 1], nlg[:, l, 4 + h:5 + h]
                    di = dl // 128 + 17
                    S.op("act", "activation", reads=["rtab", "btab", "lg"], writes=[akey], out=a_[:, :n], in_=rtab[:, :n],
                         func=AF.Exp, scale=lgf, bias=btab[:, hh, 0, di:di + 1])
                    ib = dBr.next()
                    b_ = dB[ib]
                    S.op("act", "activation", reads=["rtab", "btab", "nlg"], writes=[("dB", ib)], out=b_[:, :n], in_=rtab[:, :n],
                         func=AF.Exp, scale=nlgb, bias=btab[:, hh, 1 if ty == 3 else 2, di:di + 1])
                    if ty == 3:
                        S.op("pool", "affine_select", reads=[akey], writes=[akey], out=a_[:, :n], in_=a_[:, :n],
                             pattern=[[1, n]], compare_op=ALU.is_ge, fill=0.0, base=dl, channel_multiplier=-1)
                        S.op("pool", "affine_select", reads=[("dB", ib)], writes=[("dB", ib)], out=b_[:, :n], in_=b_[:, :n],
                             pattern=[[-1, n]], compare_op=ALU.is_ge, fill=0.0, base=-dl, channel_multiplier=1)
                    S.op("pool", "tensor_tensor", reads=[akey, ("dB", ib)], writes=[akey], out=a_[:, :n], in0=a_[:, :n],
                         in1=b_[:, :n], op=ALU.add)

                for u in range(2):
                    load_unit(u, 2 + u, 4 + u, u * 128, want_qf=False)
                    S.op("dve", "tensor_scalar", reads=["lgpp"], writes=["c4"], out=c4[:, 2:3], in0=lgpp[:, 0, 1, u:u + 1], scalar1=511.0, scalar2=None,
                         op0=ALU.mult)
                    S.op("act", "activation", reads=["ctab", "lgpp"], writes=[("cfa", 0)], out=cfa[0][:], in_=ctab[:], func=AF.Exp,
                         scale=lgpp[:, 0, 0, u:u + 1])
                    S.op("act", "activation", reads=["ctab", "lgpp", "c4"], writes=[("cfa", 1)], out=cfa[1][:], in_=ctab[:], func=AF.Exp,
                         scale=lgpp[:, 1, 1, u:u + 1], bias=c4[:, 2:3])
                    for hh in range(2):
                        h = 2 * u + hh
                        lgf, nlgb = lg[:, l, h:h + 1], nlg[:, l, 4 + h:5 + h]
                        S.op("dve", "tensor_scalar", reads=["dtab", "lg"], writes=["btab"], out=btab[:, hh, 0, :], in0=dtab[:], scalar1=lgf,
                             scalar2=LN8, op0=ALU.mult, op1=ALU.add)
                        S.op("dve", "tensor_scalar", reads=["dtab", "nlg"], writes=["btab"], out=btab[:, hh, 1, :], in0=dtab[:], scalar1=nlgb,
                             scalar2=LN8, op0=ALU.mult, op1=ALU.add)
                        S.op("dve", "tensor_scalar", reads=["lg"], writes=["c4"], out=c4[:, hh:hh + 1], in0=lg[:, l, 4 + h:5 + h],
                             scalar1=float(TT), scalar2=LN8, op0=ALU.mult, op1=ALU.add)
                        S.op("dve", "tensor_scalar", reads=["dtab", "nlg", "c4"], writes=["btab"], out=btab[:, hh, 2, :], in0=dtab[:], scalar1=nlgb,
                             scalar2=c4[:, hh:hh + 1], op0=ALU.mult, op1=ALU.add)
                        S.op("act", "activation", reads=["dmp", "lg"], writes=["rf"], out=rft[:, hh, 0, 17:32], in_=dmp[:, 17:32], func=AF.Exp, scale=lgf, bias=LN8)
                        S.op("dve", "tensor_scalar", reads=["nlg"], writes=["c4"], out=c4[:, 3:4], in0=nlgb, scalar1=511.0, scalar2=LN8,
                             op0=ALU.mult, op1=ALU.add)
                        S.op("act", "activation", reads=["dmp", "nlg", "c4"], writes=["rf"], out=rft[:, hh, 1, 0:14], in_=dmp[:, 0:14], func=AF.Exp, scale=nlgb,
                             bias=c4[:, 3:4])
                    for hh in range(2):
                        for j_ in range(4):
                            gen_dec(dDg[hh][j_], ("dDg", hh, j_), hh, u, 3, -128 * j_, 512)
                    def ret_pre(s0, n, u=u):
                        lat = s0 >= LC
                        qsl = {}
                        if lat:
                            for hh in range(2):
                                for ty in (1, 2):
                                    i = qsr[hh].next()
                                    qsl[(hh, ty)] = i
                                    S.op("pool" if ty == 1 else "dve", "tensor_tensor", reads=[("qm", hh), ("cfa", ty - 1)], writes=[("qs", hh, i)],
                                         out=qs[hh][i][HS[hh], :n], in0=qm[hh][HS[hh], s0:s0 + n], in1=cfa[ty - 1][HS[hh], :n], op=ALU.mult)
                        alljobs = []
                        for hh in range(2):
                            if not lat:
                                alljobs += [(hh, 0, 3), (hh, 1, 3)]
                            else:
                                for kt in range(NKT):
                                    k0 = kt * 128
                                    if kt < 2:
                                        alljobs.append((hh, kt, 4))
                                    elif k0 + 127 < s0:
                                        alljobs.append((hh, kt, 1))
                                    elif k0 > s0 + n - 1:
                                        alljobs.append((hh, kt, 2))
                                    else:
                                        alljobs.append((hh, kt, 3))
                        dectile = {}
                        for job in alljobs:
                            hh_, kt_, ty_ = job
                            if ty_ == 3:
                                j_ = (kt_ * 128 - s0) // 128
                                dectile[job] = (dDg[hh_][j_], ("dDg", hh_, j_))
                            elif ty_ == 4:
                                ia = dAr.next()
                                gen_dec(dA[ia], ("dA", ia), hh_, u, 4, s0 - kt_ * 128, n)
                                dectile[job] = (dA[ia], ("dA", ia))
                        return qsl, alljobs, dectile

                    pipe = JobPipe(LA)
                    pres = {0: ret_pre(*q_tiles[0])}
                    for qi_, (s0, n) in enumerate(q_tiles):
                        if qi_ + 1 < len(q_tiles):
                            pres[qi_ + 1] = ret_pre(*q_tiles[qi_ + 1])
                        qsl, alljobs, dectile = pres.pop(qi_)
                        nbk, _ = accrot.next()
                        state = {}

                        def emit_score(job, s0=s0, n=n, state=state, qsl=qsl):
                            hh, kt, ty = job
                            b = bankrot.next()
                            state[job] = [b, None]
                            if ty in (1, 2):
                                i = qsl[(hh, ty)]
                                rhs, rk = qs[hh][i][:, :n], ("qs", hh, i)
                            else:
                                rhs, rk = qm[hh][:, s0:s0 + n], ("qm", hh)
                            S.ops("pe", [mmi(ps[:, b, :n], kb[:, kt * 128:(kt + 1) * 128], rhs, True, True)], reads=["kb", rk, "qm%dz" % hh],
                                  writes=[PS(b)])

                        def emit_p(job, s0=s0, n=n, state=state, u=u, dectile=dectile):
                            hh, kt, ty = job
                            b = state[job][0]
                            dl = s0 - kt * 128
                            di = dl // 128 + 17
                            ip = ptr.next()
                            state[job][1] = ip
                            if ty in (1, 2):
                                evac_scaled(b, n, ip, rft[:, hh, ty - 1, di:di + 1])
                                return
                            a_, akey = dectile[job]
                            S.op("dve", "tensor_tensor", reads=[PS(b), akey], writes=[("pt", ip)], out=pt[ip][:, :n], in0=ps[:, b, :n],
                                 in1=a_[:, :n], op=ALU.mult)

                        def emit_av(job, first, last, n=n, state=state, nbk=nbk):
                            hh, kt, ty = job
                            ip = state[job][1]
                            S.ops("pe", [mmi(ps[:, nbk, :n], vpad[:, kt, hh, :], pt[ip][:, :n], first, last)],
                                  reads=[("pt", ip), ("vpad", hh), "vpad"], writes=[("psacc", nbk)])

                        def fin(nbk=nbk, s0=s0, n=n, u=u):
                            hi = haccr.next()
                            S.op("act", "activation", reads=[("psacc", nbk)], writes=[("hacc", hi)], out=hacc[hi][:, :n], in_=ps[:, nbk, :n],
                                 func=AF.Identity)
                            if dbg and ("ret%d" % u) in dbg:
                                S.dma("sp", dbg_d["ret%d" % u][:, s0:s0 + n], hacc[hi][:, :n], reads=[("hacc", hi)], writes=[("dbgout", "ret", u, s0)])
                            post_gate(u, AF.Silu, hi, s0, n)

                        pipe.run_stage(alljobs, emit_score, emit_p, emit_av, fin)
                    pipe.flush()

                if stage == "ret":
                    break


            with S.phase() as ph:
                qf = sb("qf", [128, TT], BF16, ph)
                kb = sb("kb", [128, TT], BF16, ph)
                gbuf = sb("gbuf", [128, TT], BF16, ph)
                vpad = sb("vpad", [128, NKT, 2, 128], BF16, ph)
                onespad = sb("onespad", [128, 2, 128], BF16, ph)
                pt = [sb("pt%d" % i, [128, 512], BF16, ph) for i in range(NPT)]
                mtile = [[sb("mt%d_%d" % (dn_, j_), [128, 512], BF16, ph) for j_ in range(4)] for dn_ in range(2)]
                fw = [sb("fw%d" % i, [128, 512], F32, ph) for i in range(4)]
                hacc = [sb("hacc%d" % i, [128, 512], F32, ph) for i in range(2)]
                cfa = [sb("cfa%d" % i, [128, 512], F32, ph) for i in range(2)]
                qs = [[sb("qs%d_%d" % (hh, i), [128, 512], BF16, ph) for i in range(4)] for hh in range(2)]
                btab = sb("btab", [128, 2, 3, 32], F32, ph)
                rft = sb("rft", [128, 2, 2, 32], F32, ph)
                c4 = sb("c4", [128, 4], F32, ph)
                lgpp = sb("lgpp", [128, 2, 2, 2], F32, ph)
                ctab = sb("ctab", [128, 512], F32, ph)
                dmp = sb("dmp", [128, 32], F32, ph)
                sel2 = sb("sel2", [8, 2, 2, 128], F32, ph)
                fref = sb("fref", [128, 2, 2, 8], F32, ph)
                rfm = [sb("rfm%d" % i, [128, NKT], F32, ph) for i in range(4)]
                nfr = [sb("nfr%d" % i, [128, 1], F32, ph) for i in range(4)]
                ptr, dAr, dBr, fwr = Rot(range(NPT)), Rot(range(8)), Rot(range(2)), Rot(range(4))
                haccr, cfr, rfmr, nfrr = Rot(range(2)), Rot(range(2)), Rot(range(4)), Rot(range(4))
                qsr = [Rot(range(4)), Rot(range(4))]
                accrot = Rot([(6, 7)])
                evr = Rot(["act", "dve"])

                S.dma("sp", ctab[:], ctab_d, writes=["ctab"])
                S.dma("sp", dmp[:], dmp_d, writes=["dmp"])
                S.dma("sp", sel2[:], sel2_d, writes=["sel2"])
                S.dma("sp", lgpp[:, 0, :, :], retdpp_d[:, l, :, :], writes=["lgpp"])
                S.op("act", "activation", reads=["lgpp"], writes=["lgpp"], out=lgpp[:, 1, :, :], in_=lgpp[:, 0, :, :], func=AF.Exp, scale=-1.0)
                S.op("act", "activation", reads=["lgpp"], writes=["lgpp"], out=lgpp[:, 1, :, :], in_=lgpp[:, 1, :, :], func=AF.Ln, bias=1.0)
                S.op("dve", "tensor_scalar", reads=["lgpp"], writes=["lgpp"], out=lgpp[:, 0, :, :], in0=lgpp[:, 1, :, :], scalar1=-1.0, scalar2=None,
                     op0=ALU.mult)
                S.op("pool", "memset", writes=["vpad"], ap=vpad[:], constant=0.0)
                S.op("pool", "memset", writes=["onespad"], ap=onespad[:], constant=0.0)
                S.op("pool", "memset", writes=["onespad"], ap=onespad[:, 0, 0:64], constant=1.0)
                S.op("pool", "memset", writes=["onespad"], ap=onespad[:, 1, 64:128], constant=1.0)
                for i in range(4):
                    S.op("pool", "memset", writes=[("qs", 0, i)], ap=qs[0][i][64:128, :], constant=0.0)
                    S.op("pool", "memset", writes=[("qs", 1, i)], ap=qs[1][i][0:64, :], constant=0.0)
                for dn_ in range(2):
                    for j_ in range(4):
                        S.op("pool", "memset", writes=["mtile"], ap=mtile[dn_][j_][:], constant=1.0)
                        dl_ = -128 * j_
                        if dn_ == 0:
                            S.op("pool", "affine_select", reads=["mtile"], writes=["mtile"], out=mtile[dn_][j_][:], in_=mtile[dn_][j_][:],
                                 pattern=[[1, 512]], compare_op=ALU.is_ge, fill=0.0, base=dl_, channel_multiplier=-1)
                        else:
                            S.op("pool", "affine_select", reads=["mtile"], writes=["mtile"], out=mtile[dn_][j_][:], in_=mtile[dn_][j_][:],
                                 pattern=[[-1, 512]], compare_op=ALU.is_ge, fill=0.0, base=-dl_, channel_multiplier=1)
                HS = [slice(0, 64), slice(64, 128)]

                def load_unit(qc, kc, gc, vcol, want_qf=True, want_qm=True):
                    if want_qf:
                        S.dma("sp", qf[:], qkg_d[qc, :, :], reads=[("qkg", qc)], writes=["qf"])
                    if want_qm:
                        S.dma("sp", qm[0][0:64, :], qkg_d[qc, 0:64, :], reads=[("qkg", qc)], writes=[("qm", 0)])
                        S.dma("sp", qm[1][64:128, :], qkg_d[qc, 64:128, :], reads=[("qkg", qc)], writes=[("qm", 1)])
                    S.dma("sp", kb[:], qkg_d[kc, :, :], reads=[("qkg", kc)], writes=["kb"])
                    if gc is not None:
                        S.dma("sp", gbuf[:], qkg_d[gc, :, :], reads=[("qkg", gc)], writes=["gbuf"])
                    vk = [("v_d", tt, vcol // 512) for tt in range(NKT)]
                    S.dma("sp", vpad[:, :, 0, 0:64], v_d[:, :, vcol:vcol + 64].rearrange("t p c -> p t c"), reads=vk + ["vpad"], writes=[("vpad", 0)])
                    S.dma("sp", vpad[:, :, 1, 64:128], v_d[:, :, vcol + 64:vcol + 128].rearrange("t p c -> p t c"), reads=vk + ["vpad"],
                          writes=[("vpad", 1)])

                def run_jobs(jobs, emit_score, emit_p, emit_av):
                    nj = len(jobs)
                    for idx in range(nj + LA):
                        if idx < nj:
                            emit_score(jobs[idx])
                            emit_p(jobs[idx])
                        if idx >= LA:
                            emit_av(jobs[idx - LA], idx - LA == 0, idx - LA == nj - 1)

                def evac_scaled(b, n, ip, scal):
                    eng = evr.next()
                    if eng == "act":
                        S.op("act", "activation", reads=[PS(b), "rf"], writes=[("pt", ip)], out=pt[ip][:, :n], in_=ps[:, b, :n], func=AF.Identity, scale=scal)
                    else:
                        S.op("dve", "tensor_scalar", reads=[PS(b), "rf"], writes=[("pt", ip)], out=pt[ip][:, :n], in0=ps[:, b, :n], scalar1=scal,
                             scalar2=None, op0=ALU.mult)

                def post_gate(chunk, func, hi, s0, n):
                    i1, i2, i3 = fwr.next(), fwr.next(), fwr.next()
                    hk = ("hacc", hi)
                    S.op("act", "activation", reads=[hk], writes=[("fw", i1)], out=fw[i1][:, :n], in_=hacc[hi][:, :n], func=AF.Square)
                    b = bankrot.next()
                    S.ops("pe", [mmi(ps[:, b, :n], onesbd[:], fw[i1][:, :n], True, True)], reads=[("fw", i1), "onesbd"], writes=[PS(b)])
                    S.op("act", "activation", reads=[PS(b)], writes=[("fw", i2)], out=fw[i2][:, :n], in_=ps[:, b, :n], func=AF.Ln,
                         scale=1.0 / 64, bias=EPS)
                    S.op("act", "activation", reads=[("fw", i2)], writes=[("fw", i2)], out=fw[i2][:, :n], in_=fw[i2][:, :n], func=AF.Exp, scale=-0.5)
                    S.op("act", "activation", reads=["gbuf"], writes=[("fw", i3)], out=fw[i3][:, :n], in_=gbuf[:, s0:s0 + n], func=func)
                    S.op("pool", "tensor_tensor", reads=[hk, ("fw", i2)], writes=[("fw", i2)], out=fw[i2][:, :n], in0=hacc[hi][:, :n],
                         in1=fw[i2][:, :n], op=ALU.mult)
                    S.op("pool", "tensor_tensor", reads=[("fw", i2), ("fw", i3)], writes=tile_keys("hT", s0, n),
                         out=hT[:, chunk, s0:s0 + n], in0=fw[i2][:, :n], in1=fw[i3][:, :n], op=ALU.mult)

                for u in range(2):
                    load_unit(6 + u, 8 + u, 10 + u, 256 + u * 128, want_qm=False)
                    for dn in range(2):
                        for hh in range(2):
                            r = dn * 4 + 2 * u + hh
                            b = bankrot.next()
                            insts = []
                            for qi, (s0, n) in enumerate(QT):
                                c0 = s0 if dn == 0 else s0 + n - 2
                                insts.append(mmi(ps[:, b, 2 * qi:2 * qi + 2], sel[:, r, :], Fq[:, c0:c0 + 2], True, True))
                            S.ops("pe", insts, reads=["sel", "Fq"], writes=[PS(b)])
                            S.op("dve", "tensor_copy", reads=[PS(b)], writes=["fref"], out=fref[:, dn, hh, 0:len(QT)],
                                 in_=ps[:, b, 0:2 * len(QT)].rearrange("p (q two) -> p q two", two=2)[:, :, dn])
                    def ml_pre(s0, n, dn, u=u):
                        qi = QT.index((s0, n))
                        tref = s0 if dn == 0 else s0 + n - 1
                        bF = bankrot.next()
                        S.ops("pe", [mmi(ps[:, bF, :n], sel2[:, dn, u, :], Fq[:, s0:s0 + n], True, True)], reads=["sel2", "Fq"], writes=[PS(bF)])
                        ni = nfrr.next()
                        S.op("dve", "tensor_scalar", reads=[PS(bF)], writes=[("nfr", ni)], out=nfr[ni][:], in0=ps[:, bF, tref - s0:tref - s0 + 1],
                             scalar1=-1.0, scalar2=None, op0=ALU.mult)
                        ci = cfr.next()
                        S.op("act", "activation", reads=[PS(bF), ("nfr", ni)], writes=[("cfa", ci)], out=cfa[ci][:, :n], in_=ps[:, bF, :n],
                             func=AF.Exp, bias=nfr[ni][:])
                        qsl = {}
                        for hh in range(2):
                            i = qsr[hh].next()
                            qsl[hh] = i
                            S.op("pool" if hh == 0 else "dve", "tensor_tensor", reads=["qf", ("cfa", ci)], writes=[("qs", hh, i)],
                                 out=qs[hh][i][HS[hh], :n], in0=qf[HS[hh], s0:s0 + n], in1=cfa[ci][HS[hh], :n], op=ALU.mult)
                        alljobs = []
                        rfi = {}
                        for hh in range(2):
                            r = dn * 4 + 2 * u + hh
                            ri = rfmr.next()
                            rfi[hh] = ri
                            if s0 < LC:
                                rngs = [(0, 2)]
                            elif dn == 0:
                                rngs = [(0, (s0 + n) // 128)]
                            else:
                                rngs = [(0, 2), (s0 // 128, NKT)]
                            for (ka, kb_) in rngs:
                                S.op("act", "activation", reads=["aT", "fref"], writes=[("rfm", ri)], out=rfm[ri][:, ka:kb_], in_=aT[:, ka:kb_, r], func=AF.Exp,
                                     bias=fref[:, dn, hh, qi:qi + 1])
                            if s0 < LC:
                                alljobs += [(hh, 0, True), (hh, 1, True)]
                            else:
                                for kt in range(NKT):
                                    k0 = kt * 128
                                    if kt < 2:
                                        alljobs.append((hh, kt, False))
                                    elif k0 + 127 < s0:
                                        if dn == 0:
                                            alljobs.append((hh, kt, False))
                                    elif k0 > s0 + n - 1:
                                        if dn == 1:
                                            alljobs.append((hh, kt, False))
                                    else:
                                        alljobs.append((hh, kt, True))
                        return qsl, rfi, alljobs

                    pipe = JobPipe(LA)
                    mstages = [(s0, n, dn) for (s0, n) in q_tiles for dn in range(2)]
                    mpre = {0: ml_pre(*mstages[0])}
                    hi = None
                    for sk, (s0, n, dn) in enumerate(mstages):
                        if sk + 1 < len(mstages):
                            mpre[sk + 1] = ml_pre(*mstages[sk + 1])
                        qsl, rfi, alljobs = mpre.pop(sk)
                        if dn == 0:
                            hi = haccr.next()
                        if True:
                            if True:
                                nbk, dbk = accrot.next()
                            state = {}

                            def emit_score(job, n=n, state=state, qsl=qsl):
                                hh, kt, dg = job
                                b = bankrot.next()
                                state[job] = [b, None]
                                i = qsl[hh]
                                S.ops("pe", [mmi(ps[:, b, :n], kb[:, kt * 128:(kt + 1) * 128], qs[hh][i][:, :n], True, True)],
                                      reads=["kb", ("qs", hh, i)], writes=[PS(b)])

                            def emit_p(job, s0=s0, n=n, state=state, rfi=rfi, dn=dn):
                                hh, kt, dg = job
                                b = state[job][0]
                                dl = s0 - kt * 128
                                ip = ptr.next()
                                state[job][1] = ip
                                ri = rfi[hh]
                                if dg:
                                    j_ = (kt * 128 - s0) // 128
                                    S.op("dve", "scalar_tensor_tensor", reads=[PS(b), ("rfm", ri), "mtile"], writes=[("pt", ip)], out=pt[ip][:, :n],
                                         in0=ps[:, b, :n], scalar=rfm[ri][:, kt:kt + 1], in1=mtile[dn][j_][:, :n], op0=ALU.mult, op1=ALU.mult)
                                    return
                                eng = evr.next()
                                if eng == "act":
                                    S.op("act", "activation", reads=[PS(b), ("rfm", ri)], writes=[("pt", ip)], out=pt[ip][:, :n], in_=ps[:, b, :n],
                                         func=AF.Identity, scale=rfm[ri][:, kt:kt + 1])
                                else:
                                    S.op("dve", "tensor_scalar", reads=[PS(b), ("rfm", ri)], writes=[("pt", ip)], out=pt[ip][:, :n], in0=ps[:, b, :n],
                                         scalar1=rfm[ri][:, kt:kt + 1], scalar2=None, op0=ALU.mult)

                            def emit_av(job, first, last, n=n, state=state, nbk=nbk, dbk=dbk):
                                hh, kt, dg = job
                                ip = state[job][1]
                                S.ops("pe", [mmi(ps[:, nbk, :n], vpad[:, kt, hh, :], pt[ip][:, :n], first, last),
                                             mmi(ps[:, dbk, :n], onespad[:, hh, :], pt[ip][:, :n], first, last)],
                                      reads=[("pt", ip), ("vpad", hh), "vpad", "onespad"], writes=[("psacc", nbk), ("psacc", dbk)])

                            def fin(nbk=nbk, dbk=dbk, s0=s0, n=n, dn=dn, hi=hi, u=u):
                                i1, i2 = fwr.next(), fwr.next()
                                S.op("act", "activation", reads=[("psacc", dbk)], writes=[("fw", i1)], out=fw[i1][:, :n], in_=ps[:, dbk, :n], func=AF.Abs)
                                S.op("dve", "tensor_copy", reads=[("psacc", nbk)], writes=[("fw", i2)], out=fw[i2][:, :n], in_=ps[:, nbk, :n])
                                S.op("dve", "tensor_scalar", reads=[("fw", i1)], writes=[("fw", i1)], out=fw[i1][:, :n], in0=fw[i1][:, :n],
                                     scalar1=1.0, scalar2=None, op0=ALU.max)
                                S.op("act", "activation", reads=[("fw", i1)], writes=[("fw", i1)], out=fw[i1][:, :n], in_=fw[i1][:, :n], func=AF.Ln)
                                S.op("act", "activation", reads=[("fw", i1)], writes=[("fw", i1)], out=fw[i1][:, :n], in_=fw[i1][:, :n], func=AF.Exp, scale=-1.0)
                                if dn == 0:
                                    S.op("pool", "tensor_tensor", reads=[("fw", i2), ("fw", i1)], writes=[("hacc", hi)], out=hacc[hi][:, :n],
                                         in0=fw[i2][:, :n], in1=fw[i1][:, :n], op=ALU.mult)
                                else:
                                    S.op("pool", "tensor_tensor", reads=[("fw", i2), ("fw", i1)], writes=[("fw", i1)], out=fw[i1][:, :n],
                                         in0=fw[i2][:, :n], in1=fw[i1][:, :n], op=ALU.mult)
                                    S.op("pool", "tensor_tensor", reads=[("hacc", hi), ("fw", i1)], writes=[("hacc", hi)], out=hacc[hi][:, :n],
                                         in0=hacc[hi][:, :n], in1=fw[i1][:, :n], op=ALU.add)
                                if dn == 1:
                                    post_gate(2 + u, AF.Sigmoid, hi, s0, n)

                            pipe.run_stage(alljobs, emit_score, emit_p, emit_av, fin)
                    pipe.flush()

                if stage == "ml":
                    break

            Ls.close()
            Wo = contextlib.ExitStack()
            wo_a = sb("wo_a", [128, 8, 256], BF16, Wo)
            with S.phase() as ph:
                qm = [sb("qm%d" % i, [128, TT], BF16, ph) for i in range(2)]
                kb = sb("kb", [128, TT], BF16, ph)
                vpad = sb("vpad", [128, NKT, 2, 128], BF16, ph)
                onespad = sb("onespad", [128, 2, 128], BF16, ph)
                pt = [sb("pt%d" % i, [128, 512], BF16, ph) for i in range(NPT)]
                fw = [sb("fw%d" % i, [128, 256], F32, ph) for i in range(4)]
                emb = [[sb("em%d_%d" % (uu, i), [128, 3, 6, 256], BF16, ph) for i in range(2)] for uu in range(2)]
                nmask = sb("nmask", [128, 3, 6, 256], BF16, ph)
                nbb = [sb("nbb%d" % i, [128, 6, 256], BF16, ph) for i in range(2)]
                ptr, fwr = Rot(range(NPT)), Rot(range(4))
                S.op("pool", "memset", writes=["qm0z"], ap=qm[0][64:128, :], constant=0.0)
                S.op("pool", "memset", writes=["qm1z"], ap=qm[1][0:64, :], constant=0.0)
                S.op("pool", "memset", writes=["vpad"], ap=vpad[:], constant=0.0)
                S.op("pool", "memset", writes=["onespad"], ap=onespad[:], constant=0.0)
                S.op("pool", "memset", writes=["onespad"], ap=onespad[:, 0, 0:64], constant=1.0)
                S.op("pool", "memset", writes=["onespad"], ap=onespad[:, 1, 64:128], constant=1.0)
                for gt in range(3):
                    S.dma("pool", nmask[:, gt, :, :], namask_d[:, gt, :, :], writes=[("nmask", gt)])
                em_steps = [(hh_, gt_) for hh_ in range(2) for gt_ in range(3)]
                nbb_of = {}

                def em_dma(u_, k):
                    hh_, gt_ = em_steps[k]
                    i = (u_ * 6 + k) % 2
                    nbb_of[(u_, k)] = i
                    S.dma("pool", nbb[i][:], nab_d[l, 2 * u_ + hh_, :, gt_, :, :], writes=[("nbb", i)])

                def em_build(u_, k):
                    hh_, gt_ = em_steps[k]
                    i = nbb_of[(u_, k)]
                    S.op("act", "activation", reads=[("nbb", i)], writes=[("nbb", i)], out=nbb[i][:], in_=nbb[i][:], func=AF.Exp)
                    S.op("dve", "tensor_tensor", reads=[("nbb", i), ("nmask", gt_)], writes=[("em", u_ % 2, hh_)], out=emb[u_ % 2][hh_][:, gt_, :, :],
                         in0=nbb[i][:], in1=nmask[:, gt_, :, :], op=ALU.mult)

                for k_ in range(6):
                    em_dma(0, k_)
                    em_build(0, k_)
                for u in range(4):
                    load_unit(12 + u, 16 + u, None, 512 + u * 128, want_qf=False)
                    if u == 2:
                        S.dma("pool", wo_a[:], wout_d[l, :, 0:256].rearrange("(kc p) n -> p kc n", p=128), writes=[("wo", 0)])
                    em = emb[u % 2]
                    pipe = JobPipe(LA)
                    groups = [("ctx", None)] if need_ctx else []
                    groups += [("lat", g) for g in range(8)]
                    for gidx_, (kind, g) in enumerate(groups):
                        if u + 1 < 4:
                            if gidx_ < 6:
                                em_dma(u + 1, gidx_)
                            if 1 <= gidx_ < 7:
                                em_build(u + 1, gidx_ - 1)
                        nbk, dbk = accrot.next()
                        if kind == "ctx":
                            s0, n = 0, 256
                            pairs = [((0, 1), None)]
                        else:
                            s0, n = LC + g * 256, 256
                            gt = 0 if g == 0 else (2 if g == 7 else 1)
                            lt0 = 0 if g == 0 else (12 if g == 7 else 2 * g - 2)
                            npair = 3 if gt == 1 else 2
                            pairs = [((0, 1), None)] + [((2 + lt0 + 2 * j, 2 + lt0 + 2 * j + 1), (gt, j)) for j in range(npair)]
                        alljobs = [(hh, kts, emi) for hh in range(2) for (kts, emi) in pairs]
                        state = {}

                        def emit_score(job, s0=s0, n=n, state=state):
                            hh, kts, emi = job
                            b = bankrot.next()
                            state[job] = [b, None]
                            S.ops("pe", [mmi(ps[:, b, 0:256], kb[:, kts[0] * 128:(kts[0] + 1) * 128], qm[hh][:, s0:s0 + n], True, True),
                                         mmi(ps[:, b, 256:512], kb[:, kts[1] * 128:(kts[1] + 1) * 128], qm[hh][:, s0:s0 + n], True, True)],
                                  reads=["kb", ("qm", hh), "qm%dz" % hh], writes=[PS(b)])

                        def emit_p(job, state=state, em=em, u=u):
                            hh, kts, emi = job
                            b = state[job][0]
                            ip = ptr.next()
                            state[job][1] = ip
                            S.op("act", "activation", reads=[PS(b)], writes=[("pt", ip)], out=pt[ip][:], in_=ps[:, b, :], func=AF.Exp, scale=0.125)
                            if emi is not None:
                                gt_, j = emi
                                S.op("dve", "tensor_tensor", reads=[("pt", ip), ("em", u % 2, hh)], writes=[("pt", ip)],
                                     out=pt[ip][:].rearrange("p (a c) -> p a c", a=2), in0=pt[ip][:].rearrange("p (a c) -> p a c", a=2),
                                     in1=em[hh][:, gt_, 2 * j:2 * j + 2, :], op=ALU.mult)

                        def emit_av(job, first, last, state=state, nbk=nbk, dbk=dbk):
                            hh, kts, emi = job
                            ip = state[job][1]
                            insts = []
                            for half in range(2):
                                kt = kts[half]
                                f_ = first and half == 0
                                l_ = last and half == 1
                                insts.append(mmi(ps[:, nbk, 0:256], vpad[:, kt, hh, :], pt[ip][:, half * 256:(half + 1) * 256], f_, l_))
                                insts.append(mmi(ps[:, dbk, 0:256], onespad[:, hh, :], pt[ip][:, half * 256:(half + 1) * 256], f_, l_))
                            S.ops("pe", insts, reads=[("pt", ip), ("vpad", hh), "vpad", "onespad"], writes=[("psacc", nbk), ("psacc", dbk)])

                        def fin(nbk=nbk, dbk=dbk, s0=s0, n=n, u=u):
                            i1, i2 = fwr.next(), fwr.next()
                            S.op("act", "activation", reads=[("psacc", dbk)], writes=[("fw", i1)], out=fw[i1][:, :n], in_=ps[:, dbk, :n], func=AF.Ln)
                            S.op("dve", "tensor_copy", reads=[("psacc", nbk)], writes=[("fw", i2)], out=fw[i2][:, :n], in_=ps[:, nbk, :n])
                            S.op("act", "activation", reads=[("fw", i1)], writes=[("fw", i1)], out=fw[i1][:, :n], in_=fw[i1][:, :n], func=AF.Exp,
                                 scale=-1.0)
                            S.op("dve", "tensor_tensor", reads=[("fw", i2), ("fw", i1)], writes=tile_keys("hT", s0, n),
                                 out=hT[:, 4 + u, s0:s0 + n], in0=fw[i2][:, :n], in1=fw[i1][:, :n], op=ALU.mult)

                        pipe.run_stage(alljobs, emit_score, emit_p, emit_av, fin)
                    pipe.flush()

            if stage == "na":
                break

            with S.phase() as ph:
                wo_b = sb("wo_b", [128, 8, 768], BF16, ph)
                S.dma("pool", wo_b[:], wout_d[l, :, 256:1024].rearrange("(kc p) n -> p kc n", p=128), writes=[("wo", 1)])
                for nn in range(NCH):
                    wo_h = wo_a if nn < 2 else wo_b
                    nh = nn if nn < 2 else nn - 2
                    for (s0, n) in q_tiles:
                        j = 1 if s0 < LC else 0
                        b = bankrot.next()
                        S.ops("pe", [mmi(ps[:, b, :n], wo_h[:, kc, nh * 128:(nh + 1) * 128], hT[:, kc, s0:s0 + n], kc == 0, kc == 7)
                                     for kc in range(8)], reads=[("wo", 0 if nn < 2 else 1)] + tile_keys("hT", s0, n), writes=[PS(b)])
                        S.op("dve", "scalar_tensor_tensor", reads=[PS(b), "mod"] + xres_keys(s0, n, [nn]), writes=xres_keys(s0, n, [nn]),
                             out=xres[:, nn, s0:s0 + n], in0=ps[:, b, :n], scalar=mod[:, 16 + nn, j:j + 1], in1=xres[:, nn, s0:s0 + n],
                             op0=ALU.mult, op1=ALU.add)
            Wo.close()
            if stage == "mid":
                break

            is_moe = (l % 2 == 1)
            with contextlib.ExitStack() as Fs:
                cb = None
                FGM = 4
                wgu = [sb("wgu%d" % i, [128, 8, 2, FGM * 128], BF16, Fs) for i in range(2)]
                Wg0 = moe_wg_d[0] if is_moe else ffn_wg_d
                Wu0 = moe_wu_d[0] if is_moe else ffn_wu_d
                S.dma("pool", wgu[0][:, :, 0, 0:FGM * 128], Wg0[:, 0:FGM * 128].rearrange("(kc p) n -> p kc n", p=128), writes=[("wg", 0)])
                S.dma("pool", wgu[0][:, :, 1, 0:FGM * 128], Wu0[:, 0:FGM * 128].rearrange("(kc p) n -> p kc n", p=128), writes=[("wu", 0)])
                if is_moe:
                    wr = sb("wr", [128, 8, NE], F32, Fs)
                    cb = sb("cb", [128, T], F32, Fs)
                    S.dma("sp", wr[:], moe_wr_d.rearrange("(kc p) e -> p kc e", p=128), writes=["wr"])
                    with S.phase() as ph:
                        lgT = sb("lgT", [8, T], F32, ph)
                        combT = sb("combT", [8, T], F32, ph)
                        ltm = sb("ltm", [128, 16, NE], F32, ph)
                        lt2 = sb("lt2", [128, 16, NE], F32, ph)
                        eq1 = sb("eq1", [128, 16, NE], F32, ph)
                        eq2 = sb("eq2", [128, 16, NE], F32, ph)
                        m1 = sb("m1", [128, 16], F32, ph)
                        m2 = sb("m2", [128, 16], F32, ph)
                        w2 = sb("w2", [128, 16], F32, ph)
                        w1 = sb("w1", [128, 16], F32, ph)
                        rmsnorm_to_hT(1, q_tiles, router=(wr, lgT))
                        bt = 7
                        S.ops("pe", [("transpose", dict(out=ps[:, bt, tt * 8:(tt + 1) * 8], in_=lgT[:, tt * 128:(tt + 1) * 128],
                                                        identity=ident[0:8, 0:8])) for tt in range(16)], reads=["lgT", "ident"], writes=[PS(bt)])
                        S.op("dve", "tensor_copy", reads=[PS(bt)], writes=["ltm"], out=ltm[:].rearrange("p t e -> p (t e)"), in_=ps[:, bt, 0:128])
                        S.op("dve", "tensor_reduce", reads=["ltm"], writes=["m1"], out=m1[:], in_=ltm[:], axis=AX.X, op=ALU.max)
                        S.op("dve", "tensor_tensor", reads=["ltm", "m1"], writes=["eq1"], out=eq1[:], in0=ltm[:],
                             in1=m1[:].unsqueeze(2).to_broadcast([128, 16, NE]), op=ALU.is_equal)
                        S.op("dve", "scalar_tensor_tensor", reads=["eq1", "ltm"], writes=["lt2"], out=lt2[:].rearrange("p t e -> p (t e)"),
                             in0=eq1[:].rearrange("p t e -> p (t e)"), scalar=-1.0e30, in1=ltm[:].rearrange("p t e -> p (t e)"),
                             op0=ALU.mult, op1=ALU.add)
                        S.op("dve", "tensor_reduce", reads=["lt2"], writes=["m2"], out=m2[:], in_=lt2[:], axis=AX.X, op=ALU.max)
                        S.op("dve", "tensor_tensor", reads=["lt2", "m2"], writes=["eq2"], out=eq2[:], in0=lt2[:],
                             in1=m2[:].unsqueeze(2).to_broadcast([128, 16, NE]), op=ALU.is_equal)
                        S.op("dve", "tensor_tensor", reads=["m1", "m2"], writes=["w2"], out=w2[:], in0=m2[:], in1=m1[:], op=ALU.subtract)
                        S.op("act", "activation", reads=["w2"], writes=["w2"], out=w2[:], in_=w2[:], func=AF.Sigmoid)
                        S.op("dve", "tensor_scalar", reads=["w2"], writes=["w1"], out=w1[:], in0=w2[:], scalar1=-1.0, scalar2=1.0,
                             op0=ALU.mult, op1=ALU.add)
                        S.op("dve", "tensor_tensor", reads=["eq1", "w1"], writes=["eq1"], out=eq1[:], in0=eq1[:],
                             in1=w1[:].unsqueeze(2).to_broadcast([128, 16, NE]), op=ALU.mult)
                        S.op("dve", "tensor_tensor", reads=["eq2", "w2"], writes=["eq2"], out=eq2[:], in0=eq2[:],
                             in1=w2[:].unsqueeze(2).to_broadcast([128, 16, NE]), op=ALU.mult)
                        S.op("dve", "tensor_tensor", reads=["eq1", "eq2"], writes=["eq1"], out=eq1[:], in0=eq1[:], in1=eq2[:], op=ALU.add)
                        dbg_dump("comb", eq1[:], ["eq1"])
                        for q4 in range(4):
                            b = bankrot.next()
                            S.ops("pe", [("transpose", dict(out=ps[0:8, b, j_ * 128:(j_ + 1) * 128], in_=eq1[:, q4 * 4 + j_, :], identity=ident[:]))
                                         for j_ in range(4)], reads=["eq1", "ident"], writes=[PS(b)])
                            S.op("act", "activation", reads=[PS(b)], writes=["combT"], out=combT[:, q4 * 512:(q4 + 1) * 512], in_=ps[0:8, b, :],
                                 func=AF.Identity)
                        S.dma("sp", comb_d[:, :], combT[:], reads=["combT"], writes=["comb_d"])
                else:
                    rmsnorm_to_hT(1, q_tiles)
                if stage == "norm2":
                    break

                with S.phase() as ph:
                    FGM = 4
                    fgroups = [(c0, min(FGM, NFC - c0)) for c0 in range(0, NFC, FGM)]
                    wdn = [sb("wdn%d" % i, [128, FGM, D], BF16, ph) for i in range(2)]
                    actb = sb("actb", [128, FGM, TT], BF16, ph)
                    sgb = [sb("sgb%d" % i, [128, 512], BF16, ph) for i in range(2)]
                    tf = [sb("tf%d" % i, [128, 512], F32, ph) for i in range(2)] if is_moe else None
                    sgr, tfr = Rot(range(2)), Rot(range(2))
                    pre_next = (not is_moe) and (l + 1 < n_layers)
                    if pre_next:
                        wmn = [sb("wmn%d" % i, [128, 8, 512], BF16, ph) for i in range(2)]
                        nbper = (12 + len(fgroups) - 1) // len(fgroups)
                    gi = 0
                    for e in range(NE if is_moe else 1):
                        Wg = moe_wg_d[e] if is_moe else ffn_wg_d
                        Wu = moe_wu_d[e] if is_moe else ffn_wu_d
                        Wd = moe_wd_d[e] if is_moe else ffn_wd_d
                        if is_moe:
                            S.dma("sp", cb[:], comb_d[e:e + 1, :].partition_broadcast(128), reads=["comb_d"], writes=["cb"])
                        for (c0, fg) in fgroups:
                            i = gi % 2
                            gi += 1
                            f0 = c0 * 128
                            if not (e == 0 and c0 == 0):
                                S.dma("pool", wgu[i][:, :, 0, 0:fg * 128], Wg[:, f0:f0 + fg * 128].rearrange("(kc p) n -> p kc n", p=128), writes=[("wg", i)])
                                S.dma("pool", wgu[i][:, :, 1, 0:fg * 128], Wu[:, f0:f0 + fg * 128].rearrange("(kc p) n -> p kc n", p=128), writes=[("wu", i)])
                            S.dma("pool", wdn[i][:, 0:fg, :], Wd[f0:f0 + fg * 128, :].rearrange("(fc p) n -> p fc n", p=128), writes=[("wd", i)])
                            nbs = []
                            if pre_next:
                                nbs = list(range((gi - 1) * nbper, min(12, gi * nbper)))
                                for nb in nbs:
                                    adaln_load(l + 1, nb, wmn)
                            for fc in range(fg):
                                for (s0, n) in q_tiles:
                                    bg, bu = bankrot.next(), bankrot.next()
                                    S.ops("pe", [mmi(ps[:, bg, :n], wgu[i][:, kc, 0, fc * 128:(fc + 1) * 128], hT[:, kc, s0:s0 + n], kc == 0, kc == 7)
                                                 for kc in range(8)], reads=[("wg", i)] + tile_keys("hT", s0, n), writes=[PS(bg)])
                                    S.ops("pe", [mmi(ps[:, bu, :n], wgu[i][:, kc, 1, fc * 128:(fc + 1) * 128], hT[:, kc, s0:s0 + n], kc == 0, kc == 7)
                                                 for kc in range(8)], reads=[("wu", i)] + tile_keys("hT", s0, n), writes=[PS(bu)])
                                    si = sgr.next()
                                    S.op("act", "activation", reads=[PS(bg)], writes=[("sgb", si)], out=sgb[si][:, :n], in_=ps[:, bg, :n], func=AF.Silu)
                                    if is_moe:
                                        ti_ = tfr.next()
                                        S.op("dve", "tensor_tensor", reads=[PS(bu), ("sgb", si)], writes=[("tf", ti_)], out=tf[ti_][:, :n],
                                             in0=ps[:, bu, :n], in1=sgb[si][:, :n], op=ALU.mult)
                                        S.op("pool", "tensor_tensor", reads=[("tf", ti_), "cb"], writes=[("actb", fc, s0)],
                                             out=actb[:, fc, s0:s0 + n], in0=tf[ti_][:, :n], in1=cb[:, s0 - LC:s0 - LC + n], op=ALU.mult)
                                    else:
                                        S.op("dve", "tensor_tensor", reads=[PS(bu), ("sgb", si)], writes=[("actb", fc, s0)],
                                             out=actb[:, fc, s0:s0 + n], in0=ps[:, bu, :n], in1=sgb[si][:, :n], op=ALU.mult)
                            for (s0, n) in q_tiles:
                                for nn in range(NCH):
                                    j = 1 if s0 < LC else 0
                                    b = bankrot.next()
                                    S.ops("pe", [mmi(ps[:, b, :n], wdn[i][:, fc, nn * 128:(nn + 1) * 128], actb[:, fc, s0:s0 + n], fc == 0, fc == fg - 1)
                                                 for fc in range(fg)], reads=[("wd", i)] + [("actb", fc, s0) for fc in range(fg)], writes=[PS(b)])
                                    xk_ = xres_keys(s0, n, [nn])
                                    S.op("dve", "scalar_tensor_tensor", reads=[PS(b), "mod"] + xk_, writes=xk_,
                                         out=xres[:, nn, s0:s0 + n], in0=ps[:, b, :n], scalar=mod[:, 40 + nn, j:j + 1], in1=xres[:, nn, s0:s0 + n],
                                         op0=ALU.mult, op1=ALU.add)
                            for nb in nbs:
                                adaln_mm(nb, wmn)
                    if pre_next:
                        adaln_fin(l + 1, modL[l + 1], AvecL[l + 1])
                        adaln_done.add(l + 1)
            if stage == "layer0":
                break

        if Ls_holder:
            Ls_holder[-1].close()
        if dbg and "hT" in dbg:
            for c in range(NCH):
                S.dma("sp", dbg_d["hT"][c, :, :], hT[:, c, :], reads=tile_keys("hT", 0, TT), writes=[("dbgout", "hT", c)])
        if dbg and "mod" in dbg:
            dbg_dump("mod", mod[:], ["mod"])
        if dbg and "xres" in dbg:
            for c in range(NCH):
                S.dma("sp", dbg_d["xres"][c, :, :], xres[:, c, :], reads=xres_keys(0, TT), writes=[("dbgout", "xres", c)])
        if stage == "full":
            with S.phase() as ph:
                xo = [sb("xo%d" % i, [128, D], F32, ph) for i in range(2)]
                for tt in range(2, NKT):
                    buf = xo[tt % 2]
                    key = ("xo", tt % 2)
                    for half in range(2):
                        b = bankrot.next()
                        S.ops("pe", [("transpose", dict(out=ps[:, b, j * 128:(j + 1) * 128],
                                                        in_=xres[:, half * 4 + j, tt * 128:(tt + 1) * 128],
                                                        identity=ident[:])) for j in range(4)],
                              reads=xres_keys(tt * 128, 128) + ["ident"], writes=[PS(b)])
                        if half == 0:
                            S.op("act", "activation", reads=[PS(b)], writes=[key], out=buf[:, 0:512], in_=ps[:, b, :], func=AF.Identity)
                        else:
                            S.op("dve", "tensor_copy", reads=[PS(b)], writes=[key], out=buf[:, 512:1024], in_=ps[:, b, :])
                    S.dma("sp", out_d[(tt - 2) * 128:(tt - 1) * 128, :], buf[:], reads=[key], writes=[("out", tt)])
        S.barrier()
        S.finish()
        global LAST_SCHED
        LAST_SCHED = S
    return nc


def build_rest(L):
    pass


def kernel(**inputs):
    shared, per_core = host_prep(inputs)
    nc = build()
    in_maps = [dict(shared, **m) for m in per_core]
    res = run_bass_kernel_spmd(nc, in_maps, core_ids=list(range(8)))
    return np.stack([r["out"] for r in res.results], axis=0)
```

```python
import contextlib
import math
import numpy as np
import concourse.bass as bass
import concourse.mybir as mybir
from concourse.bass_utils import run_bass_kernel_spmd

F32 = mybir.dt.float32
BF16 = mybir.dt.bfloat16
ALU = mybir.AluOpType
AF = mybir.ActivationFunctionType
AX = mybir.AxisListType

D = 1024
T = 2048
LC = 256
TT = T + LC
NCH = 8
DFF = 2816
NFC = DFF // 128
NE = 8
EPS = 1e-6
LN8 = math.log(0.125)
QT = [(0, 256), (256, 512), (768, 512), (1280, 512), (1792, 512)]
NKT = TT // 128


class Sched:
    ENGS = ("pe", "act", "dve", "pool", "sp")

    def __init__(self, nc, stack, n_dma_sems=24):
        self.nc = nc
        self.streams = {e: [] for e in self.ENGS}
        self.esem = {e: stack.enter_context(nc.semaphore("s_" + e)) for e in self.ENGS}
        self.ecnt = {e: 0 for e in self.ENGS}
        self.dsem = {}
        self.dcnt = {}
        self.dnext = {}
        for q in ("sp", "pool", "act"):
            self.dsem[q] = [stack.enter_context(nc.semaphore("d_%s%d" % (q, i))) for i in range(n_dma_sems)]
            self.dcnt[q] = [0] * n_dma_sems
            self.dnext[q] = 0
        self.waited = {e: {} for e in self.ENGS}
        self.st = {}
        self.ninst = 0
        self.marks = []

    def _deps(self, reads, writes):
        deps = {}

        def add(t):
            if t is None:
                return
            s, v = t
            if deps.get(id(s), (None, -1))[1] < v:
                deps[id(s)] = (s, v)

        for k in reads:
            b = self.st.get(k)
            if b:
                add(b["w"])
        for k in writes:
            b = self.st.get(k)
            if b:
                add(b["w"])
                for t in b["r"].values():
                    add(t)
        return deps

    def _emit_waits(self, eng, deps):
        for sid, (s, v) in deps.items():
            if eng == "pe" and s is self.esem["pe"]:
                continue
            if self.waited[eng].get(sid, -1) >= v:
                continue
            self.waited[eng][sid] = v
            self.streams[eng].append(lambda e, s=s, v=v: e.wait_ge(s, v))

    def _update(self, reads, writes, ticket):
        s, v = ticket
        for k in reads:
            b = self.st.setdefault(k, {"w": None, "r": {}})
            b["r"][id(s)] = ticket
        for k in writes:
            self.st[k] = {"w": ticket, "r": {}}

    def ops(self, eng, insts, reads=(), writes=()):
        deps = self._deps(reads, writes)
        self._emit_waits(eng, deps)
        self.ecnt[eng] += 1
        sem = self.esem[eng]
        for name, kw in insts[:-1]:
            self.streams[eng].append(lambda e, name=name, kw=kw: getattr(e, name)(**kw))
        name, kw = insts[-1]
        self.streams[eng].append(lambda e, name=name, kw=kw, sem=sem: getattr(e, name)(**kw).then_inc(sem, 1))
        t = (sem, self.ecnt[eng])
        self._update(reads, writes, t)
        self.ninst += len(insts)
        return t

    def op(self, eng, name, reads=(), writes=(), **kw):
        return self.ops(eng, [(name, kw)], reads, writes)

    def dma(self, q, out, in_, reads=(), writes=(), **kw):
        deps = self._deps(reads, writes)
        i = self.dnext[q]
        self.dnext[q] = (i + 1) % len(self.dsem[q])
        sem = self.dsem[q][i]
        if self.dcnt[q][i] > 0:
            deps[id(sem)] = (sem, 16 * self.dcnt[q][i])
        self._emit_waits(q, deps)
        self.dcnt[q][i] += 1
        self.streams[q].append(
            lambda e, out=out, in_=in_, sem=sem, kw=kw: e.dma_start(out=out, in_=in_, **kw).then_inc(sem, 16))
        t = (sem, 16 * self.dcnt[q][i])
        self._update(reads, writes, t)
        self.ninst += 1
        return t

    def mark(self, label):
        self.marks.append((label, dict(self.ecnt)))

    def barrier(self):
        deps = {}
        for e in self.ENGS:
            if self.ecnt[e] > 0:
                deps[id(self.esem[e])] = (self.esem[e], self.ecnt[e])
        for q in self.dsem:
            for i, s in enumerate(self.dsem[q]):
                if self.dcnt[q][i] > 0:
                    deps[id(s)] = (s, 16 * self.dcnt[q][i])
        for e in self.ENGS:
            self._emit_waits(e, deps)

    @contextlib.contextmanager
    def phase(self):
        with contextlib.ExitStack() as ph:
            yield ph
        self.barrier()
        self.mark('phase_end')

    def wait_all(self, eng, keys):
        self._emit_waits(eng, self._deps(keys, ()))

    def finish(self):
        nc = self.nc
        with nc.Block() as block:
            @block.tensor
            def _(e):
                for f in self.streams["pe"]:
                    f(e)

            @block.scalar
            def _(e):
                for f in self.streams["act"]:
                    f(e)

            @block.vector
            def _(e):
                for f in self.streams["dve"]:
                    f(e)

            @block.gpsimd
            def _(e):
                for f in self.streams["pool"]:
                    f(e)

            @block.sync
            def _(e):
                for f in self.streams["sp"]:
                    f(e)


class Rot:
    def __init__(self, items):
        self.items = list(items)
        self.i = 0

    def next(self):
        v = self.items[self.i]
        self.i = (self.i + 1) % len(self.items)
        return v


def _fm(v):
    v = np.asarray(v)
    n = v.shape[-1] // 128
    r = v.reshape(v.shape[:-1] + (n, 128))
    return np.ascontiguousarray(np.moveaxis(r, -1, 0))


def _rope_perm():
    f = np.arange(64)
    w = f % 32
    return np.where(w < 16, f + 16, f - 16)


def _fm_cols():
    perm64 = _rope_perm()
    perm256 = np.concatenate([h * 64 + perm64 for h in range(4)])
    a = np.arange
    cols = np.concatenate([
        a(0, 256), perm256, 256 + a(0, 256), 256 + perm256, 768 + a(0, 256), 1792 + a(0, 256),
        1024 + a(0, 256), 1280 + a(0, 256), 2064 + a(0, 512), 2576 + a(0, 512)])
    return cols


def _v_cols():
    a = np.arange
    return np.concatenate([512 + a(0, 256), 1536 + a(0, 256), 3088 + a(0, 512)])


def _na_tables():
    dr = np.zeros((128, 3, 6, 4, 64), dtype=np.int64)
    dc = np.zeros((128, 3, 6, 4, 64), dtype=np.int64)
    ok = np.zeros((128, 3, 6, 4, 64), dtype=bool)
    p = np.arange(128)
    par = (p // 64)[:, None, None]
    w = (p % 64)[:, None, None]
    i = np.arange(4)[None, :, None]
    cq = np.arange(64)[None, None, :]
    start_c = np.clip(cq - 8, 0, 48)
    col_ok = (w >= start_c) & (w < start_c + 16)
    dcv = np.clip(w - cq + 15, 0, 30)
    for gt in range(3):
        for a in range(6):
            m = 2 * a + par
            if gt == 1:
                row_ok = (m >= i) & (m <= i + 7)
                drv = m - i + 3
            else:
                row_ok = (m <= 7) & (i >= 0)
                drv = (m - i + 7) if gt == 0 else (m - i + 3)
            okk = row_ok & col_ok
            ok[:, gt, a] = okk
            dr[:, gt, a] = np.clip(np.broadcast_to(drv, okk.shape), 0, 14)
            dc[:, gt, a] = np.broadcast_to(dcv, okk.shape)
    return dr.reshape(128, 3, 6, 256), dc.reshape(128, 3, 6, 256), ok.reshape(128, 3, 6, 256)


def host_constants():
    c = {}
    c["ident"] = np.eye(128, dtype=np.float32)
    c["ones"] = np.ones((128, 128), dtype=np.float32)
    bd = np.zeros((128, 128), dtype=np.float32)
    bd[:64, :64] = 1.0
    bd[64:, 64:] = 1.0
    c["onesbd"] = bd
    sel = np.zeros((8, 8, 128), dtype=np.float32)
    for r in range(8):
        sel[r, r, :] = 1.0
    c["sel"] = sel
    t = np.arange(T)
    row = (t // 64).astype(np.float32)
    col = (t % 64).astype(np.float32)
    inv = (np.float32(10000.0) ** (-np.arange(0, 32, 2, dtype=np.float32) / np.float32(32))).astype(np.float32)
    f = np.arange(64)
    w = f % 32
    ii = w % 16
    pos = np.where((f < 32)[:, None], row[None, :], col[None, :]).astype(np.float32)
    ang = (pos * inv[ii][:, None]).astype(np.float32)
    cosv = np.cos(ang).astype(np.float32)
    sinv = np.sin(ang).astype(np.float32)
    sinv = np.where((w < 16)[:, None], -sinv, sinv).astype(np.float32)
    c["rope"] = np.ascontiguousarray(np.stack([np.tile(cosv, (2, 1)), np.tile(sinv, (2, 1))], axis=1))
    pp = np.arange(128, dtype=np.float32)[:, None]
    cc_ = np.arange(512, dtype=np.float32)[None, :]
    c["rtab"] = np.ascontiguousarray(cc_ - pp)
    c["dtab"] = np.ascontiguousarray(np.tile((128.0 * (np.arange(32) - 17)).astype(np.float32)[None, :], (128, 1)))
    c["ctab"] = np.ascontiguousarray(np.tile(np.arange(512, dtype=np.float32)[None, :], (128, 1)))
    c["dmp"] = np.ascontiguousarray((128.0 * (np.arange(32) - 17)).astype(np.float32)[None, :] - pp)
    sel2 = np.zeros((8, 2, 2, 128), dtype=np.float32)
    for dn in range(2):
        for u in range(2):
            sel2[dn * 4 + 2 * u, dn, u, 0:64] = 1.0
            sel2[dn * 4 + 2 * u + 1, dn, u, 64:128] = 1.0
    c["sel2"] = sel2
    _, _, ok = _na_tables()
    c["namask"] = np.ascontiguousarray(ok.astype(np.float32))
    return c


def host_prep(inp):
    shared = dict(host_constants())
    f32 = np.float32
    shared["w_mod"] = np.ascontiguousarray(inp["w_mod"])
    shared["bmod"] = _fm(inp["b_mod"])
    shared["nrm"] = np.ascontiguousarray(
        np.stack([_fm(inp["norm_mix"]), _fm(inp["norm_ffn"])], axis=2))
    w_in = np.asarray(inp["w_in"])
    shared["w_in_fm"] = np.ascontiguousarray(w_in[:, :, _fm_cols()])
    shared["w_in_v"] = np.ascontiguousarray(w_in[:, :, _v_cols()])
    shared["w_in_g"] = np.ascontiguousarray(w_in[:, :, 2048:2064])
    gb = np.asarray(inp["mlstm_gate_b"]).reshape(2, 16)
    shared["gateb"] = np.ascontiguousarray(np.stack([gb[:, 0:8], gb[:, 8:16]], axis=0).transpose(2, 1, 0))
    shared["retd"] = np.ascontiguousarray(np.tile(np.asarray(inp["ret_decay"]).reshape(1, 2, 8), (128, 1, 1)))
    rd = np.asarray(inp["ret_decay"])
    hidx = (np.arange(128) // 64)[:, None] + 2 * np.arange(2)[None, :]
    shared["retdpp"] = np.ascontiguousarray(rd[:, :, hidx].transpose(2, 0, 1, 3))
    cw = np.asarray(inp["mlstm_conv_w"])
    shared["convw"] = np.ascontiguousarray(_fm(cw).astype(f32))
    shared["convb"] = np.ascontiguousarray(_fm(np.asarray(inp["mlstm_conv_b"])))
    gq = np.tile(np.asarray(inp["na_q_gain"]), (1, 2))
    gk = np.tile(np.asarray(inp["na_k_gain"]), (1, 2))
    shared["nagain"] = np.ascontiguousarray(np.stack([gq, gk], axis=2).transpose(1, 0, 2))
    dr, dc, ok = _na_tables()
    rpb = np.asarray(inp["na_rpb"])
    shared["nab"] = np.ascontiguousarray(rpb[:, :, dr, dc])
    shared["w_out"] = np.ascontiguousarray(inp["w_out"])
    shared["ffn_wg"] = np.ascontiguousarray(inp["ffn_w_gate"][0])
    shared["ffn_wu"] = np.ascontiguousarray(inp["ffn_w_up"][0])
    shared["ffn_wd"] = np.ascontiguousarray(inp["ffn_w_down"][0])
    shared["moe_wr"] = np.ascontiguousarray(inp["moe_router"][0])
    shared["moe_wg"] = np.ascontiguousarray(inp["moe_w_gate"][0])
    shared["moe_wu"] = np.ascontiguousarray(inp["moe_w_up"][0])
    shared["moe_wd"] = np.ascontiguousarray(inp["moe_w_down"][0])
    per_core = []
    for b in range(8):
        m = {}
        m["x"] = np.ascontiguousarray(inp["x"][b])
        m["ctx"] = np.ascontiguousarray(inp["ctx"][b])
        m["cc"] = np.ascontiguousarray(np.stack([_fm(inp["c"][b]), _fm(inp["c_ctx"])], axis=2))
        per_core.append(m)
    return shared, per_core


N_LAYERS = 2
LAST_SCHED = None


class StopBuild(Exception):
    pass


class JobPipe:
    def __init__(self, la):
        self.la = la
        self.q = []

    def run_stage(self, jobs, es, ep, ea, fin):
        nj = len(jobs)
        for idx, job in enumerate(jobs):
            es(job)
            ep(job)
            self.q.append((ea, job, idx == 0, idx == nj - 1, fin if idx == nj - 1 else None))
            while len(self.q) > self.la:
                self._pop()

    def _pop(self):
        ea, job, f, l, fin = self.q.pop(0)
        ea(job, f, l)
        if fin is not None:
            fin()

    def flush(self):
        while self.q:
            self._pop()


def build(stage="full", dbg=None, n_layers=N_LAYERS):
    nc = bass.Bass("TRN2", target_bir_lowering=False)

    def dram_in(name, shape, dt=F32):
        return nc.dram_tensor(name, list(shape), dt, kind="ExternalInput").ap()

    x_d = dram_in("x", [T, D])
    ctx_d = dram_in("ctx", [LC, D])
    cc_d = dram_in("cc", [128, 8, 2])
    ident_d = dram_in("ident", [128, 128])
    ones_d = dram_in("ones", [128, 128])
    onesbd_d = dram_in("onesbd", [128, 128])
    sel_d = dram_in("sel", [8, 8, 128])
    rope_d = dram_in("rope", [128, 2, T])
    rtab_d = dram_in("rtab", [128, 512])
    dtab_d = dram_in("dtab", [128, 32])
    namask_d = dram_in("namask", [128, 3, 6, 256])
    wmod_d = dram_in("w_mod", [2, D, 6 * D])
    bmod_d = dram_in("bmod", [128, 2, 48])
    nrm_d = dram_in("nrm", [128, 2, 2, 8])
    winfm_d = dram_in("w_in_fm", [2, D, 3072])
    winv_d = dram_in("w_in_v", [2, D, 1024])
    wing_d = dram_in("w_in_g", [2, D, 16])
    gateb_d = dram_in("gateb", [8, 2, 2])
    retd_d = dram_in("retd", [128, 2, 8])
    convw_d = dram_in("convw", [128, 2, 3, 4])
    convb_d = dram_in("convb", [128, 2, 4])
    nagain_d = dram_in("nagain", [128, 2, 2])
    nab_d = dram_in("nab", [2, 8, 128, 3, 6, 256])
    ctab_d = dram_in("ctab", [128, 512])
    dmp_d = dram_in("dmp", [128, 32])
    sel2_d = dram_in("sel2", [8, 2, 2, 128])
    retdpp_d = dram_in("retdpp", [128, 2, 2, 2])
    wout_d = dram_in("w_out", [2, D, D])
    ffn_wg_d = dram_in("ffn_wg", [D, DFF])
    ffn_wu_d = dram_in("ffn_wu", [D, DFF])
    ffn_wd_d = dram_in("ffn_wd", [DFF, D])
    moe_wr_d = dram_in("moe_wr", [D, NE])
    moe_wg_d = dram_in("moe_wg", [NE, D, DFF])
    moe_wu_d = dram_in("moe_wu", [NE, D, DFF])
    moe_wd_d = dram_in("moe_wd", [NE, DFF, D])

    out_d = nc.dram_tensor("out", [T, D], F32, kind="ExternalOutput").ap()
    scr_kind = "ExternalOutput" if (dbg and "scr" in dbg) else "Internal"
    qkg_d = nc.dram_tensor("qkg_scr", [20, 128, TT], BF16, kind=scr_kind).ap()
    v_d = nc.dram_tensor("v_scr", [NKT, 128, 1024], BF16, kind=scr_kind).ap()
    comb_d = nc.dram_tensor("comb_scr", [NE, T], F32, kind="Internal").ap()
    dbg_d = {}
    if dbg:
        for name, spec in dbg.items():
            if name == "scr":
                continue
            shape, dt = spec
            dbg_d[name] = nc.dram_tensor("dbg_" + name, list(shape), dt, kind="ExternalOutput").ap()

    with contextlib.ExitStack() as st:
        S = Sched(nc, st)

        sb_cnt = [0]

        def sb(name, shape, dt, stack=None):
            sb_cnt[0] += 1
            return (stack or st).enter_context(nc.sbuf_tensor("sb%d_%s" % (sb_cnt[0], name), list(shape), dt))

        ps = st.enter_context(nc.psum_tensor("ps", [128, 8, 512], F32))
        bankrot = Rot(range(6))

        def PS(b):
            return ("ps", b)

        def mmi(out, lhsT, rhs, start, stop):
            return ("matmul", dict(out=out, lhsT=lhsT, rhs=rhs, start=start, stop=stop))

        def tile_keys(name, s0, n):
            return [(name, tt) for tt in range(s0 // 128, (s0 + n + 127) // 128)]

        def dbg_dump(name, src_ap, reads):
            if dbg and name in dbg:
                S.dma("sp", dbg_d[name], src_ap, reads=reads, writes=[("dbgout", name)])

        xres = sb("xres", [128, NCH, TT], F32)
        hT = sb("hT", [128, NCH, TT], BF16)
        ident = sb("ident", [128, 128], F32)
        ones = sb("ones", [128, 128], F32)
        onesbd = sb("onesbd", [128, 128], F32)
        cc = sb("cc", [128, 8, 2], F32)
        sc2 = sb("sc2", [128, 8, 2], BF16)
        bmod = sb("bmod", [128, 2, 48], F32)
        nrm = sb("nrm", [128, 2, 2, 8], F32)
        modL = [sb("mod%d" % i, [128, 48, 2], F32) for i in range(2)]
        AvecL = [sb("Avec%d" % i, [128, 2, 8, 2], F32) for i in range(2)]
        mod, Avec = modL[0], AvecL[0]
        convw = sb("convw", [128, 2, 3, 4], F32)
        convb = sb("convb", [128, 2, 4], F32)
        nagain = sb("nagain", [128, 2, 2], F32)
        gateb = sb("gateb", [8, 2, 2], F32)
        retd = sb("retd", [128, 2, 8], F32)
        lg = sb("lg", [128, 2, 8], F32)
        nlg = sb("nlg", [128, 2, 8], F32)
        dtab = sb("dtab", [128, 32], F32)
        rtab = sb("rtab", [128, 512], F32)
        sel = sb("sel", [8, 8, 128], F32)
        onesb = sb("onesb", [128, 64], BF16)

        for (t_, d_, k_) in [(ident, ident_d, "ident"), (ones, ones_d, "ones"), (onesbd, onesbd_d, "onesbd"),
                             (cc, cc_d, "cc"), (bmod, bmod_d, "bmod"), (nrm, nrm_d, "nrm"), (convw, convw_d, "convw"),
                             (convb, convb_d, "convb"), (nagain, nagain_d, "nagain"), (gateb, gateb_d, "gateb"),
                             (retd, retd_d, "retd"), (dtab, dtab_d, "dtab"), (rtab, rtab_d, "rtab"), (sel, sel_d, "sel")]:
            S.dma("sp", t_[:], d_, writes=[k_])
        S.op("act", "activation", reads=["cc"], writes=["sc2"], out=sc2[:], in_=cc[:], func=AF.Silu)
        S.op("pool", "memset", writes=["onesb"], ap=onesb[:], constant=1.0)
        S.op("act", "activation", reads=["retd"], writes=["lg"], out=lg[:], in_=retd[:], func=AF.Exp, scale=-1.0)
        S.op("act", "activation", reads=["lg"], writes=["nlg"], out=nlg[:], in_=lg[:], func=AF.Ln, bias=1.0)
        S.op("dve", "tensor_scalar", reads=["nlg"], writes=["lg"], out=lg[:], in0=nlg[:], scalar1=-1.0, scalar2=None, op0=ALU.mult)

        with S.phase() as ph:
            xin = [sb("xin%d" % i, [128, D], F32, ph) for i in range(2)]
            for tt in range(NKT):
                buf = xin[tt % 2]
                key = "xin%d" % (tt % 2)
                src = ctx_d[tt * 128:(tt + 1) * 128, :] if tt < 2 else x_d[(tt - 2) * 128:(tt - 1) * 128, :]
                S.dma("sp", buf[:], src, writes=[key])
                for half in range(2):
                    b = bankrot.next()
                    S.ops("pe", [("transpose", dict(out=ps[:, b, j * 128:(j + 1) * 128],
                                                    in_=buf[:, (half * 4 + j) * 128:(half * 4 + j + 1) * 128],
                                                    identity=ident[:])) for j in range(4)],
                          reads=[key, "ident"], writes=[PS(b)])
                    dst = xres[:, half * 4:half * 4 + 4, tt * 128:(tt + 1) * 128]
                    srcp = ps[:, b, :].rearrange("p (j n) -> p j n", j=4)
                    if half == 0:
                        S.op("act", "activation", reads=[PS(b)], writes=[("xres", tt, c_) for c_ in range(0, 4)], out=dst, in_=srcp, func=AF.Identity)
                    else:
                        S.op("dve", "tensor_copy", reads=[PS(b)], writes=[("xres", tt, c_) for c_ in range(4, 8)], out=dst, in_=srcp)

        def xres_keys(s0, n, chunks=None):
            cs = range(NCH) if chunks is None else chunks
            return [("xres", tt, c_) for tt in range(s0 // 128, (s0 + n + 127) // 128) for c_ in cs]

        def rmsnorm_to_hT(kind, tiles, router=None):
            b_lo = 0 if kind == 0 else 24
            with S.phase() as ph:
                sq = [sb("sq%d" % i, [128, 512], F32, ph) for i in range(4)]
                rstd = [sb("rstd%d" % i, [128, 512], F32, ph) for i in range(2)]
                tmp = [sb("ntmp%d" % i, [128, 512], F32, ph) for i in range(6)]
                sqr, tmr = Rot(range(4)), Rot(range(6))
                sq_on_dve = set(range(NCH)) if router is not None else {1, 3, 5, 7}
                nbanks = {}

                def emit_sq(ti):
                    (s0, n) = tiles[ti]
                    b = bankrot.next()
                    nbanks[ti] = b
                    for c in range(NCH):
                        i = sqr.next()
                        if c in sq_on_dve:
                            S.op("dve", "tensor_tensor", reads=xres_keys(s0, n, [c]), writes=["sq%d" % i],
                                 out=sq[i][:, :n], in0=xres[:, c, s0:s0 + n], in1=xres[:, c, s0:s0 + n], op=ALU.mult)
                        else:
                            S.op("act", "activation", reads=xres_keys(s0, n, [c]), writes=["sq%d" % i],
                                 out=sq[i][:, :n], in_=xres[:, c, s0:s0 + n], func=AF.Square)
                        S.ops("pe", [mmi(ps[:, b, :n], ones[:], sq[i][:, :n], c == 0, c == NCH - 1)],
                              reads=["sq%d" % i, "ones"], writes=[PS(b)])

                emit_sq(0)
                for ti, (s0, n) in enumerate(tiles):
                    j = 1 if s0 < LC else 0
                    xk = xres_keys(s0, n)
                    if ti + 1 < len(tiles):
                        emit_sq(ti + 1)
                    b = nbanks[ti]
                    r = rstd[ti % 2]
                    rk = "rstd%d" % (ti % 2)
                    S.op("act", "activation", reads=[PS(b)], writes=[rk],
                         out=r[:, :n], in_=ps[:, b, :n], func=AF.Ln, scale=1.0 / D, bias=EPS)
                    S.op("act", "activation", reads=[rk], writes=[rk], out=r[:, :n], in_=r[:, :n], func=AF.Exp, scale=-0.5)
                    for c in range(NCH):
                        i = tmr.next()
                        S.op("dve", "scalar_tensor_tensor", reads=xres_keys(s0, n, [c]) + [rk, "Avec"], writes=["ntmp%d" % i],
                             out=tmp[i][:, :n], in0=xres[:, c, s0:s0 + n], scalar=Avec[:, kind, c, j:j + 1], in1=r[:, :n],
                             op0=ALU.mult, op1=ALU.mult)
                        S.op("act", "activation", reads=["ntmp%d" % i, "mod"], writes=tile_keys("hT", s0, n),
                             out=hT[:, c, s0:s0 + n], in_=tmp[i][:, :n], func=AF.Identity, bias=mod[:, b_lo + c, j:j + 1])
                        if router is not None:
                            wr_, lgT_ = router
                            S.op("act", "activation", reads=["ntmp%d" % i, "mod"], writes=["ntmp%d" % i], out=tmp[i][:, :n], in_=tmp[i][:, :n],
                                 func=AF.Identity, bias=mod[:, b_lo + c, j:j + 1])
                            S.ops("pe", [mmi(ps[0:8, 7, :n], wr_[:, c, :], tmp[i][:, :n], c == 0, c == NCH - 1)],
                                  reads=["ntmp%d" % i, "wr"], writes=[PS(7)])
                    if router is not None:
                        S.op("act", "activation", reads=[PS(7)], writes=["lgT"], out=lgT_[:, s0 - LC:s0 - LC + n], in_=ps[0:8, 7, :n], func=AF.Identity)

        bmd = 6

        def adaln_load(lx, nb, wm):
            w = wm[nb % 2]
            wk = "wm%d" % (nb % 2)
            for kc in range(8):
                S.dma("pool", w[:, kc, :], wmod_d[lx, kc * 128:(kc + 1) * 128, nb * 512:(nb + 1) * 512], writes=[(wk, kc)])

        def adaln_mm(nb, wm):
            w = wm[nb % 2]
            wk = "wm%d" % (nb % 2)
            insts = []
            for mm_ in range(4):
                m = nb * 4 + mm_
                for kc in range(8):
                    insts.append(mmi(ps[:, bmd, m * 2:m * 2 + 2], w[:, kc, mm_ * 128:(mm_ + 1) * 128], sc2[:, kc, :],
                                     kc == 0, kc == 7))
            S.ops("pe", insts, reads=[(wk, kc) for kc in range(8)] + ["sc2"], writes=[PS(bmd)])

        def adaln_fin(lx, mod_t, Avec_t):
            for j in range(2):
                S.op("dve", "tensor_tensor", reads=[PS(bmd), "bmod"], writes=["mod"],
                     out=mod_t[:, :, j], in0=ps[:, bmd, 0:96].rearrange("p (m j) -> p m j", j=2)[:, :, j],
                     in1=bmod[:, lx, :], op=ALU.add)
            for kind in range(2):
                s_lo = 8 if kind == 0 else 32
                for j in range(2):
                    S.op("dve", "scalar_tensor_tensor", reads=["mod", "nrm"], writes=["Avec"],
                         out=Avec_t[:, kind, :, j], in0=mod_t[:, s_lo:s_lo + 8, j], scalar=1.0, in1=nrm[:, lx, kind, :],
                         op0=ALU.add, op1=ALU.mult)

        Ls_holder = []
        adaln_done = set()
        for l in range(n_layers):
            need_ctx = l < N_LAYERS - 1
            mod, Avec = modL[l], AvecL[l]
            if l not in adaln_done:
                with S.phase() as ph:
                    wm = [sb("wm%d" % i, [128, 8, 512], BF16, ph) for i in range(2)]
                    for nb in range(12):
                        adaln_load(l, nb, wm)
                        adaln_mm(nb, wm)
                    adaln_fin(l, mod, Avec)

            blk_i = [0]
            Ls = contextlib.ExitStack()
            Ls_holder.append(Ls)
            Fq = sb("Fq", [8, TT], F32, Ls)
            aT = sb("aT", [128, NKT, 8], F32, Ls)
            Ws = contextlib.ExitStack()
            wblk = [sb("wblk%d" % i, [128, 8, 512], BF16, Ws) for i in range(2)]
            for i_ in range(2):
                for kc in range(8):
                    S.dma("pool", wblk[i_][:, kc, :], winfm_d[l, kc * 128:(kc + 1) * 128, i_ * 512:(i_ + 1) * 512],
                          writes=[("wblk", i_, kc)])

            wcv = sb("wcv", [128, 8, 512], BF16, Ws)
            wgt = sb("wgt", [128, 8, 16], BF16, Ws)

            rmsnorm_to_hT(0, QT)
            if stage == "norm":
                break

            for kc in range(8):
                S.dma("pool", wcv[:, kc, :], winfm_d[l, kc * 128:(kc + 1) * 128, 1536:2048], writes=[("wcv", kc)])
            for kc in range(8):
                S.dma("pool", wgt[:, kc, :], wing_d[l, kc * 128:(kc + 1) * 128, :], writes=[("wgt", kc)])
            with S.phase() as ph:
                stg = [sb("stg%d" % i, [128, TT], BF16, ph) for i in range(2)]
                ropeb = [sb("ropeb%d" % i, [128, 2, 512], F32, ph) for i in range(2)]
                t12 = [sb("t12_%d" % i, [128, 512], F32, ph) for i in range(9)]
                vst = [sb("vst%d" % i, [128, 512], BF16, ph) for i in range(2)]
                t12r = Rot(range(9))

                def load_block(src2d):
                    i = blk_i[0] % 2
                    blk_i[0] += 1
                    if blk_i[0] > 2:
                        for kc in range(8):
                            S.dma("pool", wblk[i][:, kc, :], src2d[kc * 128:(kc + 1) * 128, :], writes=[("wblk", i, kc)])
                    return wblk[i], [("wblk", i, kc) for kc in range(8)]

                def proj(w, wkeys, mcol, s0, n):
                    b = bankrot.next()
                    S.ops("pe", [mmi(ps[:, b, :n], w[:, kc, mcol * 128:(mcol + 1) * 128], hT[:, kc, s0:s0 + n], kc == 0, kc == 7)
                                 for kc in range(8)], reads=wkeys + tile_keys("hT", s0, n), writes=[PS(b)])
                    return b

                def store_chunk(si, chunk_id):
                    S.dma("sp", qkg_d[chunk_id, :, :], stg[si][:], reads=[("stg", si)], writes=[("qkg", chunk_id)])

                for blk, chunk0 in ((0, 0), (1, 2)):
                    w, wk = load_block(winfm_d[l, :, blk * 512:(blk + 1) * 512])
                    for (s0, n) in QT:
                        if s0 >= LC:
                            rb = ropeb[(s0 // 512) % 2]
                            rkey = ("ropeb", (s0 // 512) % 2)
                            S.dma("sp", rb[:], rope_d[:, :, s0 - LC:s0 - LC + n], writes=[rkey])
                        for hh in range(2):
                            bA = proj(w, wk, hh, s0, n)
                            if s0 < LC:
                                S.op("act", "activation", reads=[PS(bA)], writes=[("stg", hh)],
                                     out=stg[hh][:, s0:s0 + n], in_=ps[:, bA, :n], func=AF.Identity)
                                continue
                            bB = proj(w, wk, 2 + hh, s0, n)
                            i1, i2 = t12r.next(), t12r.next()
                            S.op("dve", "tensor_tensor", reads=[PS(bA), rkey], writes=[("t12", i1)],
                                 out=t12[i1][:, :n], in0=ps[:, bA, :n], in1=rb[:, 0, :n], op=ALU.mult)
                            S.op("dve", "tensor_tensor", reads=[PS(bB), rkey], writes=[("t12", i2)],
                                 out=t12[i2][:, :n], in0=ps[:, bB, :n], in1=rb[:, 1, :n], op=ALU.mult)
                            S.op("pool", "tensor_tensor", reads=[("t12", i1), ("t12", i2)], writes=[("stg", hh)],
                                 out=stg[hh][:, s0:s0 + n], in0=t12[i1][:, :n], in1=t12[i2][:, :n], op=ALU.add)
                    for hh in range(2):
                        store_chunk(hh, chunk0 + hh)

                def plain_chunk(w, wk, mcol, si):
                    for (s0, n) in QT:
                        b = proj(w, wk, mcol, s0, n)
                        S.op("act", "activation", reads=[PS(b)], writes=[("stg", si)],
                             out=stg[si][:, s0:s0 + n], in_=ps[:, b, :n], func=AF.Identity)

                w, wk = load_block(winfm_d[l, :, 1024:1536])
                for hh in range(2):
                    plain_chunk(w, wk, hh, hh)
                    store_chunk(hh, 4 + hh)
                for hh in range(2):
                    plain_chunk(w, wk, 2 + hh, hh)
                    store_chunk(hh, 10 + hh)
                for blk, chunk0, gi in ((4, 12, 0), (5, 16, 1)):
                    w, wk = load_block(winfm_d[l, :, blk * 512:(blk + 1) * 512])
                    items = [(cidx, s0, n) for cidx in range(4) for (s0, n) in QT]
                    pend = None

                    def qk_finish(pv):
                        cidx_, s0_, n_, i1_, i2_, i3_ = pv
                        si_ = cidx_ % 2
                        b2 = bankrot.next()
                        S.ops("pe", [mmi(ps[:, b2, :n_], onesbd[:], t12[i2_][:, :n_], True, True)],
                              reads=[("t12", i2_), "onesbd"], writes=[PS(b2)])
                        S.op("act", "activation", reads=[PS(b2)], writes=[("t12", i3_)],
                             out=t12[i3_][:, :n_], in_=ps[:, b2, :n_], func=AF.Ln, scale=1.0 / 64, bias=EPS)
                        S.op("act", "activation", reads=[("t12", i3_)], writes=[("t12", i3_)], out=t12[i3_][:, :n_], in_=t12[i3_][:, :n_],
                             func=AF.Exp, scale=-0.5)
                        S.op("dve", "scalar_tensor_tensor", reads=[("t12", i1_), ("t12", i3_), "nagain"], writes=[("stg", si_)],
                             out=stg[si_][:, s0_:s0_ + n_], in0=t12[i1_][:, :n_], scalar=nagain[:, l, gi:gi + 1], in1=t12[i3_][:, :n_],
                             op0=ALU.mult, op1=ALU.mult)
                        if s0_ == QT[-1][0]:
                            store_chunk(si_, chunk0 + cidx_)

                    for (cidx, s0, n) in items:
                        b = proj(w, wk, cidx, s0, n)
                        i1, i2, i3 = t12r.next(), t12r.next(), t12r.next()
                        S.op("act", "activation", reads=[PS(b)], writes=[("t12", i1)],
                             out=t12[i1][:, :n], in_=ps[:, b, :n], func=AF.Identity)
                        S.op("act", "activation", reads=[PS(b)], writes=[("t12", i2)],
                             out=t12[i2][:, :n], in_=ps[:, b, :n], func=AF.Square)
                        if pend is not None:
                            qk_finish(pend)
                        pend = (cidx, s0, n, i1, i2, i3)
                    qk_finish(pend)
                for half in range(2):
                    w, wk = load_block(winv_d[l, :, half * 512:(half + 1) * 512])
                    for tt in range(NKT):
                        b = bankrot.next()
                        S.ops("pe", [mmi(ps[:, b, :], hT[:, kc, tt * 128:(tt + 1) * 128], w[:, kc, :], kc == 0, kc == 7)
                                     for kc in range(8)], reads=wk + [("hT", tt)], writes=[PS(b)])
                        vi = tt % 2
                        if tt % 2 == 0:
                            S.op("act", "activation", reads=[PS(b)], writes=[("vst", vi)], out=vst[vi][:], in_=ps[:, b, :], func=AF.Identity)
                        else:
                            S.op("dve", "tensor_copy", reads=[PS(b)], writes=[("vst", vi)], out=vst[vi][:], in_=ps[:, b, :])
                        S.dma("sp", v_d[tt, :, half * 512:(half + 1) * 512], vst[vi][:], reads=[("vst", vi)], writes=[("v_d", tt, half)])


            with S.phase() as ph:
                stg = [sb("stg%d" % i, [128, TT], BF16, ph) for i in range(2)]
                cpad = sb("cpad", [128, TT + 4], F32, ph)
                ybuf = sb("ybuf", [128, TT], F32, ph)
                S.op("pool", "memset", writes=["cpad"], ap=cpad[:], constant=0.0)
                igt = sb("igt", [8, TT], F32, ph)
                fgt = sb("fgt", [8, TT], F32, ph)
                gall = sb("gall", [8, TT], F32, ph)
                wgk = [("wgt", kc) for kc in range(8)]
                for (s0, n) in QT:
                    for gi, dst in enumerate((igt, fgt)):
                        b = bankrot.next()
                        S.ops("pe", [mmi(ps[0:8, b, :n], wgt[:, kc, gi * 8:(gi + 1) * 8], hT[:, kc, s0:s0 + n], kc == 0, kc == 7)
                                     for kc in range(8)], reads=wgk + tile_keys("hT", s0, n), writes=[PS(b)])
                        S.op("act", "activation", reads=[PS(b), "gateb"], writes=["igt" if gi == 0 else "fgt"],
                             out=dst[:, s0:s0 + n], in_=ps[0:8, b, :n], func=AF.Identity, bias=gateb[:, l, gi:gi + 1])

                S.op("act", "activation", reads=["fgt"], writes=["fgt"], out=fgt[:], in_=fgt[:], func=AF.Exp, scale=-1.0)
                S.op("act", "activation", reads=["fgt"], writes=["fgt"], out=fgt[:], in_=fgt[:], func=AF.Ln, bias=1.0)

                def conv_chunk(w, wk, mcol, si, cj):
                    for (s0, n) in QT:
                        b = proj(w, wk, mcol, s0, n)
                        o = 1 + s0 if s0 < LC else 3 + s0
                        S.op("dve", "tensor_copy", reads=[PS(b)], writes=["cpad"], out=cpad[:, o:o + n], in_=ps[:, b, :n])
                    for (s0, n, o) in ((0, LC, 1), (LC, T, 259)):
                        S.op("act", "activation", reads=["cpad", "convw", "convb"], writes=["ybuf"],
                             out=ybuf[:, s0:s0 + n], in_=cpad[:, o:o + n], func=AF.Identity,
                             scale=convw[:, l, 1, cj:cj + 1], bias=convb[:, l, cj:cj + 1])
                        S.op("dve", "scalar_tensor_tensor", reads=["cpad", "ybuf"], writes=["ybuf"],
                             out=ybuf[:, s0:s0 + n], in0=cpad[:, o - 1:o - 1 + n], scalar=convw[:, l, 0, cj:cj + 1],
                             in1=ybuf[:, s0:s0 + n], op0=ALU.mult, op1=ALU.add)
                        S.op("dve", "scalar_tensor_tensor", reads=["cpad", "ybuf"], writes=["ybuf"],
                             out=ybuf[:, s0:s0 + n], in0=cpad[:, o + 1:o + 1 + n], scalar=convw[:, l, 2, cj:cj + 1],
                             in1=ybuf[:, s0:s0 + n], op0=ALU.mult, op1=ALU.add)
                        S.op("act", "activation", reads=["ybuf"], writes=[("stg", si)],
                             out=stg[si][:, s0:s0 + n], in_=ybuf[:, s0:s0 + n], func=AF.Silu)

                w, wk = wcv, [("wcv", kc) for kc in range(8)]
                for hh in range(2):
                    conv_chunk(w, wk, hh, hh, hh)
                    store_chunk(hh, 6 + hh)
                for hh in range(2):
                    conv_chunk(w, wk, 2 + hh, hh, 2 + hh)
                    store_chunk(hh, 8 + hh)

                S.op("dve", "tensor_scalar", reads=["fgt"], writes=["fgt"], out=fgt[:], in0=fgt[:], scalar1=-1.0, scalar2=None, op0=ALU.mult)
                S.op("dve", "tensor_tensor_scan", reads=["fgt", "ones"], writes=["gall"],
                     out=gall[:], data0=ones[0:8, 0:1].to_broadcast([8, TT]), data1=fgt[:], initial=0.0, op0=ALU.mult, op1=ALU.add)
                dbg_dump("logf", fgt[:], ["fgt"])
                dbg_dump("gall", gall[:], ["gall"])
                sgn = sb("sgn", [8, 4], F32, ph)
                S.op("dve", "tensor_tensor", reads=["sel"], writes=["sgn"], out=sgn[:, 1:2], in0=sel[:, 4, 0:1], in1=sel[:, 5, 0:1], op=ALU.add)
                S.op("dve", "tensor_tensor", reads=["sel", "sgn"], writes=["sgn"], out=sgn[:, 2:3], in0=sel[:, 6, 0:1], in1=sel[:, 7, 0:1], op=ALU.add)
                S.op("dve", "tensor_tensor", reads=["sgn"], writes=["sgn"], out=sgn[:, 1:2], in0=sgn[:, 1:2], in1=sgn[:, 2:3], op=ALU.add)
                S.op("dve", "tensor_scalar", reads=["sgn"], writes=["sgn"], out=sgn[:, 0:1], in0=sgn[:, 1:2], scalar1=-2.0, scalar2=1.0,
                     op0=ALU.mult, op1=ALU.add)
                S.op("dve", "tensor_tensor", reads=["sgn", "gall"], writes=["sgn"], out=sgn[:, 2:3], in0=sgn[:, 1:2], in1=gall[:, LC - 1:LC], op=ALU.mult)
                S.op("dve", "tensor_tensor", reads=["sgn", "gall"], writes=["sgn"], out=sgn[:, 3:4], in0=gall[:, LC - 1:LC], in1=gall[:, TT - 1:TT], op=ALU.add)
                S.op("dve", "tensor_tensor", reads=["sgn"], writes=["sgn"], out=sgn[:, 3:4], in0=sgn[:, 3:4], in1=sgn[:, 1:2], op=ALU.mult)
                S.op("dve", "tensor_scalar", reads=["gall", "sgn"], writes=["Fq"], out=Fq[:], in0=gall[:], scalar1=sgn[:, 0:1], scalar2=None, op0=ALU.mult)
                S.op("dve", "scalar_tensor_tensor", reads=["fgt", "sgn", "Fq"], writes=["Fq"], out=Fq[:], in0=fgt[:], scalar=sgn[:, 1:2], in1=Fq[:],
                     op0=ALU.mult, op1=ALU.add)
                S.op("dve", "tensor_scalar", reads=["Fq", "sgn"], writes=["Fq"], out=Fq[:, 0:LC], in0=Fq[:, 0:LC], scalar1=sgn[:, 2:3], scalar2=None, op0=ALU.add)
                S.op("dve", "tensor_scalar", reads=["Fq", "sgn"], writes=["Fq"], out=Fq[:, LC:TT], in0=Fq[:, LC:TT], scalar1=sgn[:, 3:4], scalar2=None, op0=ALU.add)
                S.op("dve", "tensor_tensor", reads=["igt", "Fq"], writes=["igt"], out=igt[:], in0=igt[:], in1=Fq[:], op=ALU.subtract)
                S.op("dve", "tensor_scalar", reads=["igt"], writes=["igt"], out=igt[:], in0=igt[:], scalar1=LN8, scalar2=None, op0=ALU.add)
                bt = 7
                S.ops("pe", [("transpose", dict(out=ps[:, bt, kt * 8:(kt + 1) * 8], in_=igt[:, kt * 128:(kt + 1) * 128],
                                                identity=ident[0:8, 0:8])) for kt in range(NKT)],
                      reads=["igt", "ident"], writes=[PS(bt)])
                S.op("dve", "tensor_copy", reads=[PS(bt)], writes=["aT"], out=aT[:].rearrange("p k r -> p (k r)"), in_=ps[:, bt, 0:NKT * 8])
                dbg_dump("Fq", Fq[:], ["Fq"])
                dbg_dump("aT", aT[:], ["aT"])

            Ws.close()
            if stage == "inproj":
                break

            q_tiles = QT if need_ctx else QT[1:]
            NPT = 8
            LA = 5
            with S.phase() as ph:
                qm = [sb("qm%d" % i, [128, TT], BF16, ph) for i in range(2)]
                kb = sb("kb", [128, TT], BF16, ph)
                gbuf = sb("gbuf", [128, TT], BF16, ph)
                vpad = sb("vpad", [128, NKT, 2, 128], BF16, ph)
                onespad = sb("onespad", [128, 2, 128], BF16, ph)
                pt = [sb("pt%d" % i, [128, 512], BF16, ph) for i in range(NPT)]
                dA = [sb("dA%d" % i, [128, 512], BF16, ph) for i in range(8)]
                dB = [sb("dB%d" % i, [128, 512], BF16, ph) for i in range(2)]
                dDg = [[sb("dDg%d_%d" % (hh_, j_), [128, 512], BF16, ph) for j_ in range(4)] for hh_ in range(2)]
                fw = [sb("fw%d" % i, [128, 512], F32, ph) for i in range(3)]
                hacc = [sb("hacc%d" % i, [128, 512], F32, ph) for i in range(2)]
                cfa = [sb("cfa%d" % i, [128, 512], BF16, ph) for i in range(2)]
                qs = [[sb("qs%d_%d" % (hh, i), [128, 512], BF16, ph) for i in range(4)] for hh in range(2)]
                btab = sb("btab", [128, 2, 3, 32], F32, ph)
                rft = sb("rft", [128, 2, 2, 32], F32, ph)
                c4 = sb("c4", [128, 4], F32, ph)
                lgpp = sb("lgpp", [128, 2, 2, 2], F32, ph)
                ctab = sb("ctab", [128, 512], F32, ph)
                dmp = sb("dmp", [128, 32], F32, ph)
                rfm = [sb("rfm%d" % i, [128, NKT], F32, ph) for i in range(4)]
                nfr = [sb("nfr%d" % i, [128, 1], F32, ph) for i in range(4)]
                ptr, dAr, dBr, fwr = Rot(range(NPT)), Rot(range(8)), Rot(range(2)), Rot(range(3))
                haccr, cfr, rfmr, nfrr = Rot(range(2)), Rot(range(2)), Rot(range(4)), Rot(range(4))
                qsr = [Rot(range(4)), Rot(range(4))]
                accrot = Rot([(6, None), (7, None)])
                evr = Rot(["act", "dve"])

                S.dma("sp", ctab[:], ctab_d, writes=["ctab"])
                S.dma("sp", dmp[:], dmp_d, writes=["dmp"])
                S.dma("sp", lgpp[:, 0, :, :], retdpp_d[:, l, :, :], writes=["lgpp"])
                S.op("act", "activation", reads=["lgpp"], writes=["lgpp"], out=lgpp[:, 1, :, :], in_=lgpp[:, 0, :, :], func=AF.Exp, scale=-1.0)
                S.op("act", "activation", reads=["lgpp"], writes=["lgpp"], out=lgpp[:, 1, :, :], in_=lgpp[:, 1, :, :], func=AF.Ln, bias=1.0)
                S.op("dve", "tensor_scalar", reads=["lgpp"], writes=["lgpp"], out=lgpp[:, 0, :, :], in0=lgpp[:, 1, :, :], scalar1=-1.0, scalar2=None,
                     op0=ALU.mult)
                S.op("pool", "memset", writes=["qm0z"], ap=qm[0][64:128, :], constant=0.0)
                S.op("pool", "memset", writes=["qm1z"], ap=qm[1][0:64, :], constant=0.0)
                S.op("pool", "memset", writes=["vpad"], ap=vpad[:], constant=0.0)
                S.op("pool", "memset", writes=["onespad"], ap=onespad[:], constant=0.0)
                S.op("pool", "memset", writes=["onespad"], ap=onespad[:, 0, 0:64], constant=1.0)
                S.op("pool", "memset", writes=["onespad"], ap=onespad[:, 1, 64:128], constant=1.0)
                for i in range(4):
                    S.op("pool", "memset", writes=[("qs", 0, i)], ap=qs[0][i][64:128, :], constant=0.0)
                    S.op("pool", "memset", writes=[("qs", 1, i)], ap=qs[1][i][0:64, :], constant=0.0)
                HS = [slice(0, 64), slice(64, 128)]

                def load_unit(qc, kc, gc, vcol, want_qf=True, want_qm=True):
                    if want_qf:
                        S.dma("sp", qf[:], qkg_d[qc, :, :], reads=[("qkg", qc)], writes=["qf"])
                    if want_qm:
                        S.dma("sp", qm[0][0:64, :], qkg_d[qc, 0:64, :], reads=[("qkg", qc)], writes=[("qm", 0)])
                        S.dma("sp", qm[1][64:128, :], qkg_d[qc, 64:128, :], reads=[("qkg", qc)], writes=[("qm", 1)])
                    S.dma("sp", kb[:], qkg_d[kc, :, :], reads=[("qkg", kc)], writes=["kb"])
                    if gc is not None:
                        S.dma("sp", gbuf[:], qkg_d[gc, :, :], reads=[("qkg", gc)], writes=["gbuf"])
                    vk = [("v_d", tt, vcol // 512) for tt in range(NKT)]
                    S.dma("sp", vpad[:, :, 0, 0:64], v_d[:, :, vcol:vcol + 64].rearrange("t p c -> p t c"), reads=vk + ["vpad"], writes=[("vpad", 0)])
                    S.dma("sp", vpad[:, :, 1, 64:128], v_d[:, :, vcol + 64:vcol + 128].rearrange("t p c -> p t c"), reads=vk + ["vpad"],
                          writes=[("vpad", 1)])

                def run_jobs(jobs, emit_score, emit_p, emit_av):
                    nj = len(jobs)
                    for idx in range(nj + LA):
                        if idx < nj:
                            emit_score(jobs[idx])
                            emit_p(jobs[idx])
                        if idx >= LA:
                            emit_av(jobs[idx - LA], idx - LA == 0, idx - LA == nj - 1)

                def evac_scaled(b, n, ip, scal):
                    eng = evr.next()
                    if eng == "act":
                        S.op("act", "activation", reads=[PS(b), "rf"], writes=[("pt", ip)], out=pt[ip][:, :n], in_=ps[:, b, :n], func=AF.Identity, scale=scal)
                    else:
                        S.op("dve", "tensor_scalar", reads=[PS(b), "rf"], writes=[("pt", ip)], out=pt[ip][:, :n], in0=ps[:, b, :n], scalar1=scal,
                             scalar2=None, op0=ALU.mult)

                def post_gate(chunk, func, hi, s0, n):
                    i1, i2, i3 = fwr.next(), fwr.next(), fwr.next()
                    hk = ("hacc", hi)
                    S.op("act", "activation", reads=[hk], writes=[("fw", i1)], out=fw[i1][:, :n], in_=hacc[hi][:, :n], func=AF.Square)
                    b = bankrot.next()
                    S.ops("pe", [mmi(ps[:, b, :n], onesbd[:], fw[i1][:, :n], True, True)], reads=[("fw", i1), "onesbd"], writes=[PS(b)])
                    S.op("act", "activation", reads=[PS(b)], writes=[("fw", i2)], out=fw[i2][:, :n], in_=ps[:, b, :n], func=AF.Ln,
                         scale=1.0 / 64, bias=EPS)
                    S.op("act", "activation", reads=[("fw", i2)], writes=[("fw", i2)], out=fw[i2][:, :n], in_=fw[i2][:, :n], func=AF.Exp, scale=-0.5)
                    S.op("act", "activation", reads=["gbuf"], writes=[("fw", i3)], out=fw[i3][:, :n], in_=gbuf[:, s0:s0 + n], func=func)
                    S.op("pool", "tensor_tensor", reads=[hk, ("fw", i2)], writes=[("fw", i2)], out=fw[i2][:, :n], in0=hacc[hi][:, :n],
                         in1=fw[i2][:, :n], op=ALU.mult)
                    S.op("pool", "tensor_tensor", reads=[("fw", i2), ("fw", i3)], writes=tile_keys("hT", s0, n),
                         out=hT[:, chunk, s0:s0 + n], in0=fw[i2][:, :n], in1=fw[i3][:, :n], op=ALU.mult)

                def gen_dec(a_, akey, hh, u, ty, dl, n):
                    h = 2 * u + hh
                    lgf, nlgb = lg[:, l, h:h + 1], nlg[:, l, 4 + h:5 + h]
                    di = dl // 128 + 17
                    S.op("act", "activation", reads=["rtab", "btab", "lg"], writes=[akey], out=a_[:, :n], in_=rtab[:, :n],
                         func=AF.Exp, scale=lgf, bias=btab[:, hh, 0, di:di + 1])
                    ib = dBr.next()
                    b_ = dB[ib]
                    S.op("act", "activation", reads=["rtab", "btab", "nlg"], writes=[("dB", ib)], out=b_[:, :n], in_=rtab[:, :n],
                         func=AF.Exp, scale=nlgb, bias=btab[:, hh, 1 if ty == 3 else 2, di:di + 1])
                    if ty == 3:
                        S.op("pool", "affine_select", reads=[akey], writes=[akey], out=a_[:, :n], in_=a_[:, :n],
                             pattern=[[1, n]], compare_op=ALU.is_ge, fill=0.0, base=dl, channel_multiplier=-1)
                        S.op("pool", "affine_select", reads=[("dB", ib)], writes=[("dB", ib)], out=b_[:, :n], in_=b_[:, :n],
                             pattern=[[-1, n]], compare_op=ALU.is_ge, fill=0.0, base=-dl, channel_multiplier=1)
                    S.op("pool", "tensor_tensor", reads=[akey, ("dB", ib)], writes=[akey], out=a_[:, :n], in0=a_[:, :n],
                         in1=b_[:, :n], op=ALU.add)

                for u in range(2):
                    load_unit(u, 2 + u, 4 + u, u * 128, want_qf=False)
                    S.op("dve", "tensor_scalar", reads=["lgpp"], writes=["c4"], out=c4[:, 2:3], in0=lgpp[:, 0, 1, u:u + 1], scalar1=511.0, scalar2=None,
                         op0=ALU.mult)
                    S.op("act", "activation", reads=["ctab", "lgpp"], writes=[("cfa", 0)], out=cfa[0][:], in_=ctab[:], func=AF.Exp,
                         scale=lgpp[:, 0, 0, u:u + 1])
                    S.op("act", "activation", reads=["ctab", "lgpp", "c4"], writes=[("cfa", 1)], out=cfa[1][:], in_=ctab[:], func=AF.Exp,
                         scale=lgpp[:, 1, 1, u:u + 1], bias=c4[:, 2:3])
                    for hh in range(2):
                        h = 2 * u + hh
                        lgf, nlgb = lg[:, l, h:h + 1], nlg[:, l, 4 + h:5 + h]
                        S.op("dve", "tensor_scalar", reads=["dtab", "lg"], writes=["btab"], out=btab[:, hh, 0, :], in0=dtab[:], scalar1=lgf,
                             scalar2=LN8, op0=ALU.mult, op1=ALU.add)
                        S.op("dve", "tensor_scalar", reads=["dtab", "nlg"], writes=["btab"], out=btab[:, hh, 1, :], in0=dtab[:], scalar1=nlgb,
                             scalar2=LN8, op0=ALU.mult, op1=ALU.add)
                        S.op("dve", "tensor_scalar", reads=["lg"], writes=["c4"], out=c4[:, hh:hh + 1], in0=lg[:, l, 4 + h:5 + h],
                             scalar1=float(TT), scalar2=LN8, op0=ALU.mult, op1=ALU.add)
                        S.op("dve", "tensor_scalar", reads=["dtab", "nlg", "c4"], writes=["btab"], out=btab[:, hh, 2, :], in0=dtab[:], scalar1=nlgb,
                             scalar2=c4[:, hh:hh + 1], op0=ALU.mult, op1=ALU.add)
                        S.op("act", "activation", reads=["dmp", "lg"], writes=["rf"], out=rft[:, hh, 0, 17:32], in_=dmp[:, 17:32], func=AF.Exp, scale=lgf, bias=LN8)
                        S.op("dve", "tensor_scalar", reads=["nlg"], writes=["c4"], out=c4[:, 3:4], in0=nlgb, scalar1=511.0, scalar2=LN8,
                             op0=ALU.mult, op1=ALU.add)
                        S.op("act", "activation", reads=["dmp", "nlg", "c4"], writes=["rf"], out=rft[:, hh, 1, 0:14], in_=dmp[:, 0:14], func=AF.Exp, scale=nlgb,
                             bias=c4[:, 3:4])
                    for hh in range(2):
                        for j_ in range(4):
                            gen_dec(dDg[hh][j_], ("dDg", hh, j_), hh, u, 3, -128 * j_, 512)
                    def ret_pre(s0, n, u=u):
                        lat = s0 >= LC
                        qsl = {}
                        if lat:
                            for hh in range(2):
                                for ty in (1, 2):
                                    i = qsr[hh].next()
                                    qsl[(hh, ty)] = i
                                    S.op("pool" if ty == 1 else "dve", "tensor_tensor", reads=[("qm", hh), ("cfa", ty - 1)], writes=[("qs", hh, i)],
                                         out=qs[hh][i][HS[hh], :n], in0=qm[hh][HS[hh], s0:s0 + n], in1=cfa[ty - 1][HS[hh], :n], op=ALU.mult)
                        alljobs = []
                        for hh in range(2):
                            if not lat:
                                alljobs += [(hh, 0, 3), (hh, 1, 3)]
                            else:
                                for kt in range(NKT):
                                    k0 = kt * 128
                                    if kt < 2:
                                        alljobs.append((hh, kt, 4))
                                    elif k0 + 127 < s0:
                                        alljobs.append((hh, kt, 1))
                                    elif k0 > s0 + n - 1:
                                        alljobs.append((hh, kt, 2))
                                    else:
                                        alljobs.append((hh, kt, 3))
                        dectile = {}
                        for job in alljobs:
                            hh_, kt_, ty_ = job
                            if ty_ == 3:
                                j_ = (kt_ * 128 - s0) // 128
                                dectile[job] = (dDg[hh_][j_], ("dDg", hh_, j_))
                            elif ty_ == 4:
                                ia = dAr.next()
                                gen_dec(dA[ia], ("dA", ia), hh_, u, 4, s0 - kt_ * 128, n)
                                dectile[job] = (dA[ia], ("dA", ia))
                        return qsl, alljobs, dectile

                    pipe = JobPipe(LA)
                    pres = {0: ret_pre(*q_tiles[0])}
                    for qi_, (s0, n) in enumerate(q_tiles):
                        if qi_ + 1 < len(q_tiles):
                            pres[qi_ + 1] = ret_pre(*q_tiles[qi_ + 1])
                        qsl, alljobs, dectile = pres.pop(qi_)
                        nbk, _ = accrot.next()
                        state = {}

                        def emit_score(job, s0=s0, n=n, state=state, qsl=qsl):
                            hh, kt, ty = job
                            b = bankrot.next()
                            state[job] = [b, None]
                            if ty in (1, 2):
                                i = qsl[(hh, ty)]
                                rhs, rk = qs[hh][i][:, :n], ("qs", hh, i)
                            else:
                                rhs, rk = qm[hh][:, s0:s0 + n], ("qm", hh)
                            S.ops("pe", [mmi(ps[:, b, :n], kb[:, kt * 128:(kt + 1) * 128], rhs, True, True)], reads=["kb", rk, "qm%dz" % hh],
                                  writes=[PS(b)])

                        def emit_p(job, s0=s0, n=n, state=state, u=u, dectile=dectile):
                            hh, kt, ty = job
                            b = state[job][0]
                            dl = s0 - kt * 128
                            di = dl // 128 + 17
                            ip = ptr.next()
                            state[job][1] = ip
                            if ty in (1, 2):
                                evac_scaled(b, n, ip, rft[:, hh, ty - 1, di:di + 1])
                                return
                            a_, akey = dectile[job]
                            S.op("dve", "tensor_tensor", reads=[PS(b), akey], writes=[("pt", ip)], out=pt[ip][:, :n], in0=ps[:, b, :n],
                                 in1=a_[:, :n], op=ALU.mult)

                        def emit_av(job, first, last, n=n, state=state, nbk=nbk):
                            hh, kt, ty = job
                            ip = state[job][1]
                            S.ops("pe", [mmi(ps[:, nbk, :n], vpad[:, kt, hh, :], pt[ip][:, :n], first, last)],
                                  reads=[("pt", ip), ("vpad", hh), "vpad"], writes=[("psacc", nbk)])

                        def fin(nbk=nbk, s0=s0, n=n, u=u):
                            hi = haccr.next()
                            S.op("act", "activation", reads=[("psacc", nbk)], writes=[("hacc", hi)], out=hacc[hi][:, :n], in_=ps[:, nbk, :n],
                                 func=AF.Identity)
                            if dbg and ("ret%d" % u) in dbg:
                                S.dma("sp", dbg_d["ret%d" % u][:, s0:s0 + n], hacc[hi][:, :n], reads=[("hacc", hi)], writes=[("dbgout", "ret", u, s0)])
                            post_gate(u, AF.Silu, hi, s0, n)

                        pipe.run_stage(alljobs, emit_score, emit_p, emit_av, fin)
                    pipe.flush()

                if stage == "ret":
                    break


            with S.phase() as ph:
                qf = sb("qf", [128, TT], BF16, ph)
                kb = sb("kb", [128, TT], BF16, ph)
                gbuf = sb("gbuf", [128, TT], BF16, ph)
                vpad = sb("vpad", [128, NKT, 2, 128], BF16, ph)
                onespad = sb("onespad", [128, 2, 128], BF16, ph)
                pt = [sb("pt%d" % i, [128, 512], BF16, ph) for i in range(NPT)]
                mtile = [[sb("mt%d_%d" % (dn_, j_), [128, 512], BF16, ph) for j_ in range(4)] for dn_ in range(2)]
                fw = [sb("fw%d" % i, [128, 512], F32, ph) for i in range(4)]
                hacc = [sb("hacc%d" % i, [128, 512], F32, ph) for i in range(2)]
                cfa = [sb("cfa%d" % i, [128, 512], F32, ph) for i in range(2)]
                qs = [[sb("qs%d_%d" % (hh, i), [128, 512], BF16, ph) for i in range(4)] for hh in range(2)]
                btab = sb("btab", [128, 2, 3, 32], F32, ph)
                rft = sb("rft", [128, 2, 2, 32], F32, ph)
                c4 = sb("c4", [128, 4], F32, ph)
                lgpp = sb("lgpp", [128, 2, 2, 2], F32, ph)
                ctab = sb("ctab", [128, 512], F32, ph)
                dmp = sb("dmp", [128, 32], F32, ph)
                sel2 = sb("sel2", [8, 2, 2, 128], F32, ph)
                fref = sb("fref", [128, 2, 2, 8], F32, ph)
                rfm = [sb("rfm%d" % i, [128, NKT], F32, ph) for i in range(4)]
                nfr = [sb("nfr%d" % i, [128, 1], F32, ph) for i in range(4)]
                ptr, dAr, dBr, fwr = Rot(range(NPT)), Rot(range(8)), Rot(range(2)), Rot(range(4))
                haccr, cfr, rfmr, nfrr = Rot(range(2)), Rot(range(2)), Rot(range(4)), Rot(range(4))
                qsr = [Rot(range(4)), Rot(range(4))]
                accrot = Rot([(6, 7)])
                evr = Rot(["act", "dve"])

                S.dma("sp", ctab[:], ctab_d, writes=["ctab"])
                S.dma("sp", dmp[:], dmp_d, writes=["dmp"])
                S.dma("sp", sel2[:], sel2_d, writes=["sel2"])
                S.dma("sp", lgpp[:, 0, :, :], retdpp_d[:, l, :, :], writes=["lgpp"])
                S.op("act", "activation", reads=["lgpp"], writes=["lgpp"], out=lgpp[:, 1, :, :], in_=lgpp[:, 0, :, :], func=AF.Exp, scale=-1.0)
                S.op("act", "activation", reads=["lgpp"], writes=["lgpp"], out=lgpp[:, 1, :, :], in_=lgpp[:, 1, :, :], func=AF.Ln, bias=1.0)
                S.op("dve", "tensor_scalar", reads=["lgpp"], writes=["lgpp"], out=lgpp[:, 0, :, :], in0=lgpp[:, 1, :, :], scalar1=-1.0, scalar2=None,
                     op0=ALU.mult)
                S.op("pool", "memset", writes=["vpad"], ap=vpad[:], constant=0.0)
                S.op("pool", "memset", writes=["onespad"], ap=onespad[:], constant=0.0)
                S.op("pool", "memset", writes=["onespad"], ap=onespad[:, 0, 0:64], constant=1.0)
                S.op("pool", "memset", writes=["onespad"], ap=onespad[:, 1, 64:128], constant=1.0)
                for i in range(4):
                    S.op("pool", "memset", writes=[("qs", 0, i)], ap=qs[0][i][64:128, :], constant=0.0)
                    S.op("pool", "memset", writes=[("qs", 1, i)], ap=qs[1][i][0:64, :], constant=0.0)
                for dn_ in range(2):
                    for j_ in range(4):
                        S.op("pool", "memset", writes=["mtile"], ap=mtile[dn_][j_][:], constant=1.0)
                        dl_ = -128 * j_
                        if dn_ == 0:
                            S.op("pool", "affine_select", reads=["mtile"], writes=["mtile"], out=mtile[dn_][j_][:], in_=mtile[dn_][j_][:],
                                 pattern=[[1, 512]], compare_op=ALU.is_ge, fill=0.0, base=dl_, channel_multiplier=-1)
                        else:
                            S.op("pool", "affine_select", reads=["mtile"], writes=["mtile"], out=mtile[dn_][j_][:], in_=mtile[dn_][j_][:],
                                 pattern=[[-1, 512]], compare_op=ALU.is_ge, fill=0.0, base=-dl_, channel_multiplier=1)
                HS = [slice(0, 64), slice(64, 128)]

                def load_unit(qc, kc, gc, vcol, want_qf=True, want_qm=True):
                    if want_qf:
                        S.dma("sp", qf[:], qkg_d[qc, :, :], reads=[("qkg", qc)], writes=["qf"])
                    if want_qm:
                        S.dma("sp", qm[0][0:64, :], qkg_d[qc, 0:64, :], reads=[("qkg", qc)], writes=[("qm", 0)])
                        S.dma("sp", qm[1][64:128, :], qkg_d[qc, 64:128, :], reads=[("qkg", qc)], writes=[("qm", 1)])
                    S.dma("sp", kb[:], qkg_d[kc, :, :], reads=[("qkg", kc)], writes=["kb"])
                    if gc is not None:
                        S.dma("sp", gbuf[:], qkg_d[gc, :, :], reads=[("qkg", gc)], writes=["gbuf"])
                    vk = [("v_d", tt, vcol // 512) for tt in range(NKT)]
                    S.dma("sp", vpad[:, :, 0, 0:64], v_d[:, :, vcol:vcol + 64].rearrange("t p c -> p t c"), reads=vk + ["vpad"], writes=[("vpad", 0)])
                    S.dma("sp", vpad[:, :, 1, 64:128], v_d[:, :, vcol + 64:vcol + 128].rearrange("t p c -> p t c"), reads=vk + ["vpad"],
                          writes=[("vpad", 1)])

                def run_jobs(jobs, emit_score, emit_p, emit_av):
                    nj = len(jobs)
                    for idx in range(nj + LA):
                        if idx < nj:
                            emit_score(jobs[idx])
                            emit_p(jobs[idx])
                        if idx >= LA:
                            emit_av(jobs[idx - LA], idx - LA == 0, idx - LA == nj - 1)

                def evac_scaled(b, n, ip, scal):
                    eng = evr.next()
                    if eng == "act":
                        S.op("act", "activation", reads=[PS(b), "rf"], writes=[("pt", ip)], out=pt[ip][:, :n], in_=ps[:, b, :n], func=AF.Identity, scale=scal)
                    else:
                        S.op("dve", "tensor_scalar", reads=[PS(b), "rf"], writes=[("pt", ip)], out=pt[ip][:, :n], in0=ps[:, b, :n], scalar1=scal,
                             scalar2=None, op0=ALU.mult)

                def post_gate(chunk, func, hi, s0, n):
                    i1, i2, i3 = fwr.next(), fwr.next(), fwr.next()
                    hk = ("hacc", hi)
                    S.op("act", "activation", reads=[hk], writes=[("fw", i1)], out=fw[i1][:, :n], in_=hacc[hi][:, :n], func=AF.Square)
                    b = bankrot.next()
                    S.ops("pe", [mmi(ps[:, b, :n], onesbd[:], fw[i1][:, :n], True, True)], reads=[("fw", i1), "onesbd"], writes=[PS(b)])
                    S.op("act", "activation", reads=[PS(b)], writes=[("fw", i2)], out=fw[i2][:, :n], in_=ps[:, b, :n], func=AF.Ln,
                         scale=1.0 / 64, bias=EPS)
                    S.op("act", "activation", reads=[("fw", i2)], writes=[("fw", i2)], out=fw[i2][:, :n], in_=fw[i2][:, :n], func=AF.Exp, scale=-0.5)
                    S.op("act", "activation", reads=["gbuf"], writes=[("fw", i3)], out=fw[i3][:, :n], in_=gbuf[:, s0:s0 + n], func=func)
                    S.op("pool", "tensor_tensor", reads=[hk, ("fw", i2)], writes=[("fw", i2)], out=fw[i2][:, :n], in0=hacc[hi][:, :n],
                         in1=fw[i2][:, :n], op=ALU.mult)
                    S.op("pool", "tensor_tensor", reads=[("fw", i2), ("fw", i3)], writes=tile_keys("hT", s0, n),
                         out=hT[:, chunk, s0:s0 + n], in0=fw[i2][:, :n], in1=fw[i3][:, :n], op=ALU.mult)

                for u in range(2):
                    load_unit(6 + u, 8 + u, 10 + u, 256 + u * 128, want_qm=False)
                    for dn in range(2):
                        for hh in range(2):
                            r = dn * 4 + 2 * u + hh
                            b = bankrot.next()
                            insts = []
                            for qi, (s0, n) in enumerate(QT):
                                c0 = s0 if dn == 0 else s0 + n - 2
                                insts.append(mmi(ps[:, b, 2 * qi:2 * qi + 2], sel[:, r, :], Fq[:, c0:c0 + 2], True, True))
                            S.ops("pe", insts, reads=["sel", "Fq"], writes=[PS(b)])
                            S.op("dve", "tensor_copy", reads=[PS(b)], writes=["fref"], out=fref[:, dn, hh, 0:len(QT)],
                                 in_=ps[:, b, 0:2 * len(QT)].rearrange("p (q two) -> p q two", two=2)[:, :, dn])
                    def ml_pre(s0, n, dn, u=u):
                        qi = QT.index((s0, n))
                        tref = s0 if dn == 0 else s0 + n - 1
                        bF = bankrot.next()
                        S.ops("pe", [mmi(ps[:, bF, :n], sel2[:, dn, u, :], Fq[:, s0:s0 + n], True, True)], reads=["sel2", "Fq"], writes=[PS(bF)])
                        ni = nfrr.next()
                        S.op("dve", "tensor_scalar", reads=[PS(bF)], writes=[("nfr", ni)], out=nfr[ni][:], in0=ps[:, bF, tref - s0:tref - s0 + 1],
                             scalar1=-1.0, scalar2=None, op0=ALU.mult)
                        ci = cfr.next()
                        S.op("act", "activation", reads=[PS(bF), ("nfr", ni)], writes=[("cfa", ci)], out=cfa[ci][:, :n], in_=ps[:, bF, :n],
                             func=AF.Exp, bias=nfr[ni][:])
                        qsl = {}
                        for hh in range(2):
                            i = qsr[hh].next()
                            qsl[hh] = i
                            S.op("pool" if hh == 0 else "dve", "tensor_tensor", reads=["qf", ("cfa", ci)], writes=[("qs", hh, i)],
                                 out=qs[hh][i][HS[hh], :n], in0=qf[HS[hh], s0:s0 + n], in1=cfa[ci][HS[hh], :n], op=ALU.mult)
                        alljobs = []
                        rfi = {}
                        for hh in range(2):
                            r = dn * 4 + 2 * u + hh
                            ri = rfmr.next()
                            rfi[hh] = ri
                            if s0 < LC:
                                rngs = [(0, 2)]
                            elif dn == 0:
                                rngs = [(0, (s0 + n) // 128)]
                            else:
                                rngs = [(0, 2), (s0 // 128, NKT)]
                            for (ka, kb_) in rngs:
                                S.op("act", "activation", reads=["aT", "fref"], writes=[("rfm", ri)], out=rfm[ri][:, ka:kb_], in_=aT[:, ka:kb_, r], func=AF.Exp,
                                     bias=fref[:, dn, hh, qi:qi + 1])
                            if s0 < LC:
                                alljobs += [(hh, 0, True), (hh, 1, True)]
                            else:
                                for kt in range(NKT):
                                    k0 = kt * 128
                                    if kt < 2:
                                        alljobs.append((hh, kt, False))
                                    elif k0 + 127 < s0:
                                        if dn == 0:
                                            alljobs.append((hh, kt, False))
                                    elif k0 > s0 + n - 1:
                                        if dn == 1:
                                            alljobs.append((hh, kt, False))
                                    else:
                                        alljobs.append((hh, kt, True))
                        return qsl, rfi, alljobs

                    pipe = JobPipe(LA)
                    mstages = [(s0, n, dn) for (s0, n) in q_tiles for dn in range(2)]
                    mpre = {0: ml_pre(*mstages[0])}
                    hi = None
                    for sk, (s0, n, dn) in enumerate(mstages):
                        if sk + 1 < len(mstages):
                            mpre[sk + 1] = ml_pre(*mstages[sk + 1])
                        qsl, rfi, alljobs = mpre.pop(sk)
                        if dn == 0:
                            hi = haccr.next()
                        if True:
                            if True:
                                nbk, dbk = accrot.next()
                            state = {}

                            def emit_score(job, n=n, state=state, qsl=qsl):
                                hh, kt, dg = job
                                b = bankrot.next()
                                state[job] = [b, None]
                                i = qsl[hh]
                                S.ops("pe", [mmi(ps[:, b, :n], kb[:, kt * 128:(kt + 1) * 128], qs[hh][i][:, :n], True, True)],
                                      reads=["kb", ("qs", hh, i)], writes=[PS(b)])

                            def emit_p(job, s0=s0, n=n, state=state, rfi=rfi, dn=dn):
                                hh, kt, dg = job
                                b = state[job][0]
                                dl = s0 - kt * 128
                                ip = ptr.next()
                                state[job][1] = ip
                                ri = rfi[hh]
                                if dg:
                                    j_ = (kt * 128 - s0) // 128
                                    S.op("dve", "scalar_tensor_tensor", reads=[PS(b), ("rfm", ri), "mtile"], writes=[("pt", ip)], out=pt[ip][:, :n],
                                         in0=ps[:, b, :n], scalar=rfm[ri][:, kt:kt + 1], in1=mtile[dn][j_][:, :n], op0=ALU.mult, op1=ALU.mult)
                                    return
                                eng = evr.next()
                                if eng == "act":
                                    S.op("act", "activation", reads=[PS(b), ("rfm", ri)], writes=[("pt", ip)], out=pt[ip][:, :n], in_=ps[:, b, :n],
                                         func=AF.Identity, scale=rfm[ri][:, kt:kt + 1])
                                else:
                                    S.op("dve", "tensor_scalar", reads=[PS(b), ("rfm", ri)], writes=[("pt", ip)], out=pt[ip][:, :n], in0=ps[:, b, :n],
                                         scalar1=rfm[ri][:, kt:kt + 1], scalar2=None, op0=ALU.mult)

                            def emit_av(job, first, last, n=n, state=state, nbk=nbk, dbk=dbk):
                                hh, kt, dg = job
                                ip = state[job][1]
                                S.ops("pe", [mmi(ps[:, nbk, :n], vpad[:, kt, hh, :], pt[ip][:, :n], first, last),
                                             mmi(ps[:, dbk, :n], onespad[:, hh, :], pt[ip][:, :n], first, last)],
                                      reads=[("pt", ip), ("vpad", hh), "vpad", "onespad"], writes=[("psacc", nbk), ("psacc", dbk)])

                            def fin(nbk=nbk, dbk=dbk, s0=s0, n=n, dn=dn, hi=hi, u=u):
                                i1, i2 = fwr.next(), fwr.next()
                                S.op("act", "activation", reads=[("psacc", dbk)], writes=[("fw", i1)], out=fw[i1][:, :n], in_=ps[:, dbk, :n], func=AF.Abs)
                                S.op("dve", "tensor_copy", reads=[("psacc", nbk)], writes=[("fw", i2)], out=fw[i2][:, :n], in_=ps[:, nbk, :n])
                                S.op("dve", "tensor_scalar", reads=[("fw", i1)], writes=[("fw", i1)], out=fw[i1][:, :n], in0=fw[i1][:, :n],
                                     scalar1=1.0, scalar2=None, op0=ALU.max)
                                S.op("act", "activation", reads=[("fw", i1)], writes=[("fw", i1)], out=fw[i1][:, :n], in_=fw[i1][:, :n], func=AF.Ln)
                                S.op("act", "activation", reads=[("fw", i1)], writes=[("fw", i1)], out=fw[i1][:, :n], in_=fw[i1][:, :n], func=AF.Exp, scale=-1.0)
                                if dn == 0:
                                    S.op("pool", "tensor_tensor", reads=[("fw", i2), ("fw", i1)], writes=[("hacc", hi)], out=hacc[hi][:, :n],
                                         in0=fw[i2][:, :n], in1=fw[i1][:, :n], op=ALU.mult)
                                else:
                                    S.op("pool", "tensor_tensor", reads=[("fw", i2), ("fw", i1)], writes=[("fw", i1)], out=fw[i1][:, :n],
                                         in0=fw[i2][:, :n], in1=fw[i1][:, :n], op=ALU.mult)
                                    S.op("pool", "tensor_tensor", reads=[("hacc", hi), ("fw", i1)], writes=[("hacc", hi)], out=hacc[hi][:, :n],
                                         in0=hacc[hi][:, :n], in1=fw[i1][:, :n], op=ALU.add)
                                if dn == 1:
                                    post_gate(2 + u, AF.Sigmoid, hi, s0, n)

                            pipe.run_stage(alljobs, emit_score, emit_p, emit_av, fin)
                    pipe.flush()

                if stage == "ml":
                    break

            Ls.close()
            Wo = contextlib.ExitStack()
            wo_a = sb("wo_a", [128, 8, 256], BF16, Wo)
            with S.phase() as ph:
                qm = [sb("qm%d" % i, [128, TT], BF16, ph) for i in range(2)]
                kb = sb("kb", [128, TT], BF16, ph)
                vpad = sb("vpad", [128, NKT, 2, 128], BF16, ph)
                onespad = sb("onespad", [128, 2, 128], BF16, ph)
                pt = [sb("pt%d" % i, [128, 512], BF16, ph) for i in range(NPT)]
                fw = [sb("fw%d" % i, [128, 256], F32, ph) for i in range(4)]
                emb = [[sb("em%d_%d" % (uu, i), [128, 3, 6, 256], BF16, ph) for i in range(2)] for uu in range(2)]
                nmask = sb("nmask", [128, 3, 6, 256], BF16, ph)
                nbb = [sb("nbb%d" % i, [128, 6, 256], BF16, ph) for i in range(2)]
                ptr, fwr = Rot(range(NPT)), Rot(range(4))
                S.op("pool", "memset", writes=["qm0z"], ap=qm[0][64:128, :], constant=0.0)
                S.op("pool", "memset", writes=["qm1z"], ap=qm[1][0:64, :], constant=0.0)
                S.op("pool", "memset", writes=["vpad"], ap=vpad[:], constant=0.0)
                S.op("pool", "memset", writes=["onespad"], ap=onespad[:], constant=0.0)
                S.op("pool", "memset", writes=["onespad"], ap=onespad[:, 0, 0:64], constant=1.0)
                S.op("pool", "memset", writes=["onespad"], ap=onespad[:, 1, 64:128], constant=1.0)
                for gt in range(3):
                    S.dma("pool", nmask[:, gt, :, :], namask_d[:, gt, :, :], writes=[("nmask", gt)])
                em_steps = [(hh_, gt_) for hh_ in range(2) for gt_ in range(3)]
                nbb_of = {}

                def em_dma(u_, k):
                    hh_, gt_ = em_steps[k]
                    i = (u_ * 6 + k) % 2
                    nbb_of[(u_, k)] = i
                    S.dma("pool", nbb[i][:], nab_d[l, 2 * u_ + hh_, :, gt_, :, :], writes=[("nbb", i)])

                def em_build(u_, k):
                    hh_, gt_ = em_steps[k]
                    i = nbb_of[(u_, k)]
                    S.op("act", "activation", reads=[("nbb", i)], writes=[("nbb", i)], out=nbb[i][:], in_=nbb[i][:], func=AF.Exp)
                    S.op("dve", "tensor_tensor", reads=[("nbb", i), ("nmask", gt_)], writes=[("em", u_ % 2, hh_)], out=emb[u_ % 2][hh_][:, gt_, :, :],
                         in0=nbb[i][:], in1=nmask[:, gt_, :, :], op=ALU.mult)

                for k_ in range(6):
                    em_dma(0, k_)
                    em_build(0, k_)
                for u in range(4):
                    load_unit(12 + u, 16 + u, None, 512 + u * 128, want_qf=False)
                    if u == 2:
                        S.dma("pool", wo_a[:], wout_d[l, :, 0:256].rearrange("(kc p) n -> p kc n", p=128), writes=[("wo", 0)])
                    em = emb[u % 2]
                    pipe = JobPipe(LA)
                    groups = [("ctx", None)] if need_ctx else []
                    groups += [("lat", g) for g in range(8)]
                    for gidx_, (kind, g) in enumerate(groups):
                        if u + 1 < 4:
                            if gidx_ < 6:
                                em_dma(u + 1, gidx_)
                            if 1 <= gidx_ < 7:
                                em_build(u + 1, gidx_ - 1)
                        nbk, dbk = accrot.next()
                        if kind == "ctx":
                            s0, n = 0, 256
                            pairs = [((0, 1), None)]
                        else:
                            s0, n = LC + g * 256, 256
                            gt = 0 if g == 0 else (2 if g == 7 else 1)
                            lt0 = 0 if g == 0 else (12 if g == 7 else 2 * g - 2)
                            npair = 3 if gt == 1 else 2
                            pairs = [((0, 1), None)] + [((2 + lt0 + 2 * j, 2 + lt0 + 2 * j + 1), (gt, j)) for j in range(npair)]
                        alljobs = [(hh, kts, emi) for hh in range(2) for (kts, emi) in pairs]
                        state = {}

                        def emit_score(job, s0=s0, n=n, state=state):
                            hh, kts, emi = job
                            b = bankrot.next()
                            state[job] = [b, None]
                            S.ops("pe", [mmi(ps[:, b, 0:256], kb[:, kts[0] * 128:(kts[0] + 1) * 128], qm[hh][:, s0:s0 + n], True, True),
                                         mmi(ps[:, b, 256:512], kb[:, kts[1] * 128:(kts[1] + 1) * 128], qm[hh][:, s0:s0 + n], True, True)],
                                  reads=["kb", ("qm", hh), "qm%dz" % hh], writes=[PS(b)])

                        def emit_p(job, state=state, em=em, u=u):
                            hh, kts, emi = job
                            b = state[job][0]
                            ip = ptr.next()
                            state[job][1] = ip
                            S.op("act", "activation", reads=[PS(b)], writes=[("pt", ip)], out=pt[ip][:], in_=ps[:, b, :], func=AF.Exp, scale=0.125)
                            if emi is not None:
                                gt_, j = emi
                                S.op("dve", "tensor_tensor", reads=[("pt", ip), ("em", u % 2, hh)], writes=[("pt", ip)],
                                     out=pt[ip][:].rearrange("p (a c) -> p a c", a=2), in0=pt[ip][:].rearrange("p (a c) -> p a c", a=2),
                                     in1=em[hh][:, gt_, 2 * j:2 * j + 2, :], op=ALU.mult)

                        def emit_av(job, first, last, state=state, nbk=nbk, dbk=dbk):
                            hh, kts, emi = job
                            ip = state[job][1]
                            insts = []
                            for half in range(2):
                                kt = kts[half]
                                f_ = first and half == 0
                                l_ = last and half == 1
                                insts.append(mmi(ps[:, nbk, 0:256], vpad[:, kt, hh, :], pt[ip][:, half * 256:(half + 1) * 256], f_, l_))
                                insts.append(mmi(ps[:, dbk, 0:256], onespad[:, hh, :], pt[ip][:, half * 256:(half + 1) * 256], f_, l_))
                            S.ops("pe", insts, reads=[("pt", ip), ("vpad", hh), "vpad", "onespad"], writes=[("psacc", nbk), ("psacc", dbk)])

                        def fin(nbk=nbk, dbk=dbk, s0=s0, n=n, u=u):
                            i1, i2 = fwr.next(), fwr.next()
                            S.op("act", "activation", reads=[("psacc", dbk)], writes=[("fw", i1)], out=fw[i1][:, :n], in_=ps[:, dbk, :n], func=AF.Ln)
                            S.op("dve", "tensor_copy", reads=[("psacc", nbk)], writes=[("fw", i2)], out=fw[i2][:, :n], in_=ps[:, nbk, :n])
                            S.op("act", "activation", reads=[("fw", i1)], writes=[("fw", i1)], out=fw[i1][:, :n], in_=fw[i1][:, :n], func=AF.Exp,
                                 scale=-1.0)
                            S.op("dve", "tensor_tensor", reads=[("fw", i2), ("fw", i1)], writes=tile_keys("hT", s0, n),
                                 out=hT[:, 4 + u, s0:s0 + n], in0=fw[i2][:, :n], in1=fw[i1][:, :n], op=ALU.mult)

                        pipe.run_stage(alljobs, emit_score, emit_p, emit_av, fin)
                    pipe.flush()

            if stage == "na":
                break

            with S.phase() as ph:
                wo_b = sb("wo_b", [128, 8, 768], BF16, ph)
                S.dma("pool", wo_b[:], wout_d[l, :, 256:1024].rearrange("(kc p) n -> p kc n", p=128), writes=[("wo", 1)])
                for nn in range(NCH):
                    wo_h = wo_a if nn < 2 else wo_b
                    nh = nn if nn < 2 else nn - 2
                    for (s0, n) in q_tiles:
                        j = 1 if s0 < LC else 0
                        b = bankrot.next()
                        S.ops("pe", [mmi(ps[:, b, :n], wo_h[:, kc, nh * 128:(nh + 1) * 128], hT[:, kc, s0:s0 + n], kc == 0, kc == 7)
                                     for kc in range(8)], reads=[("wo", 0 if nn < 2 else 1)] + tile_keys("hT", s0, n), writes=[PS(b)])
                        S.op("dve", "scalar_tensor_tensor", reads=[PS(b), "mod"] + xres_keys(s0, n, [nn]), writes=xres_keys(s0, n, [nn]),
                             out=xres[:, nn, s0:s0 + n], in0=ps[:, b, :n], scalar=mod[:, 16 + nn, j:j + 1], in1=xres[:, nn, s0:s0 + n],
                             op0=ALU.mult, op1=ALU.add)
            Wo.close()
            if stage == "mid":
                break

            is_moe = (l % 2 == 1)
            with contextlib.ExitStack() as Fs:
                cb = None
                FGM = 4
                wgu = [sb("wgu%d" % i, [128, 8, 2, FGM * 128], BF16, Fs) for i in range(2)]
                Wg0 = moe_wg_d[0] if is_moe else ffn_wg_d
                Wu0 = moe_wu_d[0] if is_moe else ffn_wu_d
                S.dma("pool", wgu[0][:, :, 0, 0:FGM * 128], Wg0[:, 0:FGM * 128].rearrange("(kc p) n -> p kc n", p=128), writes=[("wg", 0)])
                S.dma("pool", wgu[0][:, :, 1, 0:FGM * 128], Wu0[:, 0:FGM * 128].rearrange("(kc p) n -> p kc n", p=128), writes=[("wu", 0)])
                if is_moe:
                    wr = sb("wr", [128, 8, NE], F32, Fs)
                    cb = sb("cb", [128, T], F32, Fs)
                    S.dma("sp", wr[:], moe_wr_d.rearrange("(kc p) e -> p kc e", p=128), writes=["wr"])
                    with S.phase() as ph:
                        lgT = sb("lgT", [8, T], F32, ph)
                        combT = sb("combT", [8, T], F32, ph)
                        ltm = sb("ltm", [128, 16, NE], F32, ph)
                        lt2 = sb("lt2", [128, 16, NE], F32, ph)
                        eq1 = sb("eq1", [128, 16, NE], F32, ph)
                        eq2 = sb("eq2", [128, 16, NE], F32, ph)
                        m1 = sb("m1", [128, 16], F32, ph)
                        m2 = sb("m2", [128, 16], F32, ph)
                        w2 = sb("w2", [128, 16], F32, ph)
                        w1 = sb("w1", [128, 16], F32, ph)
                        rmsnorm_to_hT(1, q_tiles, router=(wr, lgT))
                        bt = 7
                        S.ops("pe", [("transpose", dict(out=ps[:, bt, tt * 8:(tt + 1) * 8], in_=lgT[:, tt * 128:(tt + 1) * 128],
                                                        identity=ident[0:8, 0:8])) for tt in range(16)], reads=["lgT", "ident"], writes=[PS(bt)])
                        S.op("dve", "tensor_copy", reads=[PS(bt)], writes=["ltm"], out=ltm[:].rearrange("p t e -> p (t e)"), in_=ps[:, bt, 0:128])
                        S.op("dve", "tensor_reduce", reads=["ltm"], writes=["m1"], out=m1[:], in_=ltm[:], axis=AX.X, op=ALU.max)
                        S.op("dve", "tensor_tensor", reads=["ltm", "m1"], writes=["eq1"], out=eq1[:], in0=ltm[:],
                             in1=m1[:].unsqueeze(2).to_broadcast([128, 16, NE]), op=ALU.is_equal)
                        S.op("dve", "scalar_tensor_tensor", reads=["eq1", "ltm"], writes=["lt2"], out=lt2[:].rearrange("p t e -> p (t e)"),
                             in0=eq1[:].rearrange("p t e -> p (t e)"), scalar=-1.0e30, in1=ltm[:].rearrange("p t e -> p (t e)"),
                             op0=ALU.mult, op1=ALU.add)
                        S.op("dve", "tensor_reduce", reads=["lt2"], writes=["m2"], out=m2[:], in_=lt2[:], axis=AX.X, op=ALU.max)
                        S.op("dve", "tensor_tensor", reads=["lt2", "m2"], writes=["eq2"], out=eq2[:], in0=lt2[:],
                             in1=m2[:].unsqueeze(2).to_broadcast([128, 16, NE]), op=ALU.is_equal)
                        S.op("dve", "tensor_tensor", reads=["m1", "m2"], writes=["w2"], out=w2[:], in0=m2[:], in1=m1[:], op=ALU.subtract)
                        S.op("act", "activation", reads=["w2"], writes=["w2"], out=w2[:], in_=w2[:], func=AF.Sigmoid)
                        S.op("dve", "tensor_scalar", reads=["w2"], writes=["w1"], out=w1[:], in0=w2[:], scalar1=-1.0, scalar2=1.0,
                             op0=ALU.mult, op1=ALU.add)
                        S.op("dve", "tensor_tensor", reads=["eq1", "w1"], writes=["eq1"], out=eq1[:], in0=eq1[:],
                             in1=w1[:].unsqueeze(2).to_broadcast([128, 16, NE]), op=ALU.mult)
                        S.op("dve", "tensor_tensor", reads=["eq2", "w2"], writes=["eq2"], out=eq2[:], in0=eq2[:],
                             in1=w2[:].unsqueeze(2).to_broadcast([128, 16, NE]), op=ALU.mult)
                        S.op("dve", "tensor_tensor", reads=["eq1", "eq2"], writes=["eq1"], out=eq1[:], in0=eq1[:], in1=eq2[:], op=ALU.add)
                        dbg_dump("comb", eq1[:], ["eq1"])
                        for q4 in range(4):
                            b = bankrot.next()
                            S.ops("pe", [("transpose", dict(out=ps[0:8, b, j_ * 128:(j_ + 1) * 128], in_=eq1[:, q4 * 4 + j_, :], identity=ident[:]))
                                         for j_ in range(4)], reads=["eq1", "ident"], writes=[PS(b)])
                            S.op("act", "activation", reads=[PS(b)], writes=["combT"], out=combT[:, q4 * 512:(q4 + 1) * 512], in_=ps[0:8, b, :],
                                 func=AF.Identity)
                        S.dma("sp", comb_d[:, :], combT[:], reads=["combT"], writes=["comb_d"])
                else:
                    rmsnorm_to_hT(1, q_tiles)
                if stage == "norm2":
                    break

                with S.phase() as ph:
                    FGM = 4
                    fgroups = [(c0, min(FGM, NFC - c0)) for c0 in range(0, NFC, FGM)]
                    wdn = [sb("wdn%d" % i, [128, FGM, D], BF16, ph) for i in range(2)]
                    actb = sb("actb", [128, FGM, TT], BF16, ph)
                    sgb = [sb("sgb%d" % i, [128, 512], BF16, ph) for i in range(2)]
                    tf = [sb("tf%d" % i, [128, 512], F32, ph) for i in range(2)] if is_moe else None
                    sgr, tfr = Rot(range(2)), Rot(range(2))
                    frot = Rot(range(8)) if is_moe else Rot([0, 1, 2, 3, 4, 5, 7])
                    pre_next = (not is_moe) and (l + 1 < n_layers)
                    if pre_next:
                        wmn = [sb("wmn%d" % i, [128, 8, 512], BF16, ph) for i in range(2)]
                        nbper = (12 + len(fgroups) - 1) // len(fgroups)
                    gi = 0
                    for e in range(NE if is_moe else 1):
                        Wg = moe_wg_d[e] if is_moe else ffn_wg_d
                        Wu = moe_wu_d[e] if is_moe else ffn_wu_d
                        Wd = moe_wd_d[e] if is_moe else ffn_wd_d
                        if is_moe:
                            S.dma("sp", cb[:], comb_d[e:e + 1, :].partition_broadcast(128), reads=["comb_d"], writes=["cb"])
                        for (c0, fg) in fgroups:
                            i = gi % 2
                            gi += 1
                            f0 = c0 * 128
                            if not (e == 0 and c0 == 0):
                                S.dma("pool", wgu[i][:, :, 0, 0:fg * 128], Wg[:, f0:f0 + fg * 128].rearrange("(kc p) n -> p kc n", p=128), writes=[("wg", i)])
                                S.dma("pool", wgu[i][:, :, 1, 0:fg * 128], Wu[:, f0:f0 + fg * 128].rearrange("(kc p) n -> p kc n", p=128), writes=[("wu", i)])
                            S.dma("pool", wdn[i][:, 0:fg, :], Wd[f0:f0 + fg * 128, :].rearrange("(fc p) n -> p fc n", p=128), writes=[("wd", i)])
                            nbs = []
                            if pre_next:
                                nbs = list(range((gi - 1) * nbper, min(12, gi * nbper)))
                                for nb in nbs:
                                    adaln_load(l + 1, nb, wmn)
                            for fc in range(fg):
                                for (s0, n) in q_tiles:
                                    bg, bu = frot.next(), frot.next()
                                    S.ops("pe", [mmi(ps[:, bg, :n], wgu[i][:, kc, 0, fc * 128:(fc + 1) * 128], hT[:, kc, s0:s0 + n], kc == 0, kc == 7)
                                                 for kc in range(8)], reads=[("wg", i)] + tile_keys("hT", s0, n), writes=[PS(bg)])
                                    S.ops("pe", [mmi(ps[:, bu, :n], wgu[i][:, kc, 1, fc * 128:(fc + 1) * 128], hT[:, kc, s0:s0 + n], kc == 0, kc == 7)
                                                 for kc in range(8)], reads=[("wu", i)] + tile_keys("hT", s0, n), writes=[PS(bu)])
                                    si = sgr.next()
                                    S.op("act", "activation", reads=[PS(bg)], writes=[("sgb", si)], out=sgb[si][:, :n], in_=ps[:, bg, :n], func=AF.Silu)
                                    if is_moe:
                                        ti_ = tfr.next()
                                        S.op("dve", "tensor_tensor", reads=[PS(bu), ("sgb", si)], writes=[("tf", ti_)], out=tf[ti_][:, :n],
                                             in0=ps[:, bu, :n], in1=sgb[si][:, :n], op=ALU.mult)
                                        S.op("pool", "tensor_tensor", reads=[("tf", ti_), "cb"], writes=[("actb", fc, s0)],
                                             out=actb[:, fc, s0:s0 + n], in0=tf[ti_][:, :n], in1=cb[:, s0 - LC:s0 - LC + n], op=ALU.mult)
                                    else:
                                        S.op("dve", "tensor_tensor", reads=[PS(bu), ("sgb", si)], writes=[("actb", fc, s0)],
                                             out=actb[:, fc, s0:s0 + n], in0=ps[:, bu, :n], in1=sgb[si][:, :n], op=ALU.mult)
                            for (s0, n) in q_tiles:
                                for nn in range(NCH):
                                    j = 1 if s0 < LC else 0
                                    b = frot.next()
                                    S.ops("pe", [mmi(ps[:, b, :n], wdn[i][:, fc, nn * 128:(nn + 1) * 128], actb[:, fc, s0:s0 + n], fc == 0, fc == fg - 1)
                                                 for fc in range(fg)], reads=[("wd", i)] + [("actb", fc, s0) for fc in range(fg)], writes=[PS(b)])
                                    xk_ = xres_keys(s0, n, [nn])
                                    S.op("dve", "scalar_tensor_tensor", reads=[PS(b), "mod"] + xk_, writes=xk_,
                                         out=xres[:, nn, s0:s0 + n], in0=ps[:, b, :n], scalar=mod[:, 40 + nn, j:j + 1], in1=xres[:, nn, s0:s0 + n],
                                         op0=ALU.mult, op1=ALU.add)
                            for nb in nbs:
                                adaln_mm(nb, wmn)
                    if pre_next:
                        adaln_fin(l + 1, modL[l + 1], AvecL[l + 1])
                        adaln_done.add(l + 1)
            if stage == "layer0":
                break

        if Ls_holder:
            Ls_holder[-1].close()
        if dbg and "hT" in dbg:
            for c in range(NCH):
                S.dma("sp", dbg_d["hT"][c, :, :], hT[:, c, :], reads=tile_keys("hT", 0, TT), writes=[("dbgout", "hT", c)])
        if dbg and "mod" in dbg:
            dbg_dump("mod", mod[:], ["mod"])
        if dbg and "xres" in dbg:
            for c in range(NCH):
                S.dma("sp", dbg_d["xres"][c, :, :], xres[:, c, :], reads=xres_keys(0, TT), writes=[("dbgout", "xres", c)])
        if stage == "full":
            with S.phase() as ph:
                xo = [sb("xo%d" % i, [128, D], F32, ph) for i in range(2)]
                for tt in range(2, NKT):
                    buf = xo[tt % 2]
                    key = ("xo", tt % 2)
                    for half in range(2):
                        b = bankrot.next()
                        S.ops("pe", [("transpose", dict(out=ps[:, b, j * 128:(j + 1) * 128],
                                                        in_=xres[:, half * 4 + j, tt * 128:(tt + 1) * 128],
                                                        identity=ident[:])) for j in range(4)],
                              reads=xres_keys(tt * 128, 128) + ["ident"], writes=[PS(b)])
                        if half == 0:
                            S.op("act", "activation", reads=[PS(b)], writes=[key], out=buf[:, 0:512], in_=ps[:, b, :], func=AF.Identity)
                        else:
                            S.op("dve", "tensor_copy", reads=[PS(b)], writes=[key], out=buf[:, 512:1024], in_=ps[:, b, :])
                    S.dma("sp", out_d[(tt - 2) * 128:(tt - 1) * 128, :], buf[:], reads=[key], writes=[("out", tt)])
        S.barrier()
        S.finish()
        global LAST_SCHED
        LAST_SCHED = S
    return nc


def build_rest(L):
    pass


def kernel(**inputs):
    shared, per_core = host_prep(inputs)
    nc = build()
    in_maps = [dict(shared, **m) for m in per_core]
    res = run_bass_kernel_spmd(nc, in_maps, core_ids=list(range(8)))
    return np.stack([r["out"] for r in res.results], axis=0)
```
